# Optimizing a Trainium2 kernel written in Bass

```python
import math
import jax, jax.numpy as jnp
from jax import lax
import numpy as np

D_MODEL = 1024
BATCH = 8
SEQ = 4096
DEPTH = 4

N_EVEN = (DEPTH + 1) // 2
N_ODD = DEPTH // 2
EPS = 1e-6

GM_GROUPS = 4
GM_CH = D_MODEL // 8
GM_WIDTH = GM_GROUPS * GM_CH
GM_CHUNK = 128
POOL_WINDOWS = (2, 4, 8, 16)
POOL_GROUPS = len(POOL_WINDOWS)
POOL_CH = D_MODEL // 8
POOL_WIDTH = POOL_GROUPS * POOL_CH
POOL_MAXW = max(POOL_WINDOWS)
EVEN_IN = 2 * GM_WIDTH + POOL_WIDTH
EVEN_MIX = GM_WIDTH + POOL_WIDTH

DA_HEADS = D_MODEL // 128
DA_QK_DIM = 64
DA_V_DIM = 2 * DA_QK_DIM
DA_Q_WIDTH = DA_HEADS * 2 * DA_QK_DIM
DA_V_WIDTH = DA_HEADS * DA_V_DIM
ODD_IN = 2 * DA_Q_WIDTH + DA_V_WIDTH
Q_BLOCK = 128
ROPE_THETA = 10000.0

PEER_HEADS = 8
PEER_NKEYS = 128
PEER_EXPERTS = PEER_NKEYS * PEER_NKEYS
PEER_QDIM = 256
PEER_HALF = PEER_QDIM // 2
PEER_TOPK = 16
PEER_TOKEN_BLOCK = 128

kernel_name = "hybrid_gmlp_pool_diffattn_peer_adaln"


def rmsnorm(x, g):
    xf = x.astype(jnp.float32)
    y = xf * lax.rsqrt(jnp.mean(xf * xf, axis=-1, keepdims=True) + EPS) * g
    return y.astype(x.dtype)


def apply_rope(t, cos, sin):
    t1, t2 = jnp.split(t, 2, axis=-1)
    return jnp.concatenate([t1 * cos - t2 * sin, t2 * cos + t1 * sin], axis=-1)


def chunked_spatial_gating(u, v, w_s, b_s, g_v):
    B, S, _ = v.shape
    vf = v.astype(jnp.float32)
    mu = jnp.mean(vf, axis=-1, keepdims=True)
    var = jnp.mean((vf - mu) ** 2, axis=-1, keepdims=True)
    vn = ((vf - mu) * lax.rsqrt(var + EPS) * g_v).astype(v.dtype)
    vn = vn.reshape(B, S // GM_CHUNK, GM_CHUNK, GM_GROUPS, GM_CH)
    causal = jnp.tril(jnp.ones((GM_CHUNK, GM_CHUNK), dtype=bool))
    w = jnp.where(causal[None], w_s, 0)
    sv = jnp.einsum('gts,bnsgc->bntgc', w, vn) + b_s.T[None, None, :, :, None]
    return u * sv.reshape(B, S, GM_WIDTH)


def multiscale_pool(p, w_pool, ls):
    B, S, _ = p.shape
    pf = p.astype(jnp.float32)
    cs = jnp.cumsum(pf, axis=1)
    cs_pad = jnp.pad(cs, ((0, 0), (POOL_MAXW, 0), (0, 0)))
    t = jnp.arange(S)
    outs = []
    for g, w in enumerate(POOL_WINDOWS):
        sl = slice(g * POOL_CH, (g + 1) * POOL_CH)
        lag = cs_pad[:, POOL_MAXW - w:POOL_MAXW - w + S, sl]
        cnt = jnp.minimum(t + 1, w).astype(jnp.float32)[None, :, None]
        outs.append((cs[..., sl] - lag) / cnt - pf[..., sl])
    pooled = jnp.stack(outs, axis=2).astype(p.dtype)
    mixed = jnp.einsum('bsgc,gcd->bsgd', pooled, w_pool).reshape(B, S, POOL_WIDTH)
    return mixed * ls


def diff_attention(h, w_in, lam_q1, lam_k1, lam_q2, lam_k2, g_sub, w_out, cos, sin, lam_init):
    B, S, _ = h.shape
    proj = h @ w_in
    q = proj[..., :DA_Q_WIDTH].reshape(B, S, DA_HEADS, 2, DA_QK_DIM)
    k = proj[..., DA_Q_WIDTH:2 * DA_Q_WIDTH].reshape(B, S, DA_HEADS, 2, DA_QK_DIM)
    v = proj[..., 2 * DA_Q_WIDTH:].reshape(B, S, DA_HEADS, DA_V_DIM)
    q = apply_rope(q, cos, sin)
    k = apply_rope(k, cos, sin)
    lam = (jnp.exp(jnp.sum(lam_q1.astype(jnp.float32) * lam_k1.astype(jnp.float32)))
           - jnp.exp(jnp.sum(lam_q2.astype(jnp.float32) * lam_k2.astype(jnp.float32)))
           + lam_init)
    scale = DA_QK_DIM ** -0.5
    outs = []
    for i in range(S // Q_BLOCK):
        kv_len = (i + 1) * Q_BLOCK
        q_i = q[:, i * Q_BLOCK:kv_len]
        s = jnp.einsum('bqhmd,bkhmd->bhmqk', q_i, k[:, :kv_len]).astype(jnp.float32) * scale
        q_pos = i * Q_BLOCK + jnp.arange(Q_BLOCK)
        mask = jnp.arange(kv_len)[None, :] <= q_pos[:, None]
        s = jnp.where(mask, s, -jnp.inf)
        pr = jax.nn.softmax(s, axis=-1)
        a = pr[:, :, 0] - lam * pr[:, :, 1]
        outs.append(jnp.einsum('bhqk,bkhd->bqhd', a.astype(v.dtype), v[:, :kv_len]))
    o = jnp.concatenate(outs, axis=1).astype(jnp.float32)
    o = o * lax.rsqrt(jnp.mean(o * o, axis=-1, keepdims=True) + EPS) * g_sub * (1.0 - lam_init)
    return o.astype(h.dtype).reshape(B, S, DA_V_WIDTH) @ w_out


def peer(h, w_q, sub_keys, u_emb, v_emb):
    B, S, D = h.shape
    T = PEER_TOKEN_BLOCK
    K = PEER_TOPK
    hb = h.reshape(B * S // T, T, D)

    def block(x_t):
        q = jnp.einsum('td,dhk->thk', x_t, w_q).reshape(T, PEER_HEADS, 2, PEER_HALF)
        s = jnp.einsum('thpk,hpnk->thpn', q, sub_keys).astype(jnp.float32)
        sv, si = lax.top_k(s, K)
        cand = (sv[:, :, 0, :, None] + sv[:, :, 1, None, :]).reshape(T, PEER_HEADS, K * K)
        cidx = (si[:, :, 0, :, None] * PEER_NKEYS + si[:, :, 1, None, :]).reshape(T, PEER_HEADS, K * K)
        fv, fpos = lax.top_k(cand, K)
        eidx = jnp.take_along_axis(cidx, fpos, axis=-1)
        gate = jax.nn.softmax(fv, axis=-1)
        u = u_emb[eidx]
        v = v_emb[eidx]
        act = jax.nn.gelu(jnp.einsum('thkd,td->thk', u, x_t).astype(jnp.float32), approximate=False)
        return jnp.einsum('thk,thkd->td', (gate * act).astype(x_t.dtype), v)

    return lax.map(block, hb).reshape(B, S, D)


def setup_inputs(seed: int = 0) -> dict:
    key = jax.random.key(seed)
    ks = jax.random.split(key, 32)
    f32 = jnp.float32
    D = D_MODEL

    def nrm(k, shape, scale):
        return jax.random.normal(k, shape, f32) * scale

    def gain(k, shape):
        return 1.0 + 0.02 * jax.random.normal(k, shape, f32)

    return {
        "x": nrm(ks[0], (BATCH, SEQ, D), 1.0),
        "c": nrm(ks[1], (BATCH, D), 1.0),
        "positions": jnp.tile(jnp.arange(SEQ, dtype=jnp.int32)[None, :], (BATCH, 1)),
        "ada_w": nrm(ks[2], (DEPTH, D, 6 * D), 0.5 * D ** -0.5),
        "ada_b": nrm(ks[3], (DEPTH, 6 * D), 0.02),
        "norm_mix": gain(ks[4], (DEPTH, D)),
        "norm_ffn": gain(ks[5], (DEPTH, D)),
        "ev_w_in": nrm(ks[6], (N_EVEN, D, EVEN_IN), D ** -0.5),
        "ev_g_v": gain(ks[7], (N_EVEN, GM_WIDTH)),
        "ev_w_s": nrm(ks[8], (N_EVEN, GM_GROUPS, GM_CHUNK, GM_CHUNK), GM_CHUNK ** -0.5),
        "ev_b_s": gain(ks[9], (N_EVEN, GM_GROUPS, GM_CHUNK)),
        "ev_w_pool": nrm(ks[10], (N_EVEN, POOL_GROUPS, POOL_CH, POOL_CH), POOL_CH ** -0.5),
        "ev_pool_scale": gain(ks[11], (N_EVEN, POOL_WIDTH)),
        "ev_w_out": nrm(ks[12], (N_EVEN, EVEN_MIX, D), EVEN_MIX ** -0.5),
        "od_w_in": nrm(ks[13], (N_ODD, D, ODD_IN), D ** -0.5),
        "od_lam_q1": nrm(ks[14], (N_ODD, DA_QK_DIM), 0.1),
        "od_lam_k1": nrm(ks[15], (N_ODD, DA_QK_DIM), 0.1),
        "od_lam_q2": nrm(ks[16], (N_ODD, DA_QK_DIM), 0.1),
        "od_lam_k2": nrm(ks[17], (N_ODD, DA_QK_DIM), 0.1),
        "od_g_sub": gain(ks[18], (N_ODD, DA_V_DIM)),
        "od_w_out": nrm(ks[19], (N_ODD, DA_V_WIDTH, D), DA_V_WIDTH ** -0.5),
        "peer_w_q": nrm(ks[20], (DEPTH, D, PEER_HEADS, PEER_QDIM), D ** -0.5),
        "peer_sub_keys": nrm(ks[21], (DEPTH, PEER_HEADS, 2, PEER_NKEYS, PEER_HALF), PEER_HALF ** -0.5),
        "peer_u": nrm(ks[22], (DEPTH, PEER_EXPERTS, D), D ** -0.5),
        "peer_v": nrm(ks[23], (DEPTH, PEER_EXPERTS, D), PEER_HEADS ** -0.5),
        "final_norm": gain(ks[24], (D,)),
    }


def reference(x, c, positions, ada_w, ada_b, norm_mix, norm_ffn,
              ev_w_in, ev_g_v, ev_w_s, ev_b_s, ev_w_pool, ev_pool_scale, ev_w_out,
              od_w_in, od_lam_q1, od_lam_k1, od_lam_q2, od_lam_k2, od_g_sub, od_w_out,
              peer_w_q, peer_sub_keys, peer_u, peer_v, final_norm):
    B, S, D = x.shape
    inv_freq = 1.0 / (ROPE_THETA ** (jnp.arange(0, DA_QK_DIM, 2, dtype=jnp.float32) / DA_QK_DIM))
    ang = positions.astype(jnp.float32)[..., None] * inv_freq
    cos = jnp.cos(ang)[:, :, None, None, :].astype(x.dtype)
    sin = jnp.sin(ang)[:, :, None, None, :].astype(x.dtype)
    c_act = jax.nn.silu(c)
    for l in range(DEPTH):
        mod = (c_act @ ada_w[l] + ada_b[l]).reshape(B, 6, D)[:, :, None, :]
        sh1, sc1, g1, sh2, sc2, g2 = (mod[:, i] for i in range(6))
        h = rmsnorm(x, norm_mix[l]) * (1 + sc1) + sh1
        if l % 2 == 0:
            e = l // 2
            proj = h @ ev_w_in[e]
            z = jax.nn.gelu(proj[..., :2 * GM_WIDTH], approximate=False)
            ya = chunked_spatial_gating(z[..., :GM_WIDTH], z[..., GM_WIDTH:],
                                        ev_w_s[e], ev_b_s[e], ev_g_v[e])
            yb = multiscale_pool(proj[..., 2 * GM_WIDTH:], ev_w_pool[e], ev_pool_scale[e])
            y = jnp.concatenate([ya, yb], axis=-1) @ ev_w_out[e]
        else:
            o = l // 2
            lam_init = 0.8 - 0.6 * math.exp(-0.3 * l)
            y = diff_attention(h, od_w_in[o], od_lam_q1[o], od_lam_k1[o], od_lam_q2[o],
                               od_lam_k2[o], od_g_sub[o], od_w_out[o], cos, sin, lam_init)
        x = x + g1 * y
        h = rmsnorm(x, norm_ffn[l]) * (1 + sc2) + sh2
        x = x + g2 * peer(h, peer_w_q[l], peer_sub_keys[l], peer_u[l], peer_v[l])
    return rmsnorm(x, final_norm)
```

```python
import contextlib
import math
import numpy as np
import concourse.bass as bass
import concourse.mybir as mybir
from concourse.bass_utils import run_bass_kernel_spmd

F32 = mybir.dt.float32
BF16 = mybir.dt.bfloat16
U32 = mybir.dt.uint32
I32 = mybir.dt.int32
AF = mybir.ActivationFunctionType
ALU = mybir.AluOpType
AX = mybir.AxisListType

D = 1024
EPS = 1e-6
NEXP = 16384
ENGS = ("pe", "act", "dve", "pool", "sp")
SAME_ENGINE_SYNC = True


class Sched:
    def __init__(self, nc, st, n_dma_sems=12):
        self.nc = nc
        self.ops = {e: [] for e in ENGS}
        self.cnt = {e: 0 for e in ENGS}
        self.known = {e: {} for e in ENGS}
        self.lastw = {}
        self.reads = {}
        self.n_dma_sems = n_dma_sems
        self.dma_cnt = {}
        self.dma_rr = {e: 0 for e in ENGS}
        self.semh = {}
        for e in ENGS:
            self.semh[e] = st.enter_context(nc.semaphore("s_" + e))
        for q in ("sp", "pool", "act"):
            for i in range(n_dma_sems):
                self.semh[(q, i)] = st.enter_context(nc.semaphore("d_%s%d" % (q, i)))
        self.n_instr = 0

    def _deps(self, eng, reads, writes, skip_same):
        waits = {}

        def need(ev):
            if ev is None:
                return
            k, v = ev
            if k == eng and (skip_same or not SAME_ENGINE_SYNC):
                return
            if self.known[eng].get(k, 0) >= v:
                return
            if waits.get(k, 0) < v:
                waits[k] = v

        for b in reads:
            need(self.lastw.get(b))
        for b in writes:
            need(self.lastw.get(b))
            for ev in self.reads.get(b, ()):
                need(ev)
        for k, v in waits.items():
            self.known[eng][k] = v
        return waits

    def _record(self, ev, reads, writes):
        for b in reads:
            lst = self.reads.setdefault(b, [])
            lst[:] = [x for x in lst if x[0] != ev[0]]
            lst.append(ev)
        for b in writes:
            self.lastw[b] = ev
            self.reads[b] = []

    def op(self, eng, fn, reads=(), writes=(), skip_same=False):
        waits = self._deps(eng, reads, writes, skip_same)
        self.cnt[eng] += 1
        ev = (eng, self.cnt[eng])
        self.ops[eng].append(("op", fn, waits, ev))
        self._record(ev, reads, writes)
        return ev

    def dma(self, queue, fn, reads=(), writes=()):
        i = self.dma_rr[queue]
        self.dma_rr[queue] = (i + 1) % self.n_dma_sems
        key = (queue, i)
        prev = self.dma_cnt.get(key, 0)
        waits = self._deps(queue, reads, writes, False)
        if prev > 0 and self.known[queue].get(key, 0) < prev * 16:
            waits[key] = prev * 16
            self.known[queue][key] = prev * 16
        self.dma_cnt[key] = prev + 1
        ev = (key, (prev + 1) * 16)
        self.ops[queue].append(("dma", fn, waits, ev))
        self._record(ev, reads, writes)
        return ev

    def wait_all(self, eng):
        waits = {}
        for e in ENGS:
            if e != eng and self.cnt[e] > 0:
                waits[e] = self.cnt[e]
        for key, c in self.dma_cnt.items():
            waits[key] = c * 16
        self.ops[eng].append(("wait", None, waits, None))

    def flush(self):
        nc = self.nc
        if not any(self.ops[e] for e in ENGS):
            return
        with nc.Block() as block:
            regs = {"pe": block.tensor, "act": block.scalar, "dve": block.vector,
                    "pool": block.gpsimd, "sp": block.sync}

            def make(e, lst):
                def body(engobj):
                    for kind, fn, waits, ev in lst:
                        for k, v in waits.items():
                            engobj.wait_ge(self.semh[k], v)
                        if kind == "wait":
                            continue
                        ins = fn(engobj)
                        if kind == "dma":
                            ins.then_inc(self.semh[ev[0]], 16)
                        else:
                            ins.then_inc(self.semh[e], 1)
                return body

            for e in ENGS:
                lst = self.ops[e]
                self.n_instr += len(lst)
                if lst:
                    regs[e](make(e, lst))
        self.ops = {e: [] for e in ENGS}


def host_consts():
    ident = np.eye(128, dtype=np.float32)
    p = np.arange(128)[:, None]
    j = np.arange(128)[None, :]
    triu = (j >= p).astype(np.float32)
    poolm = np.zeros((12, 128, 128), np.float32)
    for g, w in enumerate((2, 4, 8, 16)):
        band = ((j - p >= 0) & (j - p <= w - 1)).astype(np.float32)
        poolm[g] = band / w - ident
        cnt = np.minimum(np.arange(128) + 1, w).astype(np.float32)[None, :]
        poolm[4 + g] = band / cnt - ident
        poolm[8 + g] = ((j - p + 128) <= (w - 1)).astype(np.float32) / w
    iota16 = np.tile(np.arange(16, dtype=np.float32)[None, :], (128, 1))
    inv_freq = 1.0 / (10000.0 ** (np.arange(0, 64, 2, dtype=np.float32) / 64.0))
    invf = np.tile((inv_freq / (2.0 * np.pi)).astype(np.float32)[None, :], (128, 1))
    return {"k_ident": ident, "k_triu": triu, "k_poolm": poolm.transpose(1, 0, 2).copy(),
            "k_iota16": iota16, "k_invf": invf}


IN_SHAPES = {
    "ada_w": ([4, 1024, 6144], F32), "ada_b": ([4, 6144], F32),
    "norm_mix": ([4, 1024], F32), "norm_ffn": ([4, 1024], F32),
    "ev_w_in": ([2, 1024, 1536], F32), "ev_g_v": ([2, 512], F32),
    "ev_w_s": ([2, 4, 128, 128], F32), "ev_b_s": ([2, 512], F32),
    "ev_w_pool": ([2, 4, 128, 128], F32), "ev_pool_scale": ([2, 512], F32),
    "ev_w_out": ([2, 1024, 1024], F32), "od_w_in": ([2, 1024, 3072], F32),
    "od_lam_q1": ([2, 64], F32), "od_lam_k1": ([2, 64], F32),
    "od_lam_q2": ([2, 64], F32), "od_lam_k2": ([2, 64], F32),
    "od_g_sub": ([2, 128], F32), "od_w_out": ([2, 1024, 1024], F32),
    "peer_w_q": ([4, 1024, 2048], F32), "peer_sub_keys": ([4, 16, 128, 128], F32),
    "peer_u": ([4, NEXP, 1024], F32), "peer_v": ([4, NEXP, 1024], F32),
    "final_norm": ([1, 1024], F32),
    "k_ident": ([128, 128], F32), "k_triu": ([128, 128], F32), "k_poolm": ([128, 12, 128], F32),
    "k_iota16": ([128, 16], F32), "k_invf": ([128, 32], F32),
}


def build(S=4096, layers=(0, 1, 2, 3), dbg=False, do_mixer=True, do_peer=True, final_norm=True):
    NT = S // 128
    nc = bass.Bass("TRN2", target_bir_lowering=False)
    IN = {}
    IN["x"] = nc.dram_tensor("x", [S, D], F32, kind="ExternalInput").ap()
    IN["c"] = nc.dram_tensor("c", [1, D], F32, kind="ExternalInput").ap()
    IN["positions"] = nc.dram_tensor("positions", [1, S], I32, kind="ExternalInput").ap()
    for k, (shp, dt) in IN_SHAPES.items():
        IN[k] = nc.dram_tensor(k, shp, dt, kind="ExternalInput").ap()
    y_out = nc.dram_tensor("y", [S, D], F32, kind="ExternalOutput").ap()
    xs = [nc.dram_tensor("xs%d" % i, [S, D], F32,
                         kind=("ExternalOutput" if dbg else "Internal")).ap() for i in range(3)]

    with contextlib.ExitStack() as st:
        s = Sched(nc, st)
        for h in s.semh.values():
            nc.gpsimd.sem_clear(h)
        nc.all_engine_barrier()

        uid = [0]

        def T(name, shape, dt=F32, stack=st):
            uid[0] += 1
            return stack.enter_context(nc.sbuf_tensor("%s_%d" % (name, uid[0]), shape, dt))

        def PS(name, shape, dt=F32, stack=st):
            uid[0] += 1
            return stack.enter_context(nc.psum_tensor("%s_%d" % (name, uid[0]), shape, dt))

        def mm(out, lhsT, rhs, start, stop, r, w):
            s.op("pe", lambda e: e.matmul(out, lhsT=lhsT, rhs=rhs, start=start, stop=stop),
                 reads=r, writes=w, skip_same=True)

        def tr(out, in_, ident, r, w):
            s.op("pe", lambda e: e.transpose(out=out, in_=in_, identity=ident),
                 reads=r, writes=w, skip_same=True)

        def ld(out, in_, w, r=(), q="sp", slow=False):
            if slow:
                s.dma(q, lambda e: e.dma_start(out=out, in_=in_, allow_slow_non_contiguous=True), reads=r, writes=w)
            else:
                s.dma(q, lambda e: e.dma_start(out=out, in_=in_), reads=r, writes=w)

        ident_f = T("ident_f", [128, 128]); ident_b = T("ident_b", [128, 128], BF16)
        triu_b = T("triu_b", [128, 128], BF16); triu_f = T("triu_f", [128, 128])
        iota16 = T("iota16", [128, 16])
        ones_row = T("ones_row", [1, 128])
        modbc = [T("modbc%d" % i, [128, D]) for i in range(6)]
        A1, B1, G1, A2, B2, G2 = modbc
        cosT = T("cosT", [128, NT, 32]); sinT = T("sinT", [128, NT, 32]); nsinT = T("nsinT", [128, NT, 32])
        c_act = T("c_act", [128, 8])

        ld(ident_f[:], IN["k_ident"][:, :], ["ident_f"])
        ld(triu_f[:], IN["k_triu"][:, :], ["triu_f"])
        ld(iota16[:], IN["k_iota16"][:, :], ["iota16"])
        s.op("dve", lambda e: e.tensor_copy(out=ident_b[:], in_=ident_f[:]), reads=["ident_f"], writes=["ident_b"])
        s.op("dve", lambda e: e.tensor_copy(out=triu_b[:], in_=triu_f[:]), reads=["triu_f"], writes=["triu_b"])
        s.op("dve", lambda e: e.memset(ones_row[:], 1.0), writes=["ones_row"])

        with contextlib.ExitStack() as ph:
            c_row = T("c_row", [1, D], stack=ph)
            one11 = T("one11", [1, 1], stack=ph)
            pc = PS("pc", [128, 8], stack=ph)
            ld(c_row[:], IN["c"][:, :], ["c_row"])
            s.op("dve", lambda e: e.memset(one11[:], 1.0), writes=["one11"])
            for k in range(8):
                mm(pc[:, k:k + 1], c_row[0:1, k * 128:(k + 1) * 128], one11[0:1, 0:1], True, True,
                   ["c_row", "one11"], ["pc"])
            s.op("act", lambda e: e.activation(out=c_act[:], in_=pc[:], func=AF.Silu), reads=["pc"], writes=["c_act"])
            pos_i = T("pos_i", [128, NT], I32, stack=ph)
            pos_f = T("pos_f", [128, NT], stack=ph)
            invf = T("invf", [128, 32], stack=ph)
            yy = T("yy", [128, NT, 32], stack=ph); y2 = T("y2", [128, NT, 32], stack=ph)
            ki = T("ki", [128, NT, 32], I32, stack=ph); kf = T("kf", [128, NT, 32], stack=ph)
            ld(pos_i[:], IN["positions"][0, :].rearrange("(n p) -> p n", p=128), ["pos_i"], slow=True)
            ld(invf[:], IN["k_invf"][:, :], ["invf"])
            s.op("dve", lambda e: e.tensor_copy(out=pos_f[:], in_=pos_i[:]), reads=["pos_i"], writes=["pos_f"])
            s.op("dve", lambda e: e.tensor_tensor(out=yy[:], in0=pos_f[:].unsqueeze(2).to_broadcast([128, NT, 32]),
                                                  in1=invf[:].unsqueeze(1).to_broadcast([128, NT, 32]), op=ALU.mult),
                 reads=["pos_f", "invf"], writes=["yy"])
            s.op("dve", lambda e: e.tensor_copy(out=ki[:], in_=yy[:]), reads=["yy"], writes=["ki"])
            s.op("dve", lambda e: e.tensor_copy(out=kf[:], in_=ki[:]), reads=["ki"], writes=["kf"])
            s.op("dve", lambda e: e.tensor_tensor(out=y2[:], in0=yy[:], in1=kf[:], op=ALU.subtract), reads=["yy", "kf"], writes=["y2"])
            s.op("act", lambda e: e.activation(out=sinT[:], in_=y2[:], func=AF.Sin, scale=2.0 * math.pi), reads=["y2"], writes=["sinT"])
            s.op("dve", lambda e: e.tensor_scalar(out=nsinT[:], in0=sinT[:], scalar1=-1.0, scalar2=None, op0=ALU.mult), reads=["sinT"], writes=["nsinT"])
            s.op("dve", lambda e: e.tensor_scalar(out=yy[:], in0=yy[:], scalar1=0.25, scalar2=None, op0=ALU.add), reads=["yy"], writes=["yy"])
            s.op("dve", lambda e: e.tensor_copy(out=ki[:], in_=yy[:]), reads=["yy"], writes=["ki"])
            s.op("dve", lambda e: e.tensor_copy(out=kf[:], in_=ki[:]), reads=["ki"], writes=["kf"])
            s.op("dve", lambda e: e.tensor_tensor(out=y2[:], in0=yy[:], in1=kf[:], op=ALU.subtract), reads=["yy", "kf"], writes=["y2"])
            s.op("act", lambda e: e.activation(out=cosT[:], in_=y2[:], func=AF.Sin, scale=2.0 * math.pi), reads=["y2"], writes=["cosT"])
            s.flush()

        def compute_mod(l):
            with contextlib.ExitStack() as ph:
                wt = [T("adaw%d" % i, [128, 8, 512], stack=ph) for i in range(2)]
                brow = T("adab", [1, 6 * D], stack=ph)
                mrow = T("mrow", [1, 6 * D], stack=ph)
                nbc = [T("nbc%d" % i, [128, D], stack=ph) for i in range(2)]
                pm = [PS("pm%d" % i, [1, 512], stack=ph) for i in range(2)]
                pb = [PS("pb%d" % i, [128, 512], stack=ph) for i in range(2)]
                ld(brow[:], IN["ada_b"][l:l + 1, :], ["adab"])
                ld(nbc[0][:], IN["norm_mix"][l, :].partition_broadcast(128), ["nbc0"])
                ld(nbc[1][:], IN["norm_ffn"][l, :].partition_broadcast(128), ["nbc1"])
                for cb in range(12):
                    w = wt[cb % 2]; wk = "adaw%d" % (cb % 2); pk = "pm%d" % (cb % 2)
                    ld(w[:], IN["ada_w"][l, :, cb * 512:(cb + 1) * 512].rearrange("(k p) f -> p k f", p=128), [wk])
                    for k in range(8):
                        mm(pm[cb % 2][:, :], c_act[:, k:k + 1], w[:, k, :], k == 0, k == 7, ["c_act", wk], [pk])
                    s.op("dve", lambda e, cb=cb: e.tensor_tensor(out=mrow[0:1, cb * 512:(cb + 1) * 512], in0=pm[cb % 2][:, :],
                                                                 in1=brow[0:1, cb * 512:(cb + 1) * 512], op=ALU.add),
                         reads=[pk, "adab"], writes=["mrow"])
                dst = {0: (B1, None), 1: (A1, 0), 2: (G1, None), 3: (B2, None), 4: (A2, 1), 5: (G2, None)}
                n = 0
                for i in range(6):
                    tgt, nb = dst[i]
                    for half in range(2):
                        pbk = "pb%d" % (n % 2); pbt = pb[n % 2]; n += 1
                        mm(pbt[:, :], ones_row[0:1, :], mrow[0:1, i * D + half * 512: i * D + (half + 1) * 512], True, True,
                           ["ones_row", "mrow"], [pbk])
                        o = tgt[:, half * 512:(half + 1) * 512]
                        if nb is None:
                            s.op("act", lambda e, o=o, pbt=pbt: e.copy(out=o, in_=pbt[:, :]), reads=[pbk], writes=[tgt.name])
                        else:
                            nbt = nbc[nb][:, half * 512:(half + 1) * 512]
                            s.op("dve", lambda e, o=o, pbt=pbt, nbt=nbt: e.scalar_tensor_tensor(
                                out=o, in0=pbt[:, :], scalar=1.0, in1=nbt, op0=ALU.add, op1=ALU.mult),
                                reads=[pbk, "nbc%d" % nb], writes=[tgt.name])
                s.flush()

        def load_x(xt, key, src, i):
            ld(xt[:], src[i * 128:(i + 1) * 128, :], [key], r=[(src.tensor.name, i)])

        def norm_mod(ph_t, xt, xkey, A, B, want_f32=False):
            junk, ss, rs, hf, hb = ph_t["junk"], ph_t["ss"], ph_t["rs"], ph_t["hf"], ph_t["hb"]
            s.op("act", lambda e: e.activation(out=junk[:], in_=xt[:], func=AF.Square, accum_out=ss[:]),
                 reads=[xkey], writes=["junk", "ss"])
            s.op("act", lambda e: e.activation(out=rs[:], in_=ss[:], func=AF.Sqrt, scale=1.0 / D, bias=EPS),
                 reads=["ss"], writes=["rs"])
            s.op("dve", lambda e: e.reciprocal(out=rs[:], in_=rs[:]), reads=["rs"], writes=["rs"])
            s.op("dve", lambda e: e.scalar_tensor_tensor(out=hf[:], in0=xt[:], scalar=rs[:, 0:1], in1=A[:],
                                                         op0=ALU.mult, op1=ALU.mult),
                 reads=[xkey, "rs", A.name], writes=["hf"])
            if want_f32:
                s.op("pool", lambda e: e.tensor_tensor(out=hf[:], in0=hf[:], in1=B[:], op=ALU.add),
                     reads=["hf", B.name], writes=["hf"])
                s.op("act", lambda e: e.copy(out=hb[:], in_=hf[:]), reads=["hf"], writes=["hb"])
            else:
                s.op("pool", lambda e: e.tensor_tensor(out=hb[:], in0=hf[:], in1=B[:], op=ALU.add),
                     reads=["hf", B.name], writes=["hb"])

        def transpose8(hb, hbkey, tp, hT):
            for k in range(8):
                tr(tp[:, k, :], hb[:, k * 128:(k + 1) * 128], ident_b[:], [hbkey, "ident_b"], ["tp"])
            s.op("act", lambda e: e.copy(out=hT[:], in_=tp[:]), reads=["tp"], writes=["hT"])

        def wload_bf16(dst, key, src, nk=8, parts=4):
            step = nk // parts
            for a in range(parts):
                ld(dst[:, a * step:(a + 1) * step, :],
                   src[a * step * 128:(a + 1) * step * 128, :].rearrange("(k p) f -> p k f", p=128), [key], q="pool")

        def residual_out(pso, psk, Gt, xres, xreskey, xo, dst, i, fin=None):
            for half in range(2):
                sl = slice(half * 512, (half + 1) * 512)
                s.op("dve", lambda e, half=half, sl=sl: e.tensor_tensor(out=xo[:, sl], in0=pso[half][:, :], in1=Gt[:, sl], op=ALU.mult),
                     reads=[psk[half], Gt.name], writes=["xo"])
            s.op("pool", lambda e: e.tensor_tensor(out=xo[:], in0=xo[:], in1=xres[:], op=ALU.add),
                 reads=["xo", xreskey], writes=["xo"])
            if fin is None:
                ld(dst[i * 128:(i + 1) * 128, :], xo[:], [(dst.tensor.name, i)], r=["xo"])
            else:
                junk, ss, rs, fnbc, yo = fin
                s.op("act", lambda e: e.activation(out=junk[:], in_=xo[:], func=AF.Square, accum_out=ss[:]),
                     reads=["xo"], writes=["junk", "ss"])
                s.op("act", lambda e: e.activation(out=rs[:], in_=ss[:], func=AF.Sqrt, scale=1.0 / D, bias=EPS),
                     reads=["ss"], writes=["rs"])
                s.op("dve", lambda e: e.reciprocal(out=rs[:], in_=rs[:]), reads=["rs"], writes=["rs"])
                s.op("dve", lambda e: e.scalar_tensor_tensor(out=yo[:], in0=xo[:], scalar=rs[:, 0:1], in1=fnbc[:],
                                                             op0=ALU.mult, op1=ALU.mult),
                     reads=["xo", "rs", "fnbc"], writes=["yo"])
                ld(dst[i * 128:(i + 1) * 128, :], yo[:], [(dst.tensor.name, i)], r=["yo"])

        def common_tiles(ph):
            d = {}
            d["junk"] = T("junk", [128, D], BF16, stack=ph)
            d["ss"] = T("ss", [128, 1], stack=ph)
            d["rs"] = T("rs", [128, 1], stack=ph)
            d["hf"] = T("hf", [128, D], stack=ph)
            d["hb"] = T("hb", [128, D], BF16, stack=ph)
            d["hT"] = T("hT", [128, 8, 128], BF16, stack=ph)
            d["xo"] = T("xo", [128, D], stack=ph)
            d["xt"] = [T("xt%d" % i, [128, D], stack=ph) for i in range(2)]
            return d

        def even_mixer(l, src, dst):
            e_ = l // 2
            with contextlib.ExitStack() as ph:
                ct = common_tiles(ph)
                Win = T("Win", [128, 8, 1536], BF16, stack=ph)
                Wout = T("Wout", [128, 8, D], BF16, stack=ph)
                wsraw = T("wsraw", [128, 4, 128], stack=ph)
                wsT = T("wsT", [128, 4, 128], BF16, stack=ph)
                bsrow = T("bsrow", [1, 512], stack=ph)
                wpool = T("wpool", [128, 4, 128], BF16, stack=ph)
                gvbc = T("gvbc", [128, 512], stack=ph)
                lscol = T("lscol", [128, 4], stack=ph)
                poolm = T("poolm", [128, 12, 128], stack=ph)
                uTs = T("uTs", [128, 4, 128], stack=ph)
                vs = T("vs", [128, 512], stack=ph)
                vn = T("vn", [128, 512], BF16, stack=ph)
                bst = T("bst", [128, 6], stack=ph); bag = T("bag", [128, 2], stack=ph); sd = T("sd", [128, 1], stack=ph)
                pcur = [T("pcur%d" % i, [128, 512], stack=ph) for i in range(2)]
                pooledT = T("pooledT", [128, 4, 128], BF16, stack=ph)
                yabT = T("yabT", [128, 8, 128], BF16, stack=ph)
                tp = PS("tp", [128, 8, 128], BF16, stack=ph)
                psU = PS("psU", [128, 4, 128], stack=ph)
                psV = PS("psV", [128, 512], stack=ph)
                psP = PS("psP", [128, 512], stack=ph)
                psS = PS("psS", [128, 4, 128], stack=ph)
                psQ = PS("psQ", [128, 4, 128], stack=ph)
                pso = [PS("pso%d" % i, [128, 512], stack=ph) for i in range(2)]

                wload_bf16(Win, "Win", IN["ev_w_in"][e_])
                wload_bf16(Wout, "Wout", IN["ev_w_out"][e_])
                ld(wsraw[:], IN["ev_w_s"][e_].rearrange("g t s -> t g s"), ["wsraw"])
                ld(bsrow[:], IN["ev_b_s"][e_:e_ + 1, :], ["bsrow"])
                ld(wpool[:], IN["ev_w_pool"][e_].rearrange("g c d -> c g d"), ["wpool"], q="pool")
                ld(gvbc[:], IN["ev_g_v"][e_, :].partition_broadcast(128), ["gvbc"])
                ld(lscol[:], IN["ev_pool_scale"][e_, :].rearrange("(g d) -> d g", d=128), ["lscol"], slow=True)
                ld(poolm[:], IN["k_poolm"][:, :, :], ["poolm"])
                for g in range(4):
                    tr(psS[:, g, :], wsraw[:, g, :], ident_f[:], ["wsraw", "ident_f"], ["psS"])
                s.op("dve", lambda e: e.tensor_tensor(out=wsT[:], in0=psS[:], in1=triu_f[:].unsqueeze(1).to_broadcast([128, 4, 128]), op=ALU.mult),
                     reads=["psS", "triu_f"], writes=["wsT"])

                load_x(ct["xt"][0], "xt0", src, 0)
                for i in range(NT):
                    xt = ct["xt"][i % 2]; xk = "xt%d" % (i % 2)
                    if i + 1 < NT:
                        load_x(ct["xt"][(i + 1) % 2], "xt%d" % ((i + 1) % 2), src, i + 1)
                    norm_mod(ct, xt, xk, A1, B1)
                    hb, hT = ct["hb"], ct["hT"]
                    transpose8(hb, "hb", tp, hT)
                    for fc in range(4):
                        for k in range(8):
                            mm(psU[:, fc, :], Win[:, k, fc * 128:(fc + 1) * 128], hT[:, k, :], k == 0, k == 7, ["Win", "hT"], ["psU"])
                    s.op("act", lambda e: e.activation(out=uTs[:], in_=psU[:], func=AF.Gelu), reads=["psU"], writes=["uTs"])
                    for k in range(8):
                        mm(psV[:, :], hT[:, k, :], Win[:, k, 512:1024], k == 0, k == 7, ["Win", "hT"], ["psV"])
                    s.op("act", lambda e: e.activation(out=vs[:], in_=psV[:], func=AF.Gelu), reads=["psV"], writes=["vs"])
                    s.op("dve", lambda e: e.bn_stats(out=bst[:], in_=vs[:]), reads=["vs"], writes=["bst"])
                    s.op("dve", lambda e: e.bn_aggr(out=bag[:], in_=bst[:]), reads=["bst"], writes=["bag"])
                    s.op("act", lambda e: e.activation(out=sd[:], in_=bag[:, 1:2], func=AF.Sqrt, scale=1.0, bias=EPS), reads=["bag"], writes=["sd"])
                    s.op("dve", lambda e: e.reciprocal(out=sd[:], in_=sd[:]), reads=["sd"], writes=["sd"])
                    s.op("dve", lambda e: e.tensor_scalar(out=vs[:], in0=vs[:], scalar1=bag[:, 0:1], scalar2=sd[:, 0:1],
                                                          op0=ALU.subtract, op1=ALU.mult), reads=["vs", "bag", "sd"], writes=["vs"])
                    s.op("pool", lambda e: e.tensor_tensor(out=vn[:], in0=vs[:], in1=gvbc[:], op=ALU.mult), reads=["vs", "gvbc"], writes=["vn"])
                    pc_ = pcur[i % 2]; pck = "pcur%d" % (i % 2); pp_ = pcur[(i + 1) % 2]; ppk = "pcur%d" % ((i + 1) % 2)
                    for k in range(8):
                        mm(psP[:, :], hT[:, k, :], Win[:, k, 1024:1536], k == 0, k == 7, ["Win", "hT"], ["psP"])
                    s.op("act", lambda e, pc_=pc_: e.copy(out=pc_[:], in_=psP[:]), reads=["psP"], writes=[pck])
                    for g in range(4):
                        mm(psS[:, g, :], vn[:, g * 128:(g + 1) * 128], wsT[:, g, :], True, False, ["vn", "wsT"], ["psS"])
                        mm(psS[:, g, :], ones_row[0:1, :], bsrow[0:1, g * 128:(g + 1) * 128], False, True, ["ones_row", "bsrow"], ["psS"])
                    s.op("dve", lambda e: e.tensor_tensor(out=yabT[:, 0:4, :], in0=psS[:], in1=uTs[:], op=ALU.mult),
                         reads=["psS", "uTs"], writes=["yabT"])
                    for g in range(4):
                        gi = (4 + g) if i == 0 else g
                        mm(psQ[:, g, :], pc_[:, g * 128:(g + 1) * 128], poolm[:, gi, :], True, i == 0, [pck, "poolm"], ["psQ"])
                        if i > 0:
                            mm(psQ[:, g, :], pp_[:, g * 128:(g + 1) * 128], poolm[:, 8 + g, :], False, True, [ppk, "poolm"], ["psQ"])
                    s.op("act", lambda e: e.copy(out=pooledT[:], in_=psQ[:]), reads=["psQ"], writes=["pooledT"])
                    for g in range(4):
                        mm(psU[:, g, :], wpool[:, g, :], pooledT[:, g, :], True, True, ["wpool", "pooledT"], ["psU"])
                    for g in range(4):
                        s.op("dve", lambda e, g=g: e.tensor_scalar(out=yabT[:, 4 + g, :], in0=psU[:, g, :], scalar1=lscol[:, g:g + 1],
                                                                   scalar2=None, op0=ALU.mult), reads=["psU", "lscol"], writes=["yabT"])
                    for half in range(2):
                        for fc in range(8):
                            mm(pso[half][:, :], yabT[:, fc, :], Wout[:, fc, half * 512:(half + 1) * 512], fc == 0, fc == 7,
                               ["yabT", "Wout"], ["pso%d" % half])
                    residual_out(pso, ["pso0", "pso1"], G1, xt, xk, ct["xo"], dst, i)
                s.flush()

        def odd_mixer(l, hsrc, rsrc, dst, hg):
            o_ = l // 2
            lam_init = 0.8 - 0.6 * math.exp(-0.3 * l)
            with contextlib.ExitStack() as ph:
                ct = common_tiles(ph)
                Wq = T("Wq_", [128, 8, 512], BF16, stack=ph)
                Wk = T("Wk_", [128, 8, 512], BF16, stack=ph)
                Wv = T("Wv_", [128, 8, 512], BF16, stack=ph)
                Wout = T("Wout", [128, 4, D], BF16, stack=ph)
                KT = T("KT", [128, 4, S], BF16, stack=ph)
                V = T("V", [128, NT, 4, 130], BF16, stack=ph)
                QT = T("QT", [128, 4, 128], BF16, stack=ph)
                lamt = T("lamt", [128, 4, 64], stack=ph)
                lj = T("lj", [128, 64], stack=ph); ld1 = T("ld1", [128, 2], stack=ph)
                nlam = T("nlam", [128, 1], stack=ph)
                gsub = T("gsub", [128, 128], stack=ph)
                xr = [T("xr%d" % i, [128, D], stack=ph) for i in range(2)] if rsrc is not hsrc else None
                ropeA = T("ropeA", [128, 512], stack=ph); ropeB = T("ropeB", [128, 512], stack=ph)
                qr = T("qr", [128, 512], BF16, stack=ph); kr = T("kr", [128, 512], BF16, stack=ph)
                PT = [T("PT%d" % i, [128, 4, 128], BF16, stack=ph) for i in range(2)]
                rz = T("rz", [128, 2], stack=ph); of = T("of", [128, 128], stack=ph); oj = T("oj", [128, 128], stack=ph)
                oss = T("oss", [128, 1], stack=ph)
                Oall = T("Oall", [128, 4, 128], BF16, stack=ph)
                oT = T("oT", [128, 4, 128], BF16, stack=ph)
                tp = PS("tp", [128, 8, 128], BF16, stack=ph)
                psq = PS("psq", [128, 512], stack=ph); psk = PS("psk", [128, 512], stack=ph); psv = PS("psv", [128, 512], stack=ph)
                pss = [PS("pss%d" % i, [128, 4, 128], stack=ph) for i in range(2)]
                pso_ = [PS("psoh%d" % i, [128, 2, 130], stack=ph) for i in range(2)]

                c0 = hg * 512
                wload_bf16(Wq, "Wq_", IN["od_w_in"][o_][:, c0:c0 + 512])
                wload_bf16(Wk, "Wk_", IN["od_w_in"][o_][:, 1024 + c0:1024 + c0 + 512])
                wload_bf16(Wv, "Wv_", IN["od_w_in"][o_][:, 2048 + c0:2048 + c0 + 512])
                wload_bf16(Wout, "Wout", IN["od_w_out"][o_][hg * 512:(hg + 1) * 512, :], nk=4, parts=2)
                for j, nm in enumerate(("od_lam_q1", "od_lam_k1", "od_lam_q2", "od_lam_k2")):
                    ld(lamt[:, j, :], IN[nm][o_, :].partition_broadcast(128), ["lamt"])
                ld(gsub[:], IN["od_g_sub"][o_, :].partition_broadcast(128), ["gsub"])
                s.op("dve", lambda e: e.tensor_scalar(out=gsub[:], in0=gsub[:], scalar1=1.0 - lam_init, scalar2=None, op0=ALU.mult),
                     reads=["gsub"], writes=["gsub"])
                for j in range(2):
                    s.op("dve", lambda e, j=j: e.scalar_tensor_tensor(out=lj[:], in0=lamt[:, 2 * j, :], scalar=1.0, in1=lamt[:, 2 * j + 1, :],
                                                                      op0=ALU.mult, op1=ALU.mult, accum_out=ld1[:, j:j + 1]),
                         reads=["lamt"], writes=["lj", "ld1"])
                s.op("act", lambda e: e.activation(out=ld1[:], in_=ld1[:], func=AF.Exp), reads=["ld1"], writes=["ld1"])
                s.op("dve", lambda e: e.tensor_tensor(out=nlam[:], in0=ld1[:, 1:2], in1=ld1[:, 0:1], op=ALU.subtract), reads=["ld1"], writes=["nlam"])
                s.op("dve", lambda e: e.tensor_scalar(out=nlam[:], in0=nlam[:], scalar1=-lam_init, scalar2=None, op0=ALU.add), reads=["nlam"], writes=["nlam"])
                s.op("dve", lambda e: e.memset(V[:, :, :, 128:130], 1.0), writes=["V"])

                def rope(ps, pskey, out, outkey, i):
                    v4 = lambda ap: ap.rearrange("p (g two i) -> p g two i", g=8, two=2)
                    cb = cosT[:, i, :].unsqueeze(1).unsqueeze(1).to_broadcast([128, 8, 2, 32])
                    sb = sinT[:, i, :].unsqueeze(1).to_broadcast([128, 8, 32])
                    nsb = nsinT[:, i, :].unsqueeze(1).to_broadcast([128, 8, 32])
                    s.op("dve", lambda e: e.tensor_tensor(out=v4(ropeA[:]), in0=v4(ps[:, :]), in1=cb, op=ALU.mult),
                         reads=[pskey, "cosT"], writes=["ropeA"])
                    s.op("dve", lambda e: e.tensor_tensor(out=v4(ropeB[:])[:, :, 0, :], in0=v4(ps[:, :])[:, :, 1, :], in1=nsb, op=ALU.mult),
                         reads=[pskey, "nsinT"], writes=["ropeB"])
                    s.op("dve", lambda e: e.tensor_tensor(out=v4(ropeB[:])[:, :, 1, :], in0=v4(ps[:, :])[:, :, 0, :], in1=sb, op=ALU.mult),
                         reads=[pskey, "sinT"], writes=["ropeB"])
                    s.op("pool", lambda e: e.tensor_tensor(out=out[:], in0=ropeA[:], in1=ropeB[:], op=ALU.add),
                         reads=["ropeA", "ropeB"], writes=[outkey])

                load_x(ct["xt"][0], "xt0", hsrc, 0)
                if xr is not None:
                    load_x(xr[0], "xr0", rsrc, 0)
                for i in range(NT):
                    xt = ct["xt"][i % 2]; xk = "xt%d" % (i % 2)
                    if i + 1 < NT:
                        load_x(ct["xt"][(i + 1) % 2], "xt%d" % ((i + 1) % 2), hsrc, i + 1)
                        if xr is not None:
                            load_x(xr[(i + 1) % 2], "xr%d" % ((i + 1) % 2), rsrc, i + 1)
                    norm_mod(ct, xt, xk, A1, B1)
                    hb, hT = ct["hb"], ct["hT"]
                    transpose8(hb, "hb", tp, hT)
                    for k in range(8):
                        mm(psq[:, :], hT[:, k, :], Wq[:, k, :], k == 0, k == 7, ["Wq_", "hT"], ["psq"])
                    for k in range(8):
                        mm(psk[:, :], hT[:, k, :], Wk[:, k, :], k == 0, k == 7, ["Wk_", "hT"], ["psk"])
                    for k in range(8):
                        mm(psv[:, :], hT[:, k, :], Wv[:, k, :], k == 0, k == 7, ["Wv_", "hT"], ["psv"])
                    rope(psq, "psq", qr, "qr", i)
                    rope(psk, "psk", kr, "kr", i)
                    s.op("act", lambda e, i=i: e.copy(out=V[:, i, :, 0:128], in_=psv[:, :].rearrange("p (h d) -> p h d", h=4)),
                         reads=["psv"], writes=["V"])
                    for hh in range(4):
                        tr(tp[:, hh, :], qr[:, hh * 128:(hh + 1) * 128], ident_b[:], ["qr", "ident_b"], ["tp"])
                    for hh in range(4):
                        tr(tp[:, 4 + hh, :], kr[:, hh * 128:(hh + 1) * 128], ident_b[:], ["kr", "ident_b"], ["tp"])
                    s.op("act", lambda e: e.copy(out=QT[:], in_=tp[:, 0:4, :]), reads=["tp"], writes=["QT"])
                    s.op("act", lambda e, i=i: e.copy(out=KT[:, :, i * 128:(i + 1) * 128], in_=tp[:, 4:8, :]), reads=["tp"], writes=["KT"])
                    cnt = 0
                    for hh in range(4):
                        po = pso_[hh % 2]; pok = "psoh%d" % (hh % 2)
                        for m in range(2):
                            rows = slice(m * 64, (m + 1) * 64)
                            for g0 in range(0, i + 1, 4):
                                kbs = list(range(g0, min(g0 + 4, i + 1)))
                                bank = pss[cnt % 2]; bk = "pss%d" % (cnt % 2)
                                pt = PT[cnt % 2]; ptk = "PT%d" % (cnt % 2)
                                cnt += 1
                                for j, kb in enumerate(kbs):
                                    mm(bank[:, j, :], KT[rows, hh, kb * 128:(kb + 1) * 128], QT[rows, hh, :], True, True, ["KT", "QT"], [bk])
                                n = len(kbs)
                                s.op("act", lambda e, pt=pt, bank=bank, n=n: e.activation(out=pt[:, 0:n, :], in_=bank[:, 0:n, :], func=AF.Exp, scale=0.125),
                                     reads=[bk], writes=[ptk])
                                if i in kbs:
                                    jd = i - g0
                                    s.op("pool", lambda e, pt=pt, jd=jd: e.tensor_tensor(out=pt[:, jd, :], in0=pt[:, jd, :], in1=triu_b[:], op=ALU.mult),
                                         reads=[ptk, "triu_b"], writes=[ptk])
                                for j, kb in enumerate(kbs):
                                    mm(po[:, m, 0:129], pt[:, j, :], V[:, kb, hh, 0:129], kb == 0, kb == i, [ptk, "V"], [pok])
                        s.op("dve", lambda e, po=po: e.reciprocal(out=rz[:], in_=po[:, :, 128]), reads=[pok], writes=["rz"])
                        s.op("dve", lambda e: e.tensor_tensor(out=rz[:, 1:2], in0=rz[:, 1:2], in1=nlam[:], op=ALU.mult), reads=["rz", "nlam"], writes=["rz"])
                        s.op("dve", lambda e, po=po: e.tensor_scalar(out=of[:], in0=po[:, 0, 0:128], scalar1=rz[:, 0:1], scalar2=None, op0=ALU.mult),
                             reads=[pok, "rz"], writes=["of"])
                        s.op("dve", lambda e, po=po: e.scalar_tensor_tensor(out=of[:], in0=po[:, 1, 0:128], scalar=rz[:, 1:2], in1=of[:], op0=ALU.mult, op1=ALU.add),
                             reads=[pok, "rz", "of"], writes=["of"])
                        s.op("act", lambda e: e.activation(out=oj[:], in_=of[:], func=AF.Square, accum_out=oss[:]), reads=["of"], writes=["oj", "oss"])
                        s.op("act", lambda e: e.activation(out=oss[:], in_=oss[:], func=AF.Sqrt, scale=1.0 / 128, bias=EPS), reads=["oss"], writes=["oss"])
                        s.op("dve", lambda e: e.reciprocal(out=oss[:], in_=oss[:]), reads=["oss"], writes=["oss"])
                        s.op("dve", lambda e, hh=hh: e.scalar_tensor_tensor(out=Oall[:, hh, :], in0=of[:], scalar=oss[:, 0:1], in1=gsub[:], op0=ALU.mult, op1=ALU.mult),
                             reads=["of", "oss", "gsub"], writes=["Oall"])
                    for hh in range(4):
                        tr(tp[:, hh, :], Oall[:, hh, :], ident_b[:], ["Oall", "ident_b"], ["tp"])
                    s.op("act", lambda e: e.copy(out=oT[:], in_=tp[:, 0:4, :]), reads=["tp"], writes=["oT"])
                    pso = [psq, psk]
                    for half in range(2):
                        for hh in range(4):
                            mm(pso[half][:, :], oT[:, hh, :], Wout[:, hh, half * 512:(half + 1) * 512], hh == 0, hh == 3,
                               ["oT", "Wout"], [["psq", "psk"][half]])
                    if xr is not None:
                        residual_out(pso, ["psq", "psk"], G1, xr[i % 2], "xr%d" % (i % 2), ct["xo"], dst, i)
                    else:
                        residual_out(pso, ["psq", "psk"], G1, xt, xk, ct["xo"], dst, i)
                s.flush()

        def peer_pass(l, src, dst, fin):
            with contextlib.ExitStack() as ph:
                ct = common_tiles(ph)
                Wq = T("Wqp", [128, 8, 2048], BF16, stack=ph)
                skT = T("skT", [128, 16, 128], stack=ph)
                qT = T("qT", [128, 16, 128], stack=ph)
                sc = T("sc", [128, 16, 128], stack=ph)
                sc2 = T("sc2", [128, 16, 128], stack=ph)
                skraw = sc2
                sv = T("sv", [128, 16, 16], stack=ph)
                si = T("si", [128, 16, 16], U32, stack=ph)
                sif = T("sif", [128, 16, 16], stack=ph)
                cand = sc[:].rearrange("p (h a) n -> p h (a n)", h=8)
                cand2 = sc2[:].rearrange("p (h a) n -> p h (a n)", h=8)
                fv = T("fv", [128, 8, 16], stack=ph)
                fpos = T("fpos", [128, 8, 16], U32, stack=ph)
                fa = T("fa", [128, 8, 16], U32, stack=ph); fb = T("fb", [128, 8, 16], U32, stack=ph)
                faf = T("faf", [128, 8, 16], stack=ph); fbf = T("fbf", [128, 8, 16], stack=ph)
                oh = sc2[:].rearrange("p (h a) (b c) -> p h (a b) c", h=8, c=16)
                Ii = T("Ii", [128, 8, 16], stack=ph); Jj = T("Jj", [128, 8, 16], stack=ph)
                eif = T("eif", [128, 128], stack=ph); eidx = T("eidx", [128, 128], U32, stack=ph)
                ge = T("ge", [128, 8, 16], stack=ph); gz = T("gz", [128, 8], stack=ph)
                gate = T("gate", [128, 128], stack=ph)
                act = T("act", [128, 128], stack=ph)
                wgt = T("wgt", [128, 128], stack=ph)
                NB = 3
                Ug = [T("Ug%d" % i, [128, D], stack=ph) for i in range(NB)]
                Vg = [T("Vg%d" % i, [128, D], stack=ph) for i in range(NB)]
                dj = T("dj", [128, D], stack=ph)
                diag = [T("diag%d" % i, [128, 128], stack=ph) for i in range(2)]
                fin_t = None
                if fin:
                    fnbc = T("fnbc", [128, D], stack=ph)
                    yo = T("yo", [128, D], stack=ph)
                    ld(fnbc[:], IN["final_norm"][0, :].partition_broadcast(128), ["fnbc"])
                    fin_t = (ct["junk"], ct["ss"], ct["rs"], fnbc, yo)
                tp = PS("tp", [128, 8, 128], BF16, stack=ph)
                psq = [PS("psqp%d" % i, [128, 4, 128], stack=ph) for i in range(2)]
                pss = [PS("pssp%d" % i, [128, 4, 128], stack=ph) for i in range(2)]
                pso = [PS("psop%d" % i, [128, 512], stack=ph) for i in range(2)]

                wload_bf16(Wq, "Wqp", IN["peer_w_q"][l])
                ld(skraw[:], IN["peer_sub_keys"][l].rearrange("g n k -> n g k"), ["sc2"])
                for r in range(4):
                    for c_ in range(4):
                        tr(pss[r % 2][:, c_, :], skraw[:, r * 4 + c_, :], ident_f[:], ["sc2", "ident_f"], ["pssp%d" % (r % 2)])
                    s.op("act", lambda e, r=r: e.copy(out=skT[:, r * 4:(r + 1) * 4, :], in_=pss[r % 2][:]), reads=["pssp%d" % (r % 2)], writes=["skT"])

                load_x(ct["xt"][0], "xt0", src, 0)
                for i in range(NT):
                    xt = ct["xt"][i % 2]; xk = "xt%d" % (i % 2)
                    if i + 1 < NT:
                        load_x(ct["xt"][(i + 1) % 2], "xt%d" % ((i + 1) % 2), src, i + 1)
                    norm_mod(ct, xt, xk, A2, B2, want_f32=True)
                    hf, hb, hT = ct["hf"], ct["hb"], ct["hT"]
                    transpose8(hb, "hb", tp, hT)
                    for r in range(4):
                        pq = psq[r % 2]; pqk = "psqp%d" % (r % 2)
                        for c_ in range(4):
                            hp = r * 4 + c_
                            for k in range(8):
                                mm(pq[:, c_, :], Wq[:, k, hp * 128:(hp + 1) * 128], hT[:, k, :], k == 0, k == 7, ["Wqp", "hT"], [pqk])
                        s.op("act", lambda e, r=r, pq=pq: e.copy(out=qT[:, r * 4:(r + 1) * 4, :], in_=pq[:]), reads=[pqk], writes=["qT"])
                    for r in range(4):
                        pz = pss[r % 2]; pzk = "pssp%d" % (r % 2)
                        for c_ in range(4):
                            hp = r * 4 + c_
                            mm(pz[:, c_, :], qT[:, hp, :], skT[:, hp, :], True, True, ["qT", "skT"], [pzk])
                        s.op("act", lambda e, r=r, pz=pz: e.copy(out=sc[:, r * 4:(r + 1) * 4, :], in_=pz[:]), reads=[pzk], writes=["sc"])
                    for g in range(16):
                        s.op("dve", lambda e, g=g: e.max(out=sv[:, g, 0:8], in_=sc[:, g, :]), reads=["sc"], writes=["sv"])
                        s.op("dve", lambda e, g=g: e.max_index(out=si[:, g, 0:8], in_max=sv[:, g, 0:8], in_values=sc[:, g, :]), reads=["sc", "sv"], writes=["si"])
                        s.op("dve", lambda e, g=g: e.match_replace(out=sc2[:, g, :], in_to_replace=sv[:, g, 0:8], in_values=sc[:, g, :], imm_value=-1e30),
                             reads=["sc", "sv"], writes=["sc2"])
                        s.op("dve", lambda e, g=g: e.max(out=sv[:, g, 8:16], in_=sc2[:, g, :]), reads=["sc2"], writes=["sv"])
                        s.op("dve", lambda e, g=g: e.max_index(out=si[:, g, 8:16], in_max=sv[:, g, 8:16], in_values=sc2[:, g, :]), reads=["sc2", "sv"], writes=["si"])
                    s.op("dve", lambda e: e.tensor_copy(out=sif[:], in_=si[:]), reads=["si"], writes=["sif"])
                    sv4 = sv[:].rearrange("p (h two) k -> p h two k", two=2)
                    sif4 = sif[:].rearrange("p (h two) k -> p h two k", two=2)
                    s.op("dve", lambda e: e.tensor_tensor(out=cand.rearrange("p h (a b) -> p h a b", a=16),
                                                          in0=sv4[:, :, 0, :].unsqueeze(3).to_broadcast([128, 8, 16, 16]),
                                                          in1=sv4[:, :, 1, :].unsqueeze(2).to_broadcast([128, 8, 16, 16]), op=ALU.add),
                         reads=["sv"], writes=["sc"])
                    for h in range(8):
                        s.op("dve", lambda e, h=h: e.max(out=fv[:, h, 0:8], in_=cand[:, h, :]), reads=["sc"], writes=["fv"])
                        s.op("dve", lambda e, h=h: e.max_index(out=fpos[:, h, 0:8], in_max=fv[:, h, 0:8], in_values=cand[:, h, :]), reads=["sc", "fv"], writes=["fpos"])
                        s.op("dve", lambda e, h=h: e.match_replace(out=cand2[:, h, :], in_to_replace=fv[:, h, 0:8], in_values=cand[:, h, :], imm_value=-1e30),
                             reads=["sc", "fv"], writes=["sc2"])
                        s.op("dve", lambda e, h=h: e.max(out=fv[:, h, 8:16], in_=cand2[:, h, :]), reads=["sc2"], writes=["fv"])
                        s.op("dve", lambda e, h=h: e.max_index(out=fpos[:, h, 8:16], in_max=fv[:, h, 8:16], in_values=cand2[:, h, :]), reads=["sc2", "fv"], writes=["fpos"])
                    s.op("dve", lambda e: e.tensor_tensor(out=ge[:], in0=fv[:], in1=fv[:, :, 0:1].to_broadcast([128, 8, 16]), op=ALU.subtract),
                         reads=["fv"], writes=["ge"])
                    s.op("act", lambda e: e.activation(out=ge[:], in_=ge[:], func=AF.Exp), reads=["ge"], writes=["ge"])
                    s.op("dve", lambda e: e.tensor_reduce(out=gz[:], in_=ge[:], axis=AX.X, op=ALU.add), reads=["ge"], writes=["gz"])
                    s.op("dve", lambda e: e.reciprocal(out=gz[:], in_=gz[:]), reads=["gz"], writes=["gz"])
                    s.op("dve", lambda e: e.tensor_tensor(out=gate[:].rearrange("p (h k) -> p h k", h=8), in0=ge[:],
                                                          in1=gz[:].unsqueeze(2).to_broadcast([128, 8, 16]), op=ALU.mult),
                         reads=["ge", "gz"], writes=["gate"])
                    s.op("dve", lambda e: e.tensor_single_scalar(out=fa[:], in_=fpos[:], scalar=4, op=ALU.logical_shift_right), reads=["fpos"], writes=["fa"])
                    s.op("dve", lambda e: e.tensor_single_scalar(out=fb[:], in_=fpos[:], scalar=15, op=ALU.bitwise_and), reads=["fpos"], writes=["fb"])
                    s.op("dve", lambda e: e.tensor_copy(out=faf[:], in_=fa[:]), reads=["fa"], writes=["faf"])
                    s.op("dve", lambda e: e.tensor_copy(out=fbf[:], in_=fb[:]), reads=["fb"], writes=["fbf"])
                    io4 = iota16[:].unsqueeze(1).unsqueeze(1).to_broadcast([128, 8, 16, 16])
                    for (srcf, sk_, half, dstt, dk) in ((faf, "faf", 0, Ii, "Ii"), (fbf, "fbf", 1, Jj, "Jj")):
                        s.op("dve", lambda e, srcf=srcf: e.tensor_tensor(out=oh, in0=srcf[:].unsqueeze(3).to_broadcast([128, 8, 16, 16]), in1=io4, op=ALU.is_equal),
                             reads=[sk_, "iota16"], writes=["sc2"])
                        s.op("dve", lambda e, half=half: e.tensor_tensor(out=oh, in0=oh, in1=sif4[:, :, half, :].unsqueeze(2).to_broadcast([128, 8, 16, 16]), op=ALU.mult),
                             reads=["sc2", "sif"], writes=["sc2"])
                        s.op("dve", lambda e, dstt=dstt: e.tensor_reduce(out=dstt[:], in_=oh, axis=AX.X, op=ALU.add), reads=["sc2"], writes=[dk])
                    s.op("dve", lambda e: e.scalar_tensor_tensor(out=eif[:].rearrange("p (h k) -> p h k", h=8), in0=Ii[:], scalar=128.0, in1=Jj[:], op0=ALU.mult, op1=ALU.add),
                         reads=["Ii", "Jj"], writes=["eif"])
                    if l > 0:
                        s.op("dve", lambda e: e.tensor_scalar(out=eif[:], in0=eif[:], scalar1=float(l * NEXP), scalar2=None, op0=ALU.add),
                             reads=["eif"], writes=["eif"])
                    s.op("dve", lambda e: e.tensor_copy(out=eidx[:], in_=eif[:]), reads=["eif"], writes=["eidx"])
                    nu = 0; nv = 0
                    for h in range(8):
                        for k in range(16):
                            sl = h * 16 + k
                            ub = Ug[nu % NB]; ubk = "Ug%d" % (nu % NB); nu += 1
                            s.dma("pool", lambda e, ub=ub, sl=sl: e.indirect_dma_start(
                                out=ub[:], out_offset=None, in_=IN["peer_u"].rearrange("l n d -> (l n) d"),
                                in_offset=bass.IndirectOffsetOnAxis(ap=eidx[:, sl:sl + 1], axis=0)), reads=["eidx"], writes=[ubk])
                            s.op("dve", lambda e, ub=ub, sl=sl: e.scalar_tensor_tensor(out=dj[:], in0=ub[:], scalar=1.0, in1=hf[:], op0=ALU.mult, op1=ALU.mult,
                                                                                       accum_out=act[:, sl:sl + 1]),
                                 reads=[ubk, "hf"], writes=["dj", "act"])
                        hs = slice(h * 16, (h + 1) * 16)
                        s.op("act", lambda e, hs=hs: e.activation(out=wgt[:, hs], in_=act[:, hs], func=AF.Gelu), reads=["act"], writes=["wgt"])
                        s.op("dve", lambda e, hs=hs: e.tensor_tensor(out=wgt[:, hs], in0=wgt[:, hs], in1=gate[:, hs], op=ALU.mult), reads=["wgt", "gate"], writes=["wgt"])
                        for k in range(16):
                            sl = h * 16 + k
                            vb = Vg[nv % NB]; vbk = "Vg%d" % (nv % NB)
                            dg = diag[nv % 2]; dgk = "diag%d" % (nv % 2); nv += 1
                            s.dma("pool", lambda e, vb=vb, sl=sl: e.indirect_dma_start(
                                out=vb[:], out_offset=None, in_=IN["peer_v"].rearrange("l n d -> (l n) d"),
                                in_offset=bass.IndirectOffsetOnAxis(ap=eidx[:, sl:sl + 1], axis=0)), reads=["eidx"], writes=[vbk])
                            s.op("act", lambda e, dg=dg, sl=sl: e.activation(out=dg[:], in_=ident_f[:], func=AF.Copy, scale=wgt[:, sl:sl + 1]),
                                 reads=["ident_f", "wgt"], writes=[dgk])
                            for half in range(2):
                                mm(pso[half][:, :], dg[:], vb[:, half * 512:(half + 1) * 512], sl == 0, sl == 127, [dgk, vbk], ["psop%d" % half])
                    residual_out(pso, ["psop0", "psop1"], G2, xt, xk, ct["xo"], dst, i, fin=fin_t)
                s.flush()

        cur = IN["x"]
        free = [0, 1, 2]

        def take(exclude):
            for b in free:
                if xs[b] is not exclude and all(xs[b] is not e_ for e_ in exclude if e_ is not None):
                    return xs[b]
            raise RuntimeError("no buffer")

        nl = len(layers)
        for li, l in enumerate(layers):
            compute_mod(l)
            if do_mixer:
                if l % 2 == 0:
                    d1 = [b for b in xs if b is not cur][0]
                    even_mixer(l, cur, d1)
                    cur = d1
                else:
                    others = [b for b in xs if b is not cur]
                    odd_mixer(l, cur, cur, others[0], 0)
                    odd_mixer(l, cur, others[0], others[1], 1)
                    cur = others[1]
            if do_peer:
                last = (li == nl - 1)
                d2 = y_out if last else [b for b in xs if b is not cur][0]
                peer_pass(l, cur, d2, fin=(last and final_norm))
                cur = d2
        if cur is not y_out:
            with contextlib.ExitStack() as ph:
                tt = T("cp", [128, D], stack=ph)
                for i in range(NT):
                    ld(tt[:], cur[i * 128:(i + 1) * 128, :], ["cp"], r=[(cur.tensor.name, i)])
                    ld(y_out[i * 128:(i + 1) * 128, :], tt[:], [("y", i)], r=["cp"])
                s.flush()
        s.wait_all("sp")
        s.flush()
        build.n_instr = s.n_instr
    return nc


def make_in_maps(inputs, n_cores, S=4096):
    consts = host_consts()
    shared = {}
    for k in IN_SHAPES:
        if k in consts:
            shared[k] = consts[k]
        else:
            shared[k] = np.ascontiguousarray(np.asarray(inputs[k], dtype=np.float32)).reshape(IN_SHAPES[k][0])
    maps = []
    x = np.asarray(inputs["x"], dtype=np.float32)
    c = np.asarray(inputs["c"], dtype=np.float32)
    pos = np.asarray(inputs["positions"]).astype(np.int32)
    for b in range(n_cores):
        m = dict(shared)
        m["x"] = np.ascontiguousarray(x[b, :S])
        m["c"] = np.ascontiguousarray(c[b:b + 1])
        m["positions"] = np.ascontiguousarray(pos[b:b + 1, :S])
        maps.append(m)
    return maps


def kernel(**inputs):
    n = 8
    nc = build(4096)
    maps = make_in_maps(inputs, n)
    res = run_bass_kernel_spmd(nc, maps, core_ids=list(range(n)))
    return np.stack([np.asarray(r["y"], dtype=np.float32) for r in res.results], axis=0)
```

```python
import contextlib
import math
import numpy as np
import concourse.bass as bass
import concourse.mybir as mybir
from concourse.bass_utils import run_bass_kernel_spmd

F32 = mybir.dt.float32
BF16 = mybir.dt.bfloat16
U32 = mybir.dt.uint32
I32 = mybir.dt.int32
AF = mybir.ActivationFunctionType
ALU = mybir.AluOpType
AX = mybir.AxisListType

D = 1024
EPS = 1e-6
NEXP = 16384
ENGS = ("pe", "act", "dve", "pool", "sp")
SAME_ENGINE_SYNC = True


class Sched:
    def __init__(self, nc, st, n_dma_sems=12):
        self.nc = nc
        self.ops = {e: [] for e in ENGS}
        self.cnt = {e: 0 for e in ENGS}
        self.known = {e: {} for e in ENGS}
        self.lastw = {}
        self.reads = {}
        self.n_dma_sems = n_dma_sems
        self.dma_cnt = {}
        self.dma_rr = {e: 0 for e in ENGS}
        self.semh = {}
        for e in ENGS:
            self.semh[e] = st.enter_context(nc.semaphore("s_" + e))
        for q in ("sp", "pool", "act"):
            for i in range(n_dma_sems):
                self.semh[(q, i)] = st.enter_context(nc.semaphore("d_%s%d" % (q, i)))
        self.n_instr = 0

    def _deps(self, eng, reads, writes, skip_same):
        waits = {}

        def need(ev):
            if ev is None:
                return
            k, v = ev
            if k == eng and (skip_same or not SAME_ENGINE_SYNC):
                return
            if self.known[eng].get(k, 0) >= v:
                return
            if waits.get(k, 0) < v:
                waits[k] = v

        for b in reads:
            need(self.lastw.get(b))
        for b in writes:
            need(self.lastw.get(b))
            for ev in self.reads.get(b, ()):
                need(ev)
        for k, v in waits.items():
            self.known[eng][k] = v
        return waits

    def _record(self, ev, reads, writes):
        for b in reads:
            lst = self.reads.setdefault(b, [])
            lst[:] = [x for x in lst if x[0] != ev[0]]
            lst.append(ev)
        for b in writes:
            self.lastw[b] = ev
            self.reads[b] = []

    def op(self, eng, fn, reads=(), writes=(), skip_same=False):
        waits = self._deps(eng, reads, writes, skip_same)
        self.cnt[eng] += 1
        ev = (eng, self.cnt[eng])
        self.ops[eng].append(("op", fn, waits, ev))
        self._record(ev, reads, writes)
        return ev

    def dma(self, queue, fn, reads=(), writes=()):
        i = self.dma_rr[queue]
        self.dma_rr[queue] = (i + 1) % self.n_dma_sems
        key = (queue, i)
        prev = self.dma_cnt.get(key, 0)
        waits = self._deps(queue, reads, writes, False)
        if prev > 0 and self.known[queue].get(key, 0) < prev * 16:
            waits[key] = prev * 16
            self.known[queue][key] = prev * 16
        self.dma_cnt[key] = prev + 1
        ev = (key, (prev + 1) * 16)
        self.ops[queue].append(("dma", fn, waits, ev))
        self._record(ev, reads, writes)
        return ev

    def wait_all(self, eng):
        waits = {}
        for e in ENGS:
            if e != eng and self.cnt[e] > 0:
                waits[e] = self.cnt[e]
        for key, c in self.dma_cnt.items():
            waits[key] = c * 16
        self.ops[eng].append(("wait", None, waits, None))

    def flush(self):
        nc = self.nc
        if not any(self.ops[e] for e in ENGS):
            return
        with nc.Block() as block:
            regs = {"pe": block.tensor, "act": block.scalar, "dve": block.vector,
                    "pool": block.gpsimd, "sp": block.sync}

            def make(e, lst):
                def body(engobj):
                    for kind, fn, waits, ev in lst:
                        for k, v in waits.items():
                            engobj.wait_ge(self.semh[k], v)
                        if kind == "wait":
                            continue
                        ins = fn(engobj)
                        if kind == "dma":
                            ins.then_inc(self.semh[ev[0]], 16)
                        else:
                            ins.then_inc(self.semh[e], 1)
                return body

            for e in ENGS:
                lst = self.ops[e]
                self.n_instr += len(lst)
                if lst:
                    regs[e](make(e, lst))
        self.ops = {e: [] for e in ENGS}


class Deferred:
    def __init__(self):
        self.q = []

    def op(self, *a, **k):
        self.q.append(("op", a, k))

    def dma(self, *a, **k):
        self.q.append(("dma", a, k))

    def run(self, s, n):
        m = len(self.q) if n is None else min(n, len(self.q))
        for kind, a, k in self.q[:m]:
            getattr(s, kind)(*a, **k)
        del self.q[:m]


def host_consts():
    ident = np.eye(128, dtype=np.float32)
    p = np.arange(128)[:, None]
    j = np.arange(128)[None, :]
    triu = (j >= p).astype(np.float32)
    poolm = np.zeros((12, 128, 128), np.float32)
    for g, w in enumerate((2, 4, 8, 16)):
        band = ((j - p >= 0) & (j - p <= w - 1)).astype(np.float32)
        poolm[g] = band / w - ident
        cnt = np.minimum(np.arange(128) + 1, w).astype(np.float32)[None, :]
        poolm[4 + g] = band / cnt - ident
        poolm[8 + g] = ((j - p + 128) <= (w - 1)).astype(np.float32) / w
    iota16 = np.tile(np.arange(16, dtype=np.float32)[None, :], (128, 1))
    inv_freq = 1.0 / (10000.0 ** (np.arange(0, 64, 2, dtype=np.float32) / 64.0))
    invf = np.tile((inv_freq / (2.0 * np.pi)).astype(np.float32)[None, :], (128, 1))
    return {"k_ident": ident, "k_triu": triu, "k_poolm": poolm.transpose(1, 0, 2).copy(),
            "k_iota16": iota16, "k_invf": invf}


IN_SHAPES = {
    "ada_w": ([4, 1024, 6144], F32), "ada_b": ([4, 6144], F32),
    "norm_mix": ([4, 1024], F32), "norm_ffn": ([4, 1024], F32),
    "ev_w_in": ([2, 1024, 1536], F32), "ev_g_v": ([2, 512], F32),
    "ev_w_s": ([2, 4, 128, 128], F32), "ev_b_s": ([2, 512], F32),
    "ev_w_pool": ([2, 4, 128, 128], F32), "ev_pool_scale": ([2, 512], F32),
    "ev_w_out": ([2, 1024, 1024], F32), "od_w_in": ([2, 1024, 3072], F32),
    "od_lam_q1": ([2, 64], F32), "od_lam_k1": ([2, 64], F32),
    "od_lam_q2": ([2, 64], F32), "od_lam_k2": ([2, 64], F32),
    "od_g_sub": ([2, 128], F32), "od_w_out": ([2, 1024, 1024], F32),
    "peer_w_q": ([4, 1024, 2048], F32), "peer_sub_keys": ([4, 16, 128, 128], F32),
    "peer_u": ([4, NEXP, 1024], F32), "peer_v": ([4, NEXP, 1024], F32),
    "final_norm": ([1, 1024], F32),
    "k_ident": ([128, 128], F32), "k_triu": ([128, 128], F32), "k_poolm": ([128, 12, 128], F32),
    "k_iota16": ([128, 16], F32), "k_invf": ([128, 32], F32),
}


def build(S=4096, layers=(0, 1, 2, 3), dbg=False, do_mixer=True, do_peer=True, final_norm=True):
    NT = S // 128
    nc = bass.Bass("TRN2", target_bir_lowering=False)
    IN = {}
    IN["x"] = nc.dram_tensor("x", [S, D], F32, kind="ExternalInput").ap()
    IN["c"] = nc.dram_tensor("c", [1, D], F32, kind="ExternalInput").ap()
    IN["positions"] = nc.dram_tensor("positions", [1, S], I32, kind="ExternalInput").ap()
    for k, (shp, dt) in IN_SHAPES.items():
        IN[k] = nc.dram_tensor(k, shp, dt, kind="ExternalInput").ap()
    y_out = nc.dram_tensor("y", [S, D], F32, kind="ExternalOutput").ap()
    xs = [nc.dram_tensor("xs%d" % i, [S, D], F32,
                         kind=("ExternalOutput" if dbg else "Internal")).ap() for i in range(3)]

    uvb = nc.dram_tensor("uvb", [4 * NEXP, 2 * D], BF16, kind="Internal").ap()

    with contextlib.ExitStack() as st:
        s = Sched(nc, st)
        for h in s.semh.values():
            nc.gpsimd.sem_clear(h)
        nc.all_engine_barrier()

        uid = [0]

        def T(name, shape, dt=F32, stack=st):
            uid[0] += 1
            return stack.enter_context(nc.sbuf_tensor("%s_%d" % (name, uid[0]), shape, dt))

        def PS(name, shape, dt=F32, stack=st):
            uid[0] += 1
            return stack.enter_context(nc.psum_tensor("%s_%d" % (name, uid[0]), shape, dt))

        sink = [s]

        def S_():
            return sink[0]

        def mm(out, lhsT, rhs, start, stop, r, w):
            sink[0].op("pe", lambda e: e.matmul(out, lhsT=lhsT, rhs=rhs, start=start, stop=stop),
                 reads=r, writes=w, skip_same=True)

        def tr(out, in_, ident, r, w):
            sink[0].op("pe", lambda e: e.transpose(out=out, in_=in_, identity=ident),
                 reads=r, writes=w, skip_same=True)

        def ld(out, in_, w, r=(), q="sp", slow=False):
            if slow:
                sink[0].dma(q, lambda e: e.dma_start(out=out, in_=in_, allow_slow_non_contiguous=True), reads=r, writes=w)
            else:
                sink[0].dma(q, lambda e: e.dma_start(out=out, in_=in_), reads=r, writes=w)

        ident_f = T("ident_f", [128, 128]); ident_b = T("ident_b", [128, 128], BF16)
        triu_b = T("triu_b", [128, 128], BF16); triu_f = T("triu_f", [128, 128])
        iota16 = T("iota16", [128, 16])
        ones_row = T("ones_row", [1, 128])
        modbc = [T("modbc%d" % i, [128, D]) for i in range(6)]
        A1, B1, G1, A2, B2, G2 = modbc
        cosT = T("cosT", [128, NT, 32]); sinT = T("sinT", [128, NT, 32]); nsinT = T("nsinT", [128, NT, 32])
        c_act = T("c_act", [128, 8])

        ld(ident_f[:], IN["k_ident"][:, :], ["ident_f"])
        ld(triu_f[:], IN["k_triu"][:, :], ["triu_f"])
        ld(iota16[:], IN["k_iota16"][:, :], ["iota16"])
        s.op("dve", lambda e: e.tensor_copy(out=ident_b[:], in_=ident_f[:]), reads=["ident_f"], writes=["ident_b"])
        s.op("dve", lambda e: e.tensor_copy(out=triu_b[:], in_=triu_f[:]), reads=["triu_f"], writes=["triu_b"])
        s.op("dve", lambda e: e.memset(ones_row[:], 1.0), writes=["ones_row"])

        if do_peer:
            for l_ in layers:
                for c_ in range(16):
                    r0 = l_ * NEXP + c_ * 1024
                    s.dma("pool", lambda e, l_=l_, c_=c_, r0=r0: e.dma_start(out=uvb[r0:r0 + 1024, 0:D], in_=IN["peer_u"][l_, c_ * 1024:(c_ + 1) * 1024, :]),
                          writes=[("uvb", l_)])
                    s.dma("pool", lambda e, l_=l_, c_=c_, r0=r0: e.dma_start(out=uvb[r0:r0 + 1024, D:2 * D], in_=IN["peer_v"][l_, c_ * 1024:(c_ + 1) * 1024, :]),
                          writes=[("uvb", l_)])

        with contextlib.ExitStack() as ph:
            c_row = T("c_row", [1, D], stack=ph)
            one11 = T("one11", [1, 1], stack=ph)
            pc = PS("pc", [128, 8], stack=ph)
            ld(c_row[:], IN["c"][:, :], ["c_row"])
            s.op("dve", lambda e: e.memset(one11[:], 1.0), writes=["one11"])
            for k in range(8):
                mm(pc[:, k:k + 1], c_row[0:1, k * 128:(k + 1) * 128], one11[0:1, 0:1], True, True,
                   ["c_row", "one11"], ["pc"])
            s.op("act", lambda e: e.activation(out=c_act[:], in_=pc[:], func=AF.Silu), reads=["pc"], writes=["c_act"])
            pos_i = T("pos_i", [128, NT], I32, stack=ph)
            pos_f = T("pos_f", [128, NT], stack=ph)
            invf = T("invf", [128, 32], stack=ph)
            yy = T("yy", [128, NT, 32], stack=ph); y2 = T("y2", [128, NT, 32], stack=ph)
            ki = T("ki", [128, NT, 32], I32, stack=ph); kf = T("kf", [128, NT, 32], stack=ph)
            ld(pos_i[:], IN["positions"][0, :].rearrange("(n p) -> p n", p=128), ["pos_i"], slow=True)
            ld(invf[:], IN["k_invf"][:, :], ["invf"])
            s.op("dve", lambda e: e.tensor_copy(out=pos_f[:], in_=pos_i[:]), reads=["pos_i"], writes=["pos_f"])
            s.op("dve", lambda e: e.tensor_tensor(out=yy[:], in0=pos_f[:].unsqueeze(2).to_broadcast([128, NT, 32]),
                                                  in1=invf[:].unsqueeze(1).to_broadcast([128, NT, 32]), op=ALU.mult),
                 reads=["pos_f", "invf"], writes=["yy"])
            s.op("dve", lambda e: e.tensor_copy(out=ki[:], in_=yy[:]), reads=["yy"], writes=["ki"])
            s.op("dve", lambda e: e.tensor_copy(out=kf[:], in_=ki[:]), reads=["ki"], writes=["kf"])
            s.op("dve", lambda e: e.tensor_tensor(out=y2[:], in0=yy[:], in1=kf[:], op=ALU.subtract), reads=["yy", "kf"], writes=["y2"])
            s.op("act", lambda e: e.activation(out=sinT[:], in_=y2[:], func=AF.Sin, scale=2.0 * math.pi), reads=["y2"], writes=["sinT"])
            s.op("dve", lambda e: e.tensor_scalar(out=nsinT[:], in0=sinT[:], scalar1=-1.0, scalar2=None, op0=ALU.mult), reads=["sinT"], writes=["nsinT"])
            s.op("dve", lambda e: e.tensor_scalar(out=yy[:], in0=yy[:], scalar1=0.25, scalar2=None, op0=ALU.add), reads=["yy"], writes=["yy"])
            s.op("dve", lambda e: e.tensor_copy(out=ki[:], in_=yy[:]), reads=["yy"], writes=["ki"])
            s.op("dve", lambda e: e.tensor_copy(out=kf[:], in_=ki[:]), reads=["ki"], writes=["kf"])
            s.op("dve", lambda e: e.tensor_tensor(out=y2[:], in0=yy[:], in1=kf[:], op=ALU.subtract), reads=["yy", "kf"], writes=["y2"])
            s.op("act", lambda e: e.activation(out=cosT[:], in_=y2[:], func=AF.Sin, scale=2.0 * math.pi), reads=["y2"], writes=["cosT"])
            s.flush()

        def compute_mod(l):
            with contextlib.ExitStack() as ph:
                wt = [T("adaw%d" % i, [128, 8, 512], stack=ph) for i in range(2)]
                brow = T("adab", [1, 6 * D], stack=ph)
                mrow = T("mrow", [1, 6 * D], stack=ph)
                nbc = [T("nbc%d" % i, [128, D], stack=ph) for i in range(2)]
                pm = [PS("pm%d" % i, [1, 512], stack=ph) for i in range(2)]
                pb = [PS("pb%d" % i, [128, 512], stack=ph) for i in range(2)]
                ld(brow[:], IN["ada_b"][l:l + 1, :], ["adab"])
                ld(nbc[0][:], IN["norm_mix"][l, :].partition_broadcast(128), ["nbc0"])
                ld(nbc[1][:], IN["norm_ffn"][l, :].partition_broadcast(128), ["nbc1"])
                for cb in range(12):
                    w = wt[cb % 2]; wk = "adaw%d" % (cb % 2); pk = "pm%d" % (cb % 2)
                    ld(w[:], IN["ada_w"][l, :, cb * 512:(cb + 1) * 512].rearrange("(k p) f -> p k f", p=128), [wk])
                    for k in range(8):
                        mm(pm[cb % 2][:, :], c_act[:, k:k + 1], w[:, k, :], k == 0, k == 7, ["c_act", wk], [pk])
                    s.op("dve", lambda e, cb=cb: e.tensor_tensor(out=mrow[0:1, cb * 512:(cb + 1) * 512], in0=pm[cb % 2][:, :],
                                                                 in1=brow[0:1, cb * 512:(cb + 1) * 512], op=ALU.add),
                         reads=[pk, "adab"], writes=["mrow"])
                dst = {0: (B1, None), 1: (A1, 0), 2: (G1, None), 3: (B2, None), 4: (A2, 1), 5: (G2, None)}
                n = 0
                for i in range(6):
                    tgt, nb = dst[i]
                    for half in range(2):
                        pbk = "pb%d" % (n % 2); pbt = pb[n % 2]; n += 1
                        mm(pbt[:, :], ones_row[0:1, :], mrow[0:1, i * D + half * 512: i * D + (half + 1) * 512], True, True,
                           ["ones_row", "mrow"], [pbk])
                        o = tgt[:, half * 512:(half + 1) * 512]
                        if nb is None:
                            s.op("act", lambda e, o=o, pbt=pbt: e.copy(out=o, in_=pbt[:, :]), reads=[pbk], writes=[tgt.name])
                        else:
                            nbt = nbc[nb][:, half * 512:(half + 1) * 512]
                            s.op("dve", lambda e, o=o, pbt=pbt, nbt=nbt: e.scalar_tensor_tensor(
                                out=o, in0=pbt[:, :], scalar=1.0, in1=nbt, op0=ALU.add, op1=ALU.mult),
                                reads=[pbk, "nbc%d" % nb], writes=[tgt.name])
                s.flush()

        def load_x(xt, key, src, i):
            ld(xt[:], src[i * 128:(i + 1) * 128, :], [key], r=[(src.tensor.name, i)])

        def norm_mod(ph_t, xt, xkey, A, B, want_f32=False):
            junk, ss, rs, hf, hb = ph_t["junk"], ph_t["ss"], ph_t["rs"], ph_t["hf"], ph_t["hb"]
            s.op("act", lambda e: e.activation(out=junk[:], in_=xt[:], func=AF.Square, accum_out=ss[:]),
                 reads=[xkey], writes=["junk", "ss"])
            s.op("act", lambda e: e.activation(out=rs[:], in_=ss[:], func=AF.Sqrt, scale=1.0 / D, bias=EPS),
                 reads=["ss"], writes=["rs"])
            s.op("dve", lambda e: e.reciprocal(out=rs[:], in_=rs[:]), reads=["rs"], writes=["rs"])
            s.op("dve", lambda e: e.scalar_tensor_tensor(out=hf[:], in0=xt[:], scalar=rs[:, 0:1], in1=A[:],
                                                         op0=ALU.mult, op1=ALU.mult),
                 reads=[xkey, "rs", A.name], writes=["hf"])
            if want_f32:
                s.op("pool", lambda e: e.tensor_tensor(out=hf[:], in0=hf[:], in1=B[:], op=ALU.add),
                     reads=["hf", B.name], writes=["hf"])
                s.op("act", lambda e: e.copy(out=hb[:], in_=hf[:]), reads=["hf"], writes=["hb"])
            else:
                s.op("pool", lambda e: e.tensor_tensor(out=hb[:], in0=hf[:], in1=B[:], op=ALU.add),
                     reads=["hf", B.name], writes=["hb"])

        def transpose8(hb, hbkey, tp, hT):
            for k in range(8):
                tr(tp[:, k, :], hb[:, k * 128:(k + 1) * 128], ident_b[:], [hbkey, "ident_b"], ["tp"])
            s.op("act", lambda e: e.copy(out=hT[:], in_=tp[:]), reads=["tp"], writes=["hT"])

        def wload_bf16(dst, key, src, nk=8, parts=4):
            step = nk // parts
            for a in range(parts):
                ld(dst[:, a * step:(a + 1) * step, :],
                   src[a * step * 128:(a + 1) * step * 128, :].rearrange("(k p) f -> p k f", p=128), [key], q="pool")

        def residual_out(pso, psk, Gt, xres, xreskey, xo, dst, i, fin=None):
            for half in range(2):
                sl = slice(half * 512, (half + 1) * 512)
                s.op("dve", lambda e, half=half, sl=sl: e.tensor_tensor(out=xo[:, sl], in0=pso[half][:, :], in1=Gt[:, sl], op=ALU.mult),
                     reads=[psk[half], Gt.name], writes=["xo"])
            s.op("pool", lambda e: e.tensor_tensor(out=xo[:], in0=xo[:], in1=xres[:], op=ALU.add),
                 reads=["xo", xreskey], writes=["xo"])
            if fin is None:
                ld(dst[i * 128:(i + 1) * 128, :], xo[:], [(dst.tensor.name, i)], r=["xo"])
            else:
                junk, ss, rs, fnbc, yo = fin
                s.op("act", lambda e: e.activation(out=junk[:], in_=xo[:], func=AF.Square, accum_out=ss[:]),
                     reads=["xo"], writes=["junk", "ss"])
                s.op("act", lambda e: e.activation(out=rs[:], in_=ss[:], func=AF.Sqrt, scale=1.0 / D, bias=EPS),
                     reads=["ss"], writes=["rs"])
                s.op("dve", lambda e: e.reciprocal(out=rs[:], in_=rs[:]), reads=["rs"], writes=["rs"])
                s.op("dve", lambda e: e.scalar_tensor_tensor(out=yo[:], in0=xo[:], scalar=rs[:, 0:1], in1=fnbc[:],
                                                             op0=ALU.mult, op1=ALU.mult),
                     reads=["xo", "rs", "fnbc"], writes=["yo"])
                ld(dst[i * 128:(i + 1) * 128, :], yo[:], [(dst.tensor.name, i)], r=["yo"])

        def common_tiles(ph):
            d = {}
            d["junk"] = T("junk", [128, D], BF16, stack=ph)
            d["ss"] = T("ss", [128, 1], stack=ph)
            d["rs"] = T("rs", [128, 1], stack=ph)
            d["hf"] = T("hf", [128, D], stack=ph)
            d["hb"] = T("hb", [128, D], BF16, stack=ph)
            d["hT"] = T("hT", [128, 8, 128], BF16, stack=ph)
            d["xo"] = T("xo", [128, D], stack=ph)
            d["xt"] = [T("xt%d" % i, [128, D], stack=ph) for i in range(2)]
            return d

        def even_mixer(l, src, dst):
            e_ = l // 2
            with contextlib.ExitStack() as ph:
                ct = common_tiles(ph)
                Win = T("Win", [128, 8, 1536], BF16, stack=ph)
                Wout = T("Wout", [128, 8, D], BF16, stack=ph)
                wsraw = T("wsraw", [128, 4, 128], stack=ph)
                wsT = T("wsT", [128, 4, 128], BF16, stack=ph)
                bsrow = T("bsrow", [1, 512], stack=ph)
                wpool = T("wpool", [128, 4, 128], BF16, stack=ph)
                gvbc = T("gvbc", [128, 512], stack=ph)
                lscol = T("lscol", [128, 4], stack=ph)
                poolm = T("poolm", [128, 12, 128], stack=ph)
                uTs = T("uTs", [128, 4, 128], stack=ph)
                vs = T("vs", [128, 512], stack=ph)
                vn = T("vn", [128, 512], BF16, stack=ph)
                bst = T("bst", [128, 6], stack=ph); bag = T("bag", [128, 2], stack=ph); sd = T("sd", [128, 1], stack=ph)
                pcur = [T("pcur%d" % i, [128, 512], stack=ph) for i in range(2)]
                pooledT = T("pooledT", [128, 4, 128], BF16, stack=ph)
                yabT = T("yabT", [128, 8, 128], BF16, stack=ph)
                tp = PS("tp", [128, 8, 128], BF16, stack=ph)
                psU = PS("psU", [128, 4, 128], stack=ph)
                psV = PS("psV", [128, 512], stack=ph)
                psP = PS("psP", [128, 512], stack=ph)
                psS = PS("psS", [128, 4, 128], stack=ph)
                psQ = PS("psQ", [128, 4, 128], stack=ph)
                pso = [PS("pso%d" % i, [128, 512], stack=ph) for i in range(2)]

                wload_bf16(Win, "Win", IN["ev_w_in"][e_])
                wload_bf16(Wout, "Wout", IN["ev_w_out"][e_])
                ld(wsraw[:], IN["ev_w_s"][e_].rearrange("g t s -> t g s"), ["wsraw"])
                ld(bsrow[:], IN["ev_b_s"][e_:e_ + 1, :], ["bsrow"])
                ld(wpool[:], IN["ev_w_pool"][e_].rearrange("g c d -> c g d"), ["wpool"], q="pool")
                ld(gvbc[:], IN["ev_g_v"][e_, :].partition_broadcast(128), ["gvbc"])
                ld(lscol[:], IN["ev_pool_scale"][e_, :].rearrange("(g d) -> d g", d=128), ["lscol"], slow=True)
                ld(poolm[:], IN["k_poolm"][:, :, :], ["poolm"])
                for g in range(4):
                    tr(psS[:, g, :], wsraw[:, g, :], ident_f[:], ["wsraw", "ident_f"], ["psS"])
                s.op("dve", lambda e: e.tensor_tensor(out=wsT[:], in0=psS[:], in1=triu_f[:].unsqueeze(1).to_broadcast([128, 4, 128]), op=ALU.mult),
                     reads=["psS", "triu_f"], writes=["wsT"])

                load_x(ct["xt"][0], "xt0", src, 0)
                for i in range(NT):
                    xt = ct["xt"][i % 2]; xk = "xt%d" % (i % 2)
                    if i + 1 < NT:
                        load_x(ct["xt"][(i + 1) % 2], "xt%d" % ((i + 1) % 2), src, i + 1)
                    norm_mod(ct, xt, xk, A1, B1)
                    hb, hT = ct["hb"], ct["hT"]
                    transpose8(hb, "hb", tp, hT)
                    for fc in range(4):
                        for k in range(8):
                            mm(psU[:, fc, :], Win[:, k, fc * 128:(fc + 1) * 128], hT[:, k, :], k == 0, k == 7, ["Win", "hT"], ["psU"])
                    s.op("act", lambda e: e.activation(out=uTs[:], in_=psU[:], func=AF.Gelu), reads=["psU"], writes=["uTs"])
                    for k in range(8):
                        mm(psV[:, :], hT[:, k, :], Win[:, k, 512:1024], k == 0, k == 7, ["Win", "hT"], ["psV"])
                    s.op("act", lambda e: e.activation(out=vs[:], in_=psV[:], func=AF.Gelu), reads=["psV"], writes=["vs"])
                    s.op("dve", lambda e: e.bn_stats(out=bst[:], in_=vs[:]), reads=["vs"], writes=["bst"])
                    s.op("dve", lambda e: e.bn_aggr(out=bag[:], in_=bst[:]), reads=["bst"], writes=["bag"])
                    s.op("act", lambda e: e.activation(out=sd[:], in_=bag[:, 1:2], func=AF.Sqrt, scale=1.0, bias=EPS), reads=["bag"], writes=["sd"])
                    s.op("dve", lambda e: e.reciprocal(out=sd[:], in_=sd[:]), reads=["sd"], writes=["sd"])
                    s.op("dve", lambda e: e.tensor_scalar(out=vs[:], in0=vs[:], scalar1=bag[:, 0:1], scalar2=sd[:, 0:1],
                                                          op0=ALU.subtract, op1=ALU.mult), reads=["vs", "bag", "sd"], writes=["vs"])
                    s.op("pool", lambda e: e.tensor_tensor(out=vn[:], in0=vs[:], in1=gvbc[:], op=ALU.mult), reads=["vs", "gvbc"], writes=["vn"])
                    pc_ = pcur[i % 2]; pck = "pcur%d" % (i % 2); pp_ = pcur[(i + 1) % 2]; ppk = "pcur%d" % ((i + 1) % 2)
                    for k in range(8):
                        mm(psP[:, :], hT[:, k, :], Win[:, k, 1024:1536], k == 0, k == 7, ["Win", "hT"], ["psP"])
                    s.op("act", lambda e, pc_=pc_: e.copy(out=pc_[:], in_=psP[:]), reads=["psP"], writes=[pck])
                    for g in range(4):
                        mm(psS[:, g, :], vn[:, g * 128:(g + 1) * 128], wsT[:, g, :], True, False, ["vn", "wsT"], ["psS"])
                        mm(psS[:, g, :], ones_row[0:1, :], bsrow[0:1, g * 128:(g + 1) * 128], False, True, ["ones_row", "bsrow"], ["psS"])
                    s.op("dve", lambda e: e.tensor_tensor(out=yabT[:, 0:4, :], in0=psS[:], in1=uTs[:], op=ALU.mult),
                         reads=["psS", "uTs"], writes=["yabT"])
                    for g in range(4):
                        gi = (4 + g) if i == 0 else g
                        mm(psQ[:, g, :], pc_[:, g * 128:(g + 1) * 128], poolm[:, gi, :], True, i == 0, [pck, "poolm"], ["psQ"])
                        if i > 0:
                            mm(psQ[:, g, :], pp_[:, g * 128:(g + 1) * 128], poolm[:, 8 + g, :], False, True, [ppk, "poolm"], ["psQ"])
                    s.op("act", lambda e: e.copy(out=pooledT[:], in_=psQ[:]), reads=["psQ"], writes=["pooledT"])
                    for g in range(4):
                        mm(psU[:, g, :], wpool[:, g, :], pooledT[:, g, :], True, True, ["wpool", "pooledT"], ["psU"])
                    for g in range(4):
                        s.op("dve", lambda e, g=g: e.tensor_scalar(out=yabT[:, 4 + g, :], in0=psU[:, g, :], scalar1=lscol[:, g:g + 1],
                                                                   scalar2=None, op0=ALU.mult), reads=["psU", "lscol"], writes=["yabT"])
                    for half in range(2):
                        for fc in range(8):
                            mm(pso[half][:, :], yabT[:, fc, :], Wout[:, fc, half * 512:(half + 1) * 512], fc == 0, fc == 7,
                               ["yabT", "Wout"], ["pso%d" % half])
                    residual_out(pso, ["pso0", "pso1"], G1, xt, xk, ct["xo"], dst, i)
                s.flush()

        def odd_mixer(l, hsrc, rsrc, dst, hg):
            o_ = l // 2
            lam_init = 0.8 - 0.6 * math.exp(-0.3 * l)
            with contextlib.ExitStack() as ph:
                ct = common_tiles(ph)
                Wq = T("Wq_", [128, 8, 512], BF16, stack=ph)
                Wk = T("Wk_", [128, 8, 512], BF16, stack=ph)
                Wv = T("Wv_", [128, 8, 512], BF16, stack=ph)
                Wout = T("Wout", [128, 4, D], BF16, stack=ph)
                KT = T("KT", [128, 4, S], BF16, stack=ph)
                V = T("V", [128, NT, 4, 130], BF16, stack=ph)
                QT = T("QT", [128, 4, 128], BF16, stack=ph)
                lamt = T("lamt", [128, 4, 64], stack=ph)
                lj = T("lj", [128, 64], stack=ph); ld1 = T("ld1", [128, 2], stack=ph)
                nlam = T("nlam", [128, 1], stack=ph)
                gsub = T("gsub", [128, 128], stack=ph)
                xr = [T("xr%d" % i, [128, D], stack=ph) for i in range(2)] if rsrc is not hsrc else None
                ropeA = T("ropeA", [128, 512], stack=ph); ropeB = T("ropeB", [128, 512], stack=ph)
                qr = T("qr", [128, 512], BF16, stack=ph); kr = T("kr", [128, 512], BF16, stack=ph)
                PT = [T("PT%d" % i, [128, 4, 128], BF16, stack=ph) for i in range(2)]
                rz = T("rz", [128, 2], stack=ph); of = T("of", [128, 128], stack=ph); oj = T("oj", [128, 128], stack=ph)
                oss = T("oss", [128, 1], stack=ph)
                Oall = T("Oall", [128, 4, 128], BF16, stack=ph)
                oT = T("oT", [128, 4, 128], BF16, stack=ph)
                tp = PS("tp", [128, 8, 128], BF16, stack=ph)
                psq = PS("psq", [128, 512], stack=ph); psk = PS("psk", [128, 512], stack=ph); psv = PS("psv", [128, 512], stack=ph)
                pss = [PS("pss%d" % i, [128, 4, 128], stack=ph) for i in range(2)]
                pso_ = [PS("psoh%d" % i, [128, 2, 130], stack=ph) for i in range(2)]

                c0 = hg * 512
                wload_bf16(Wq, "Wq_", IN["od_w_in"][o_][:, c0:c0 + 512])
                wload_bf16(Wk, "Wk_", IN["od_w_in"][o_][:, 1024 + c0:1024 + c0 + 512])
                wload_bf16(Wv, "Wv_", IN["od_w_in"][o_][:, 2048 + c0:2048 + c0 + 512])
                wload_bf16(Wout, "Wout", IN["od_w_out"][o_][hg * 512:(hg + 1) * 512, :], nk=4, parts=2)
                for j, nm in enumerate(("od_lam_q1", "od_lam_k1", "od_lam_q2", "od_lam_k2")):
                    ld(lamt[:, j, :], IN[nm][o_, :].partition_broadcast(128), ["lamt"])
                ld(gsub[:], IN["od_g_sub"][o_, :].partition_broadcast(128), ["gsub"])
                s.op("dve", lambda e: e.tensor_scalar(out=gsub[:], in0=gsub[:], scalar1=1.0 - lam_init, scalar2=None, op0=ALU.mult),
                     reads=["gsub"], writes=["gsub"])
                for j in range(2):
                    s.op("dve", lambda e, j=j: e.scalar_tensor_tensor(out=lj[:], in0=lamt[:, 2 * j, :], scalar=1.0, in1=lamt[:, 2 * j + 1, :],
                                                                      op0=ALU.mult, op1=ALU.mult, accum_out=ld1[:, j:j + 1]),
                         reads=["lamt"], writes=["lj", "ld1"])
                s.op("act", lambda e: e.activation(out=ld1[:], in_=ld1[:], func=AF.Exp), reads=["ld1"], writes=["ld1"])
                s.op("dve", lambda e: e.tensor_tensor(out=nlam[:], in0=ld1[:, 1:2], in1=ld1[:, 0:1], op=ALU.subtract), reads=["ld1"], writes=["nlam"])
                s.op("dve", lambda e: e.tensor_scalar(out=nlam[:], in0=nlam[:], scalar1=-lam_init, scalar2=None, op0=ALU.add), reads=["nlam"], writes=["nlam"])
                s.op("dve", lambda e: e.memset(V[:, :, :, 128:130], 1.0), writes=["V"])

                def rope(ps, pskey, out, outkey, i):
                    v4 = lambda ap: ap.rearrange("p (g two i) -> p g two i", g=8, two=2)
                    cb = cosT[:, i, :].unsqueeze(1).unsqueeze(1).to_broadcast([128, 8, 2, 32])
                    sb = sinT[:, i, :].unsqueeze(1).to_broadcast([128, 8, 32])
                    nsb = nsinT[:, i, :].unsqueeze(1).to_broadcast([128, 8, 32])
                    s.op("dve", lambda e: e.tensor_tensor(out=v4(ropeA[:]), in0=v4(ps[:, :]), in1=cb, op=ALU.mult),
                         reads=[pskey, "cosT"], writes=["ropeA"])
                    s.op("dve", lambda e: e.tensor_tensor(out=v4(ropeB[:])[:, :, 0, :], in0=v4(ps[:, :])[:, :, 1, :], in1=nsb, op=ALU.mult),
                         reads=[pskey, "nsinT"], writes=["ropeB"])
                    s.op("dve", lambda e: e.tensor_tensor(out=v4(ropeB[:])[:, :, 1, :], in0=v4(ps[:, :])[:, :, 0, :], in1=sb, op=ALU.mult),
                         reads=[pskey, "sinT"], writes=["ropeB"])
                    s.op("pool", lambda e: e.tensor_tensor(out=out[:], in0=ropeA[:], in1=ropeB[:], op=ALU.add),
                         reads=["ropeA", "ropeB"], writes=[outkey])

                load_x(ct["xt"][0], "xt0", hsrc, 0)
                if xr is not None:
                    load_x(xr[0], "xr0", rsrc, 0)
                for i in range(NT):
                    xt = ct["xt"][i % 2]; xk = "xt%d" % (i % 2)
                    if i + 1 < NT:
                        load_x(ct["xt"][(i + 1) % 2], "xt%d" % ((i + 1) % 2), hsrc, i + 1)
                        if xr is not None:
                            load_x(xr[(i + 1) % 2], "xr%d" % ((i + 1) % 2), rsrc, i + 1)
                    norm_mod(ct, xt, xk, A1, B1)
                    hb, hT = ct["hb"], ct["hT"]
                    transpose8(hb, "hb", tp, hT)
                    for k in range(8):
                        mm(psq[:, :], hT[:, k, :], Wq[:, k, :], k == 0, k == 7, ["Wq_", "hT"], ["psq"])
                    for k in range(8):
                        mm(psk[:, :], hT[:, k, :], Wk[:, k, :], k == 0, k == 7, ["Wk_", "hT"], ["psk"])
                    for k in range(8):
                        mm(psv[:, :], hT[:, k, :], Wv[:, k, :], k == 0, k == 7, ["Wv_", "hT"], ["psv"])
                    rope(psq, "psq", qr, "qr", i)
                    rope(psk, "psk", kr, "kr", i)
                    s.op("act", lambda e, i=i: e.copy(out=V[:, i, :, 0:128], in_=psv[:, :].rearrange("p (h d) -> p h d", h=4)),
                         reads=["psv"], writes=["V"])
                    for hh in range(4):
                        tr(tp[:, hh, :], qr[:, hh * 128:(hh + 1) * 128], ident_b[:], ["qr", "ident_b"], ["tp"])
                    for hh in range(4):
                        tr(tp[:, 4 + hh, :], kr[:, hh * 128:(hh + 1) * 128], ident_b[:], ["kr", "ident_b"], ["tp"])
                    s.op("act", lambda e: e.copy(out=QT[:], in_=tp[:, 0:4, :]), reads=["tp"], writes=["QT"])
                    s.op("act", lambda e, i=i: e.copy(out=KT[:, :, i * 128:(i + 1) * 128], in_=tp[:, 4:8, :]), reads=["tp"], writes=["KT"])
                    cnt = 0
                    for hh in range(4):
                        po = pso_[hh % 2]; pok = "psoh%d" % (hh % 2)
                        for m in range(2):
                            rows = slice(m * 64, (m + 1) * 64)
                            for g0 in range(0, i + 1, 4):
                                kbs = list(range(g0, min(g0 + 4, i + 1)))
                                bank = pss[cnt % 2]; bk = "pss%d" % (cnt % 2)
                                pt = PT[cnt % 2]; ptk = "PT%d" % (cnt % 2)
                                cnt += 1
                                for j, kb in enumerate(kbs):
                                    mm(bank[:, j, :], KT[rows, hh, kb * 128:(kb + 1) * 128], QT[rows, hh, :], True, True, ["KT", "QT"], [bk])
                                n = len(kbs)
                                s.op("act", lambda e, pt=pt, bank=bank, n=n: e.activation(out=pt[:, 0:n, :], in_=bank[:, 0:n, :], func=AF.Exp, scale=0.125),
                                     reads=[bk], writes=[ptk])
                                if i in kbs:
                                    jd = i - g0
                                    s.op("pool", lambda e, pt=pt, jd=jd: e.tensor_tensor(out=pt[:, jd, :], in0=pt[:, jd, :], in1=triu_b[:], op=ALU.mult),
                                         reads=[ptk, "triu_b"], writes=[ptk])
                                for j, kb in enumerate(kbs):
                                    mm(po[:, m, 0:129], pt[:, j, :], V[:, kb, hh, 0:129], kb == 0, kb == i, [ptk, "V"], [pok])
                        s.op("dve", lambda e, po=po: e.reciprocal(out=rz[:], in_=po[:, :, 128]), reads=[pok], writes=["rz"])
                        s.op("dve", lambda e: e.tensor_tensor(out=rz[:, 1:2], in0=rz[:, 1:2], in1=nlam[:], op=ALU.mult), reads=["rz", "nlam"], writes=["rz"])
                        s.op("dve", lambda e, po=po: e.tensor_scalar(out=of[:], in0=po[:, 0, 0:128], scalar1=rz[:, 0:1], scalar2=None, op0=ALU.mult),
                             reads=[pok, "rz"], writes=["of"])
                        s.op("dve", lambda e, po=po: e.scalar_tensor_tensor(out=of[:], in0=po[:, 1, 0:128], scalar=rz[:, 1:2], in1=of[:], op0=ALU.mult, op1=ALU.add),
                             reads=[pok, "rz", "of"], writes=["of"])
                        s.op("act", lambda e: e.activation(out=oj[:], in_=of[:], func=AF.Square, accum_out=oss[:]), reads=["of"], writes=["oj", "oss"])
                        s.op("act", lambda e: e.activation(out=oss[:], in_=oss[:], func=AF.Sqrt, scale=1.0 / 128, bias=EPS), reads=["oss"], writes=["oss"])
                        s.op("dve", lambda e: e.reciprocal(out=oss[:], in_=oss[:]), reads=["oss"], writes=["oss"])
                        s.op("dve", lambda e, hh=hh: e.scalar_tensor_tensor(out=Oall[:, hh, :], in0=of[:], scalar=oss[:, 0:1], in1=gsub[:], op0=ALU.mult, op1=ALU.mult),
                             reads=["of", "oss", "gsub"], writes=["Oall"])
                    for hh in range(4):
                        tr(tp[:, hh, :], Oall[:, hh, :], ident_b[:], ["Oall", "ident_b"], ["tp"])
                    s.op("act", lambda e: e.copy(out=oT[:], in_=tp[:, 0:4, :]), reads=["tp"], writes=["oT"])
                    pso = [psq, psk]
                    for half in range(2):
                        for hh in range(4):
                            mm(pso[half][:, :], oT[:, hh, :], Wout[:, hh, half * 512:(half + 1) * 512], hh == 0, hh == 3,
                               ["oT", "Wout"], [["psq", "psk"][half]])
                    if xr is not None:
                        residual_out(pso, ["psq", "psk"], G1, xr[i % 2], "xr%d" % (i % 2), ct["xo"], dst, i)
                    else:
                        residual_out(pso, ["psq", "psk"], G1, xt, xk, ct["xo"], dst, i)
                s.flush()

        def peer_pass(l, src, dst, fin):
            with contextlib.ExitStack() as ph:
                junk = T("junk", [128, D], BF16, stack=ph)
                ss = T("ss", [128, 1], stack=ph); rs = T("rs", [128, 1], stack=ph)
                hf = T("hf", [128, D], stack=ph); hb = T("hb", [128, D], BF16, stack=ph)
                hT = T("hT", [128, 8, 128], BF16, stack=ph)
                xo = T("xo", [128, D], stack=ph)
                xts = [T("xt%d" % i, [128, D], stack=ph) for i in range(2)]
                Wq = T("Wqp", [128, 8, 2048], BF16, stack=ph)
                skT = T("skT", [128, 16, 128], stack=ph)
                qT = T("qT", [128, 16, 128], stack=ph)
                sc = T("sc", [128, 16, 128], stack=ph)
                sc2 = T("sc2", [128, 16, 128], stack=ph)
                skraw = sc2
                sv = T("sv", [128, 16, 16], stack=ph)
                si = T("si", [128, 16, 16], U32, stack=ph)
                sif = T("sif", [128, 16, 16], stack=ph)
                cand = sc[:].rearrange("p (h a) n -> p h (a n)", h=8)
                cand2 = sc2[:].rearrange("p (h a) n -> p h (a n)", h=8)
                fv = T("fv", [128, 8, 16], stack=ph)
                fpos = T("fpos", [128, 8, 16], U32, stack=ph)
                fa = T("fa", [128, 8, 16], U32, stack=ph); fb = T("fb", [128, 8, 16], U32, stack=ph)
                faf = T("faf", [128, 8, 16], stack=ph); fbf = T("fbf", [128, 8, 16], stack=ph)
                oh = sc2[:].rearrange("p (h a) (b c) -> p h (a b) c", h=8, c=16)
                Ii = T("Ii", [128, 8, 16], stack=ph); Jj = T("Jj", [128, 8, 16], stack=ph)
                eif = T("eif", [128, 128], stack=ph)
                eidxs = [T("eidx%d" % i, [128, 128], U32, stack=ph) for i in range(2)]
                ge = T("ge", [128, 8, 16], stack=ph); gz = T("gz", [128, 8], stack=ph)
                gates = [T("gate%d" % i, [128, 128], stack=ph) for i in range(2)]
                act = T("act", [128, 128], stack=ph)
                wgt = T("wgt", [128, 128], stack=ph)
                NB = 8
                UV = [T("UV%d" % i, [128, 2 * D], BF16, stack=ph) for i in range(NB)]
                dj = T("dj", [128, D], BF16, stack=ph)
                diag = [T("diag%d" % i, [128, 128], BF16, stack=ph) for i in range(2)]
                fin_t = None
                if fin:
                    fnbc = T("fnbc", [128, D], stack=ph)
                    yo = T("yo", [128, D], stack=ph)
                    fj = T("fj", [128, D], BF16, stack=ph); fss = T("fss", [128, 1], stack=ph); frs = T("frs", [128, 1], stack=ph)
                    ld(fnbc[:], IN["final_norm"][0, :].partition_broadcast(128), ["fnbc"])
                    fin_t = (fj, fss, frs, fnbc, yo)
                tp = PS("tp", [128, 8, 128], BF16, stack=ph)
                psx = [PS("psx%d" % i, [128, 4, 128], stack=ph) for i in range(2)]
                pso = [PS("psop%d" % i, [128, 512], stack=ph) for i in range(2)]
                hps = [PS("hps%d" % i, [128, D], BF16, stack=ph) for i in range(2)]

                wload_bf16(Wq, "Wqp", IN["peer_w_q"][l])
                ld(skraw[:], IN["peer_sub_keys"][l].rearrange("g n k -> n g k"), ["sc2"])
                for r in range(4):
                    for c_ in range(4):
                        tr(psx[r % 2][:, c_, :], skraw[:, r * 4 + c_, :], ident_f[:], ["sc2", "ident_f"], ["psx%d" % (r % 2)])
                    S_().op("act", lambda e, r=r: e.copy(out=skT[:, r * 4:(r + 1) * 4, :], in_=psx[r % 2][:]), reads=["psx%d" % (r % 2)], writes=["skT"])

                def stageA(i):
                    xt = xts[i % 2]; xk = "xt%d" % (i % 2)
                    eidx = eidxs[i % 2]; ek = "eidx%d" % (i % 2)
                    gate = gates[i % 2]; gk = "gate%d" % (i % 2)
                    hp_ = hps[i % 2]; hpk = "hps%d" % (i % 2)
                    o = S_()
                    load_x(xt, xk, src, i)
                    o.op("act", lambda e: e.activation(out=junk[:], in_=xt[:], func=AF.Square, accum_out=ss[:]), reads=[xk], writes=["junk", "ss"])
                    o.op("act", lambda e: e.activation(out=rs[:], in_=ss[:], func=AF.Sqrt, scale=1.0 / D, bias=EPS), reads=["ss"], writes=["rs"])
                    o.op("dve", lambda e: e.reciprocal(out=rs[:], in_=rs[:]), reads=["rs"], writes=["rs"])
                    o.op("dve", lambda e: e.scalar_tensor_tensor(out=hf[:], in0=xt[:], scalar=rs[:, 0:1], in1=A2[:], op0=ALU.mult, op1=ALU.mult),
                         reads=[xk, "rs", A2.name], writes=["hf"])
                    o.op("pool", lambda e: e.tensor_tensor(out=hb[:], in0=hf[:], in1=B2[:], op=ALU.add), reads=["hf", B2.name], writes=["hb"])
                    for k in range(8):
                        tr(tp[:, k, :], hb[:, k * 128:(k + 1) * 128], ident_b[:], ["hb", "ident_b"], ["tp"])
                    o.op("act", lambda e: e.copy(out=hT[:], in_=tp[:]), reads=["tp"], writes=["hT"])
                    for k in range(8):
                        tr(hp_[:, k * 128:(k + 1) * 128], hT[:, k, :], ident_b[:], ["hT", "ident_b"], [hpk])
                    for r in range(4):
                        pq = psx[r % 2]; pqk = "psx%d" % (r % 2)
                        for c_ in range(4):
                            hp = r * 4 + c_
                            for k in range(8):
                                mm(pq[:, c_, :], Wq[:, k, hp * 128:(hp + 1) * 128], hT[:, k, :], k == 0, k == 7, ["Wqp", "hT"], [pqk])
                        o.op("act", lambda e, r=r, pq=pq: e.copy(out=qT[:, r * 4:(r + 1) * 4, :], in_=pq[:]), reads=[pqk], writes=["qT"])
                    for r in range(4):
                        pz = psx[r % 2]; pzk = "psx%d" % (r % 2)
                        for c_ in range(4):
                            hp = r * 4 + c_
                            mm(pz[:, c_, :], qT[:, hp, :], skT[:, hp, :], True, True, ["qT", "skT"], [pzk])
                        o.op("act", lambda e, r=r, pz=pz: e.copy(out=sc[:, r * 4:(r + 1) * 4, :], in_=pz[:]), reads=[pzk], writes=["sc"])
                    for g in range(16):
                        o.op("dve", lambda e, g=g: e.max(out=sv[:, g, 0:8], in_=sc[:, g, :]), reads=["sc"], writes=["sv"])
                        o.op("dve", lambda e, g=g: e.max_index(out=si[:, g, 0:8], in_max=sv[:, g, 0:8], in_values=sc[:, g, :]), reads=["sc", "sv"], writes=["si"])
                        o.op("dve", lambda e, g=g: e.match_replace(out=sc2[:, g, :], in_to_replace=sv[:, g, 0:8], in_values=sc[:, g, :], imm_value=-1e30),
                             reads=["sc", "sv"], writes=["sc2"])
                        o.op("dve", lambda e, g=g: e.max(out=sv[:, g, 8:16], in_=sc2[:, g, :]), reads=["sc2"], writes=["sv"])
                        o.op("dve", lambda e, g=g: e.max_index(out=si[:, g, 8:16], in_max=sv[:, g, 8:16], in_values=sc2[:, g, :]), reads=["sc2", "sv"], writes=["si"])
                    o.op("dve", lambda e: e.tensor_copy(out=sif[:], in_=si[:]), reads=["si"], writes=["sif"])
                    sv4 = sv[:].rearrange("p (h two) k -> p h two k", two=2)
                    sif4 = sif[:].rearrange("p (h two) k -> p h two k", two=2)
                    o.op("dve", lambda e: e.tensor_tensor(out=cand.rearrange("p h (a b) -> p h a b", a=16),
                                                          in0=sv4[:, :, 0, :].unsqueeze(3).to_broadcast([128, 8, 16, 16]),
                                                          in1=sv4[:, :, 1, :].unsqueeze(2).to_broadcast([128, 8, 16, 16]), op=ALU.add),
                         reads=["sv"], writes=["sc"])
                    for h in range(8):
                        o.op("dve", lambda e, h=h: e.max(out=fv[:, h, 0:8], in_=cand[:, h, :]), reads=["sc"], writes=["fv"])
                        o.op("dve", lambda e, h=h: e.max_index(out=fpos[:, h, 0:8], in_max=fv[:, h, 0:8], in_values=cand[:, h, :]), reads=["sc", "fv"], writes=["fpos"])
                        o.op("dve", lambda e, h=h: e.match_replace(out=cand2[:, h, :], in_to_replace=fv[:, h, 0:8], in_values=cand[:, h, :], imm_value=-1e30),
                             reads=["sc", "fv"], writes=["sc2"])
                        o.op("dve", lambda e, h=h: e.max(out=fv[:, h, 8:16], in_=cand2[:, h, :]), reads=["sc2"], writes=["fv"])
                        o.op("dve", lambda e, h=h: e.max_index(out=fpos[:, h, 8:16], in_max=fv[:, h, 8:16], in_values=cand2[:, h, :]), reads=["sc2", "fv"], writes=["fpos"])
                    o.op("dve", lambda e: e.tensor_tensor(out=ge[:], in0=fv[:], in1=fv[:, :, 0:1].to_broadcast([128, 8, 16]), op=ALU.subtract),
                         reads=["fv"], writes=["ge"])
                    o.op("act", lambda e: e.activation(out=ge[:], in_=ge[:], func=AF.Exp), reads=["ge"], writes=["ge"])
                    o.op("dve", lambda e: e.tensor_reduce(out=gz[:], in_=ge[:], axis=AX.X, op=ALU.add), reads=["ge"], writes=["gz"])
                    o.op("dve", lambda e: e.reciprocal(out=gz[:], in_=gz[:]), reads=["gz"], writes=["gz"])
                    o.op("dve", lambda e: e.tensor_tensor(out=gate[:].rearrange("p (h k) -> p h k", h=8), in0=ge[:],
                                                          in1=gz[:].unsqueeze(2).to_broadcast([128, 8, 16]), op=ALU.mult),
                         reads=["ge", "gz"], writes=[gk])
                    o.op("dve", lambda e: e.tensor_single_scalar(out=fa[:], in_=fpos[:], scalar=4, op=ALU.logical_shift_right), reads=["fpos"], writes=["fa"])
                    o.op("dve", lambda e: e.tensor_single_scalar(out=fb[:], in_=fpos[:], scalar=15, op=ALU.bitwise_and), reads=["fpos"], writes=["fb"])
                    o.op("dve", lambda e: e.tensor_copy(out=faf[:], in_=fa[:]), reads=["fa"], writes=["faf"])
                    o.op("dve", lambda e: e.tensor_copy(out=fbf[:], in_=fb[:]), reads=["fb"], writes=["fbf"])
                    io4 = iota16[:].unsqueeze(1).unsqueeze(1).to_broadcast([128, 8, 16, 16])
                    for (srcf, sk_, half, dstt, dk) in ((faf, "faf", 0, Ii, "Ii"), (fbf, "fbf", 1, Jj, "Jj")):
                        o.op("dve", lambda e, srcf=srcf: e.tensor_tensor(out=oh, in0=srcf[:].unsqueeze(3).to_broadcast([128, 8, 16, 16]), in1=io4, op=ALU.is_equal),
                             reads=[sk_, "iota16"], writes=["sc2"])
                        o.op("dve", lambda e, half=half: e.tensor_tensor(out=oh, in0=oh, in1=sif4[:, :, half, :].unsqueeze(2).to_broadcast([128, 8, 16, 16]), op=ALU.mult),
                             reads=["sc2", "sif"], writes=["sc2"])
                        o.op("dve", lambda e, dstt=dstt: e.tensor_reduce(out=dstt[:], in_=oh, axis=AX.X, op=ALU.add), reads=["sc2"], writes=[dk])
                    o.op("dve", lambda e: e.scalar_tensor_tensor(out=eif[:].rearrange("p (h k) -> p h k", h=8), in0=Ii[:], scalar=128.0, in1=Jj[:], op0=ALU.mult, op1=ALU.add),
                         reads=["Ii", "Jj"], writes=["eif"])
                    if l > 0:
                        o.op("dve", lambda e: e.tensor_scalar(out=eif[:], in0=eif[:], scalar1=float(l * NEXP), scalar2=None, op0=ALU.add),
                             reads=["eif"], writes=["eif"])
                    o.op("dve", lambda e: e.tensor_copy(out=eidx[:], in_=eif[:]), reads=["eif"], writes=[ek])

                nuv = [0]

                def stageB(i, filler):
                    xt = xts[i % 2]; xk = "xt%d" % (i % 2)
                    eidx = eidxs[i % 2]; ek = "eidx%d" % (i % 2)
                    gate = gates[i % 2]; gk = "gate%d" % (i % 2)
                    hp_ = hps[i % 2]; hpk = "hps%d" % (i % 2)
                    o = S_()
                    LOOK = 4
                    order = list(range(128))
                    bufs = {}

                    def issue(sl):
                        b = nuv[0] % NB; nuv[0] += 1
                        bufs[sl] = b
                        ub = UV[b]
                        o.dma("pool", lambda e, ub=ub, sl=sl: e.indirect_dma_start(
                            out=ub[:], out_offset=None, in_=uvb[:, :],
                            in_offset=bass.IndirectOffsetOnAxis(ap=eidx[:, sl:sl + 1], axis=0)), reads=[ek, ("uvb", l)], writes=["UV%d" % b])

                    for sl in range(min(LOOK, 128)):
                        issue(sl)
                    GS = 4
                    for g0 in range(0, 128, GS):
                        for sl in range(g0, g0 + GS):
                            if sl + LOOK < 128:
                                issue(sl + LOOK)
                            b = bufs[sl]; ub = UV[b]; ubk = "UV%d" % b
                            o.op("dve", lambda e, ub=ub, sl=sl: e.scalar_tensor_tensor(out=dj[:], in0=ub[:, 0:D], scalar=1.0, in1=hp_[:], op0=ALU.mult, op1=ALU.mult,
                                                                                       accum_out=act[:, sl:sl + 1]),
                                 reads=[ubk, hpk], writes=["dj", "act"])
                            filler()
                        hs = slice(g0, g0 + GS)
                        o.op("act", lambda e, hs=hs: e.activation(out=wgt[:, hs], in_=act[:, hs], func=AF.Gelu), reads=["act"], writes=["wgt"])
                        o.op("dve", lambda e, hs=hs: e.tensor_tensor(out=wgt[:, hs], in0=wgt[:, hs], in1=gate[:, hs], op=ALU.mult), reads=["wgt", gk], writes=["wgt"])
                        for sl in range(g0, g0 + GS):
                            b = bufs[sl]; ub = UV[b]; ubk = "UV%d" % b
                            dg = diag[sl % 2]; dgk = "diag%d" % (sl % 2)
                            o.op("act", lambda e, dg=dg, sl=sl: e.activation(out=dg[:], in_=ident_f[:], func=AF.Copy, scale=wgt[:, sl:sl + 1]),
                                 reads=["ident_f", "wgt"], writes=[dgk])
                            for half in range(2):
                                mm(pso[half][:, :], dg[:], ub[:, D + half * 512: D + (half + 1) * 512], sl == 0, sl == 127, [dgk, ubk], ["psop%d" % half])
                    residual_out(pso, ["psop0", "psop1"], G2, xt, xk, xo, dst, i, fin=fin_t)

                stageA(0)
                for i in range(NT):
                    nxt = Deferred()
                    if i + 1 < NT:
                        sink[0] = nxt
                        stageA(i + 1)
                        sink[0] = s
                    per = (len(nxt.q) + 127) // 128
                    stageB(i, lambda: nxt.run(s, per))
                    nxt.run(s, None)
                s.flush()

        cur = IN["x"]
        free = [0, 1, 2]

        def take(exclude):
            for b in free:
                if xs[b] is not exclude and all(xs[b] is not e_ for e_ in exclude if e_ is not None):
                    return xs[b]
            raise RuntimeError("no buffer")

        nl = len(layers)
        for li, l in enumerate(layers):
            compute_mod(l)
            if do_mixer:
                if l % 2 == 0:
                    d1 = [b for b in xs if b is not cur][0]
                    even_mixer(l, cur, d1)
                    cur = d1
                else:
                    others = [b for b in xs if b is not cur]
                    odd_mixer(l, cur, cur, others[0], 0)
                    odd_mixer(l, cur, others[0], others[1], 1)
                    cur = others[1]
            if do_peer:
                last = (li == nl - 1)
                d2 = y_out if last else [b for b in xs if b is not cur][0]
                peer_pass(l, cur, d2, fin=(last and final_norm))
                cur = d2
        if cur is not y_out:
            with contextlib.ExitStack() as ph:
                tt = T("cp", [128, D], stack=ph)
                for i in range(NT):
                    ld(tt[:], cur[i * 128:(i + 1) * 128, :], ["cp"], r=[(cur.tensor.name, i)])
                    ld(y_out[i * 128:(i + 1) * 128, :], tt[:], [("y", i)], r=["cp"])
                s.flush()
        s.wait_all("sp")
        s.flush()
        build.n_instr = s.n_instr
    return nc


def make_in_maps(inputs, n_cores, S=4096):
    consts = host_consts()
    shared = {}
    for k in IN_SHAPES:
        if k in consts:
            shared[k] = consts[k]
        else:
            shared[k] = np.ascontiguousarray(np.asarray(inputs[k], dtype=np.float32)).reshape(IN_SHAPES[k][0])
    maps = []
    x = np.asarray(inputs["x"], dtype=np.float32)
    c = np.asarray(inputs["c"], dtype=np.float32)
    pos = np.asarray(inputs["positions"]).astype(np.int32)
    for b in range(n_cores):
        m = dict(shared)
        m["x"] = np.ascontiguousarray(x[b, :S])
        m["c"] = np.ascontiguousarray(c[b:b + 1])
        m["positions"] = np.ascontiguousarray(pos[b:b + 1, :S])
        maps.append(m)
    return maps


def kernel(**inputs):
    n = 8
    nc = build(4096)
    maps = make_in_maps(inputs, n)
    res = run_bass_kernel_spmd(nc, maps, core_ids=list(range(n)))
    return np.stack([np.asarray(r["y"], dtype=np.float32) for r in res.results], axis=0)
```

```python
import contextlib
import math
import numpy as np
import concourse.bass as bass
import concourse.mybir as mybir
from concourse.bass_utils import run_bass_kernel_spmd

F32 = mybir.dt.float32
BF16 = mybir.dt.bfloat16
U32 = mybir.dt.uint32
I32 = mybir.dt.int32
AF = mybir.ActivationFunctionType
ALU = mybir.AluOpType
AX = mybir.AxisListType

D = 1024
EPS = 1e-6
NEXP = 16384
ENGS = ("pe", "act", "dve", "pool", "sp")
SAME_ENGINE_SYNC = True
NO_SAME_SYNC = set()


class Sched:
    def __init__(self, nc, st, n_dma_sems=12):
        self.nc = nc
        self.ops = {e: [] for e in ENGS}
        self.cnt = {e: 0 for e in ENGS}
        self.known = {e: {} for e in ENGS}
        self.lastw = {}
        self.reads = {}
        self.n_dma_sems = n_dma_sems
        self.dma_cnt = {}
        self.dma_rr = {e: 0 for e in ENGS}
        self.semh = {}
        for e in ENGS:
            self.semh[e] = st.enter_context(nc.semaphore("s_" + e))
        for q in ("sp", "pool", "act"):
            for i in range(n_dma_sems):
                self.semh[(q, i)] = st.enter_context(nc.semaphore("d_%s%d" % (q, i)))
        self.n_instr = 0

    def _deps(self, eng, reads, writes, skip_same):
        waits = {}

        def need(ev):
            if ev is None:
                return
            k, v = ev
            if k == eng and (skip_same or not SAME_ENGINE_SYNC or eng in NO_SAME_SYNC):
                return
            if self.known[eng].get(k, 0) >= v:
                return
            if waits.get(k, 0) < v:
                waits[k] = v

        for b in reads:
            need(self.lastw.get(b))
        for b in writes:
            need(self.lastw.get(b))
            for ev in self.reads.get(b, ()):
                need(ev)
        for k, v in waits.items():
            self.known[eng][k] = v
        return waits

    def _record(self, ev, reads, writes):
        for b in reads:
            lst = self.reads.setdefault(b, [])
            lst[:] = [x for x in lst if x[0] != ev[0]]
            lst.append(ev)
        for b in writes:
            self.lastw[b] = ev
            self.reads[b] = []

    def op(self, eng, fn, reads=(), writes=(), skip_same=False):
        waits = self._deps(eng, reads, writes, skip_same)
        self.cnt[eng] += 1
        ev = (eng, self.cnt[eng])
        self.ops[eng].append(("op", fn, waits, ev))
        self._record(ev, reads, writes)
        return ev

    def dma(self, queue, fn, reads=(), writes=()):
        i = self.dma_rr[queue]
        self.dma_rr[queue] = (i + 1) % self.n_dma_sems
        key = (queue, i)
        prev = self.dma_cnt.get(key, 0)
        waits = self._deps(queue, reads, writes, False)
        if prev > 0 and self.known[queue].get(key, 0) < prev * 16:
            waits[key] = prev * 16
            self.known[queue][key] = prev * 16
        self.dma_cnt[key] = prev + 1
        ev = (key, (prev + 1) * 16)
        self.ops[queue].append(("dma", fn, waits, ev))
        self._record(ev, reads, writes)
        return ev

    def wait_all(self, eng):
        waits = {}
        for e in ENGS:
            if e != eng and self.cnt[e] > 0:
                waits[e] = self.cnt[e]
        for key, c in self.dma_cnt.items():
            waits[key] = c * 16
        self.ops[eng].append(("wait", None, waits, None))

    def flush(self):
        nc = self.nc
        if not any(self.ops[e] for e in ENGS):
            return
        with nc.Block() as block:
            regs = {"pe": block.tensor, "act": block.scalar, "dve": block.vector,
                    "pool": block.gpsimd, "sp": block.sync}

            def make(e, lst):
                def body(engobj):
                    for kind, fn, waits, ev in lst:
                        for k, v in waits.items():
                            engobj.wait_ge(self.semh[k], v)
                        if kind == "wait":
                            continue
                        ins = fn(engobj)
                        if kind == "dma":
                            ins.then_inc(self.semh[ev[0]], 16)
                        else:
                            ins.then_inc(self.semh[e], 1)
                return body

            for e in ENGS:
                lst = self.ops[e]
                self.n_instr += len(lst)
                if lst:
                    regs[e](make(e, lst))
        self.ops = {e: [] for e in ENGS}


class Deferred:
    def __init__(self):
        self.q = []

    def op(self, *a, **k):
        self.q.append(("op", a, k))

    def dma(self, *a, **k):
        self.q.append(("dma", a, k))

    def run(self, s, n):
        m = len(self.q) if n is None else min(n, len(self.q))
        for kind, a, k in self.q[:m]:
            getattr(s, kind)(*a, **k)
        del self.q[:m]


def host_consts():
    ident = np.eye(128, dtype=np.float32)
    p = np.arange(128)[:, None]
    j = np.arange(128)[None, :]
    triu = (j >= p).astype(np.float32)
    poolm = np.zeros((12, 128, 128), np.float32)
    for g, w in enumerate((2, 4, 8, 16)):
        band = ((j - p >= 0) & (j - p <= w - 1)).astype(np.float32)
        poolm[g] = band / w - ident
        cnt = np.minimum(np.arange(128) + 1, w).astype(np.float32)[None, :]
        poolm[4 + g] = band / cnt - ident
        poolm[8 + g] = ((j - p + 128) <= (w - 1)).astype(np.float32) / w
    iota16 = np.tile(np.arange(16, dtype=np.float32)[None, :], (128, 1))
    inv_freq = 1.0 / (10000.0 ** (np.arange(0, 64, 2, dtype=np.float32) / 64.0))
    invf = np.tile((inv_freq / (2.0 * np.pi)).astype(np.float32)[None, :], (128, 1))
    return {"k_ident": ident, "k_triu": triu, "k_poolm": poolm.transpose(1, 0, 2).copy(),
            "k_iota16": iota16, "k_invf": invf}


IN_SHAPES = {
    "ada_w": ([4, 1024, 6144], F32), "ada_b": ([4, 6144], F32),
    "norm_mix": ([4, 1024], F32), "norm_ffn": ([4, 1024], F32),
    "ev_w_in": ([2, 1024, 1536], F32), "ev_g_v": ([2, 512], F32),
    "ev_w_s": ([2, 4, 128, 128], F32), "ev_b_s": ([2, 512], F32),
    "ev_w_pool": ([2, 4, 128, 128], F32), "ev_pool_scale": ([2, 512], F32),
    "ev_w_out": ([2, 1024, 1024], F32), "od_w_in": ([2, 1024, 3072], F32),
    "od_lam_q1": ([2, 64], F32), "od_lam_k1": ([2, 64], F32),
    "od_lam_q2": ([2, 64], F32), "od_lam_k2": ([2, 64], F32),
    "od_g_sub": ([2, 128], F32), "od_w_out": ([2, 1024, 1024], F32),
    "peer_w_q": ([4, 1024, 2048], F32), "peer_sub_keys": ([4, 16, 128, 128], F32),
    "peer_u": ([4, NEXP, 1024], F32), "peer_v": ([4, NEXP, 1024], F32),
    "final_norm": ([1, 1024], F32),
    "k_ident": ([128, 128], F32), "k_triu": ([128, 128], F32), "k_poolm": ([128, 12, 128], F32),
    "k_iota16": ([128, 16], F32), "k_invf": ([128, 32], F32),
}


def build(S=4096, layers=(0, 1, 2, 3), dbg=False, do_mixer=True, do_peer=True, final_norm=True):
    NT = S // 128
    nc = bass.Bass("TRN2", target_bir_lowering=False)
    IN = {}
    IN["x"] = nc.dram_tensor("x", [S, D], F32, kind="ExternalInput").ap()
    IN["c"] = nc.dram_tensor("c", [1, D], F32, kind="ExternalInput").ap()
    IN["positions"] = nc.dram_tensor("positions", [1, S], I32, kind="ExternalInput").ap()
    for k, (shp, dt) in IN_SHAPES.items():
        IN[k] = nc.dram_tensor(k, shp, dt, kind="ExternalInput").ap()
    y_out = nc.dram_tensor("y", [S, D], F32, kind="ExternalOutput").ap()
    xs = [nc.dram_tensor("xs%d" % i, [S, D], F32,
                         kind=("ExternalOutput" if dbg else "Internal")).ap() for i in range(3)]

    uvb = nc.dram_tensor("uvb", [4 * NEXP, 2 * D], BF16, kind="Internal").ap()

    with contextlib.ExitStack() as st:
        s = Sched(nc, st)
        for h in s.semh.values():
            nc.gpsimd.sem_clear(h)
        nc.all_engine_barrier()

        uid = [0]

        def T(name, shape, dt=F32, stack=st):
            uid[0] += 1
            return stack.enter_context(nc.sbuf_tensor("%s_%d" % (name, uid[0]), shape, dt))

        def PS(name, shape, dt=F32, stack=st):
            uid[0] += 1
            return stack.enter_context(nc.psum_tensor("%s_%d" % (name, uid[0]), shape, dt))

        sink = [s]

        def S_():
            return sink[0]

        def mm(out, lhsT, rhs, start, stop, r, w):
            sink[0].op("pe", lambda e: e.matmul(out, lhsT=lhsT, rhs=rhs, start=start, stop=stop),
                 reads=r, writes=w, skip_same=True)

        def tr(out, in_, ident, r, w):
            sink[0].op("pe", lambda e: e.transpose(out=out, in_=in_, identity=ident),
                 reads=r, writes=w, skip_same=True)

        def ld(out, in_, w, r=(), q="sp", slow=False):
            if slow:
                sink[0].dma(q, lambda e: e.dma_start(out=out, in_=in_, allow_slow_non_contiguous=True), reads=r, writes=w)
            else:
                sink[0].dma(q, lambda e: e.dma_start(out=out, in_=in_), reads=r, writes=w)

        ident_f = T("ident_f", [128, 128]); ident_b = T("ident_b", [128, 128], BF16)
        triu_b = T("triu_b", [128, 128], BF16); triu_f = T("triu_f", [128, 128])
        iota16 = T("iota16", [128, 16])
        ones_row = T("ones_row", [1, 128])
        modbc = [T("modbc%d" % i, [128, D]) for i in range(6)]
        A1, B1, G1, A2, B2, G2 = modbc
        cosT = T("cosT", [128, NT, 32]); sinT = T("sinT", [128, NT, 32]); nsinT = T("nsinT", [128, NT, 32])
        c_act = T("c_act", [128, 8])

        ld(ident_f[:], IN["k_ident"][:, :], ["ident_f"])
        ld(triu_f[:], IN["k_triu"][:, :], ["triu_f"])
        ld(iota16[:], IN["k_iota16"][:, :], ["iota16"])
        s.op("dve", lambda e: e.tensor_copy(out=ident_b[:], in_=ident_f[:]), reads=["ident_f"], writes=["ident_b"])
        s.op("dve", lambda e: e.tensor_copy(out=triu_b[:], in_=triu_f[:]), reads=["triu_f"], writes=["triu_b"])
        s.op("dve", lambda e: e.memset(ones_row[:], 1.0), writes=["ones_row"])

        if do_peer:
            for l_ in layers:
                for c_ in range(16):
                    r0 = l_ * NEXP + c_ * 1024
                    s.dma("pool", lambda e, l_=l_, c_=c_, r0=r0: e.dma_start(out=uvb[r0:r0 + 1024, 0:D], in_=IN["peer_u"][l_, c_ * 1024:(c_ + 1) * 1024, :]),
                          writes=[("uvb", l_)])
                    s.dma("pool", lambda e, l_=l_, c_=c_, r0=r0: e.dma_start(out=uvb[r0:r0 + 1024, D:2 * D], in_=IN["peer_v"][l_, c_ * 1024:(c_ + 1) * 1024, :]),
                          writes=[("uvb", l_)])

        with contextlib.ExitStack() as ph:
            c_row = T("c_row", [1, D], stack=ph)
            one11 = T("one11", [1, 1], stack=ph)
            pc = PS("pc", [128, 8], stack=ph)
            ld(c_row[:], IN["c"][:, :], ["c_row"])
            s.op("dve", lambda e: e.memset(one11[:], 1.0), writes=["one11"])
            for k in range(8):
                mm(pc[:, k:k + 1], c_row[0:1, k * 128:(k + 1) * 128], one11[0:1, 0:1], True, True,
                   ["c_row", "one11"], ["pc"])
            s.op("act", lambda e: e.activation(out=c_act[:], in_=pc[:], func=AF.Silu), reads=["pc"], writes=["c_act"])
            pos_i = T("pos_i", [128, NT], I32, stack=ph)
            pos_f = T("pos_f", [128, NT], stack=ph)
            invf = T("invf", [128, 32], stack=ph)
            yy = T("yy", [128, NT, 32], stack=ph); y2 = T("y2", [128, NT, 32], stack=ph)
            ki = T("ki", [128, NT, 32], I32, stack=ph); kf = T("kf", [128, NT, 32], stack=ph)
            ld(pos_i[:], IN["positions"][0, :].rearrange("(n p) -> p n", p=128), ["pos_i"], slow=True)
            ld(invf[:], IN["k_invf"][:, :], ["invf"])
            s.op("dve", lambda e: e.tensor_copy(out=pos_f[:], in_=pos_i[:]), reads=["pos_i"], writes=["pos_f"])
            s.op("dve", lambda e: e.tensor_tensor(out=yy[:], in0=pos_f[:].unsqueeze(2).to_broadcast([128, NT, 32]),
                                                  in1=invf[:].unsqueeze(1).to_broadcast([128, NT, 32]), op=ALU.mult),
                 reads=["pos_f", "invf"], writes=["yy"])
            s.op("dve", lambda e: e.tensor_copy(out=ki[:], in_=yy[:]), reads=["yy"], writes=["ki"])
            s.op("dve", lambda e: e.tensor_copy(out=kf[:], in_=ki[:]), reads=["ki"], writes=["kf"])
            s.op("dve", lambda e: e.tensor_tensor(out=y2[:], in0=yy[:], in1=kf[:], op=ALU.subtract), reads=["yy", "kf"], writes=["y2"])
            s.op("act", lambda e: e.activation(out=sinT[:], in_=y2[:], func=AF.Sin, scale=2.0 * math.pi), reads=["y2"], writes=["sinT"])
            s.op("dve", lambda e: e.tensor_scalar(out=nsinT[:], in0=sinT[:], scalar1=-1.0, scalar2=None, op0=ALU.mult), reads=["sinT"], writes=["nsinT"])
            s.op("dve", lambda e: e.tensor_scalar(out=yy[:], in0=yy[:], scalar1=0.25, scalar2=None, op0=ALU.add), reads=["yy"], writes=["yy"])
            s.op("dve", lambda e: e.tensor_copy(out=ki[:], in_=yy[:]), reads=["yy"], writes=["ki"])
            s.op("dve", lambda e: e.tensor_copy(out=kf[:], in_=ki[:]), reads=["ki"], writes=["kf"])
            s.op("dve", lambda e: e.tensor_tensor(out=y2[:], in0=yy[:], in1=kf[:], op=ALU.subtract), reads=["yy", "kf"], writes=["y2"])
            s.op("act", lambda e: e.activation(out=cosT[:], in_=y2[:], func=AF.Sin, scale=2.0 * math.pi), reads=["y2"], writes=["cosT"])
            s.flush()

        def compute_mod(l):
            with contextlib.ExitStack() as ph:
                wt = [T("adaw%d" % i, [128, 8, 512], stack=ph) for i in range(2)]
                brow = T("adab", [1, 6 * D], stack=ph)
                mrow = T("mrow", [1, 6 * D], stack=ph)
                nbc = [T("nbc%d" % i, [128, D], stack=ph) for i in range(2)]
                pm = [PS("pm%d" % i, [1, 512], stack=ph) for i in range(2)]
                pb = [PS("pb%d" % i, [128, 512], stack=ph) for i in range(2)]
                ld(brow[:], IN["ada_b"][l:l + 1, :], ["adab"])
                ld(nbc[0][:], IN["norm_mix"][l, :].partition_broadcast(128), ["nbc0"])
                ld(nbc[1][:], IN["norm_ffn"][l, :].partition_broadcast(128), ["nbc1"])
                for cb in range(12):
                    w = wt[cb % 2]; wk = "adaw%d" % (cb % 2); pk = "pm%d" % (cb % 2)
                    ld(w[:], IN["ada_w"][l, :, cb * 512:(cb + 1) * 512].rearrange("(k p) f -> p k f", p=128), [wk])
                    for k in range(8):
                        mm(pm[cb % 2][:, :], c_act[:, k:k + 1], w[:, k, :], k == 0, k == 7, ["c_act", wk], [pk])
                    s.op("dve", lambda e, cb=cb: e.tensor_tensor(out=mrow[0:1, cb * 512:(cb + 1) * 512], in0=pm[cb % 2][:, :],
                                                                 in1=brow[0:1, cb * 512:(cb + 1) * 512], op=ALU.add),
                         reads=[pk, "adab"], writes=["mrow"])
                dst = {0: (B1, None), 1: (A1, 0), 2: (G1, None), 3: (B2, None), 4: (A2, 1), 5: (G2, None)}
                n = 0
                for i in range(6):
                    tgt, nb = dst[i]
                    for half in range(2):
                        pbk = "pb%d" % (n % 2); pbt = pb[n % 2]; n += 1
                        mm(pbt[:, :], ones_row[0:1, :], mrow[0:1, i * D + half * 512: i * D + (half + 1) * 512], True, True,
                           ["ones_row", "mrow"], [pbk])
                        o = tgt[:, half * 512:(half + 1) * 512]
                        if nb is None:
                            s.op("act", lambda e, o=o, pbt=pbt: e.copy(out=o, in_=pbt[:, :]), reads=[pbk], writes=[tgt.name])
                        else:
                            nbt = nbc[nb][:, half * 512:(half + 1) * 512]
                            s.op("dve", lambda e, o=o, pbt=pbt, nbt=nbt: e.scalar_tensor_tensor(
                                out=o, in0=pbt[:, :], scalar=1.0, in1=nbt, op0=ALU.add, op1=ALU.mult),
                                reads=[pbk, "nbc%d" % nb], writes=[tgt.name])
                s.flush()

        def load_x(xt, key, src, i):
            ld(xt[:], src[i * 128:(i + 1) * 128, :], [key], r=[(src.tensor.name, i)])

        def norm_mod(ph_t, xt, xkey, A, B, want_f32=False):
            junk, ss, rs, hf, hb = ph_t["junk"], ph_t["ss"], ph_t["rs"], ph_t["hf"], ph_t["hb"]
            s.op("act", lambda e: e.activation(out=junk[:], in_=xt[:], func=AF.Square, accum_out=ss[:]),
                 reads=[xkey], writes=["junk", "ss"])
            s.op("act", lambda e: e.activation(out=rs[:], in_=ss[:], func=AF.Sqrt, scale=1.0 / D, bias=EPS),
                 reads=["ss"], writes=["rs"])
            s.op("dve", lambda e: e.reciprocal(out=rs[:], in_=rs[:]), reads=["rs"], writes=["rs"])
            s.op("dve", lambda e: e.scalar_tensor_tensor(out=hf[:], in0=xt[:], scalar=rs[:, 0:1], in1=A[:],
                                                         op0=ALU.mult, op1=ALU.mult),
                 reads=[xkey, "rs", A.name], writes=["hf"])
            if want_f32:
                s.op("pool", lambda e: e.tensor_tensor(out=hf[:], in0=hf[:], in1=B[:], op=ALU.add),
                     reads=["hf", B.name], writes=["hf"])
                s.op("act", lambda e: e.copy(out=hb[:], in_=hf[:]), reads=["hf"], writes=["hb"])
            else:
                s.op("pool", lambda e: e.tensor_tensor(out=hb[:], in0=hf[:], in1=B[:], op=ALU.add),
                     reads=["hf", B.name], writes=["hb"])

        def transpose8(hb, hbkey, tp, hT):
            for k in range(8):
                tr(tp[:, k, :], hb[:, k * 128:(k + 1) * 128], ident_b[:], [hbkey, "ident_b"], ["tp"])
            s.op("act", lambda e: e.copy(out=hT[:], in_=tp[:]), reads=["tp"], writes=["hT"])

        def wload_bf16(dst, key, src, nk=8, parts=4):
            step = nk // parts
            for a in range(parts):
                ld(dst[:, a * step:(a + 1) * step, :],
                   src[a * step * 128:(a + 1) * step * 128, :].rearrange("(k p) f -> p k f", p=128), [key], q="pool")

        def residual_out(pso, psk, Gt, xres, xreskey, xo, dst, i, fin=None):
            for half in range(2):
                sl = slice(half * 512, (half + 1) * 512)
                s.op("dve", lambda e, half=half, sl=sl: e.tensor_tensor(out=xo[:, sl], in0=pso[half][:, :], in1=Gt[:, sl], op=ALU.mult),
                     reads=[psk[half], Gt.name], writes=["xo"])
            s.op("pool", lambda e: e.tensor_tensor(out=xo[:], in0=xo[:], in1=xres[:], op=ALU.add),
                 reads=["xo", xreskey], writes=["xo"])
            if fin is None:
                ld(dst[i * 128:(i + 1) * 128, :], xo[:], [(dst.tensor.name, i)], r=["xo"])
            else:
                junk, ss, rs, fnbc, yo = fin
                s.op("act", lambda e: e.activation(out=junk[:], in_=xo[:], func=AF.Square, accum_out=ss[:]),
                     reads=["xo"], writes=["junk", "ss"])
                s.op("act", lambda e: e.activation(out=rs[:], in_=ss[:], func=AF.Sqrt, scale=1.0 / D, bias=EPS),
                     reads=["ss"], writes=["rs"])
                s.op("dve", lambda e: e.reciprocal(out=rs[:], in_=rs[:]), reads=["rs"], writes=["rs"])
                s.op("dve", lambda e: e.scalar_tensor_tensor(out=yo[:], in0=xo[:], scalar=rs[:, 0:1], in1=fnbc[:],
                                                             op0=ALU.mult, op1=ALU.mult),
                     reads=["xo", "rs", "fnbc"], writes=["yo"])
                ld(dst[i * 128:(i + 1) * 128, :], yo[:], [(dst.tensor.name, i)], r=["yo"])

        def common_tiles(ph):
            d = {}
            d["junk"] = T("junk", [128, D], BF16, stack=ph)
            d["ss"] = T("ss", [128, 1], stack=ph)
            d["rs"] = T("rs", [128, 1], stack=ph)
            d["hf"] = T("hf", [128, D], stack=ph)
            d["hb"] = T("hb", [128, D], BF16, stack=ph)
            d["hT"] = T("hT", [128, 8, 128], BF16, stack=ph)
            d["xo"] = T("xo", [128, D], stack=ph)
            d["xt"] = [T("xt%d" % i, [128, D], stack=ph) for i in range(2)]
            return d

        def even_mixer(l, src, dst):
            e_ = l // 2
            with contextlib.ExitStack() as ph:
                ct = common_tiles(ph)
                Win = T("Win", [128, 8, 1536], BF16, stack=ph)
                Wout = T("Wout", [128, 8, D], BF16, stack=ph)
                wsraw = T("wsraw", [128, 4, 128], stack=ph)
                wsT = T("wsT", [128, 4, 128], BF16, stack=ph)
                bsrow = T("bsrow", [1, 512], stack=ph)
                wpool = T("wpool", [128, 4, 128], BF16, stack=ph)
                gvbc = T("gvbc", [128, 512], stack=ph)
                lscol = T("lscol", [128, 4], stack=ph)
                poolm = T("poolm", [128, 12, 128], stack=ph)
                uTs = T("uTs", [128, 4, 128], stack=ph)
                vs = T("vs", [128, 512], stack=ph)
                vn = T("vn", [128, 512], BF16, stack=ph)
                bst = T("bst", [128, 6], stack=ph); bag = T("bag", [128, 2], stack=ph); sd = T("sd", [128, 1], stack=ph)
                pcur = [T("pcur%d" % i, [128, 512], stack=ph) for i in range(2)]
                pooledT = T("pooledT", [128, 4, 128], BF16, stack=ph)
                yabT = T("yabT", [128, 8, 128], BF16, stack=ph)
                tp = PS("tp", [128, 8, 128], BF16, stack=ph)
                psU = PS("psU", [128, 4, 128], stack=ph)
                psV = PS("psV", [128, 512], stack=ph)
                psP = PS("psP", [128, 512], stack=ph)
                psS = PS("psS", [128, 4, 128], stack=ph)
                psQ = PS("psQ", [128, 4, 128], stack=ph)
                pso = [PS("pso%d" % i, [128, 512], stack=ph) for i in range(2)]

                wload_bf16(Win, "Win", IN["ev_w_in"][e_])
                wload_bf16(Wout, "Wout", IN["ev_w_out"][e_])
                ld(wsraw[:], IN["ev_w_s"][e_].rearrange("g t s -> t g s"), ["wsraw"])
                ld(bsrow[:], IN["ev_b_s"][e_:e_ + 1, :], ["bsrow"])
                ld(wpool[:], IN["ev_w_pool"][e_].rearrange("g c d -> c g d"), ["wpool"], q="pool")
                ld(gvbc[:], IN["ev_g_v"][e_, :].partition_broadcast(128), ["gvbc"])
                ld(lscol[:], IN["ev_pool_scale"][e_, :].rearrange("(g d) -> d g", d=128), ["lscol"], slow=True)
                ld(poolm[:], IN["k_poolm"][:, :, :], ["poolm"])
                for g in range(4):
                    tr(psS[:, g, :], wsraw[:, g, :], ident_f[:], ["wsraw", "ident_f"], ["psS"])
                s.op("dve", lambda e: e.tensor_tensor(out=wsT[:], in0=psS[:], in1=triu_f[:].unsqueeze(1).to_broadcast([128, 4, 128]), op=ALU.mult),
                     reads=["psS", "triu_f"], writes=["wsT"])

                load_x(ct["xt"][0], "xt0", src, 0)
                for i in range(NT):
                    xt = ct["xt"][i % 2]; xk = "xt%d" % (i % 2)
                    if i + 1 < NT:
                        load_x(ct["xt"][(i + 1) % 2], "xt%d" % ((i + 1) % 2), src, i + 1)
                    norm_mod(ct, xt, xk, A1, B1)
                    hb, hT = ct["hb"], ct["hT"]
                    transpose8(hb, "hb", tp, hT)
                    for fc in range(4):
                        for k in range(8):
                            mm(psU[:, fc, :], Win[:, k, fc * 128:(fc + 1) * 128], hT[:, k, :], k == 0, k == 7, ["Win", "hT"], ["psU"])
                    s.op("act", lambda e: e.activation(out=uTs[:], in_=psU[:], func=AF.Gelu), reads=["psU"], writes=["uTs"])
                    for k in range(8):
                        mm(psV[:, :], hT[:, k, :], Win[:, k, 512:1024], k == 0, k == 7, ["Win", "hT"], ["psV"])
                    s.op("act", lambda e: e.activation(out=vs[:], in_=psV[:], func=AF.Gelu), reads=["psV"], writes=["vs"])
                    s.op("dve", lambda e: e.bn_stats(out=bst[:], in_=vs[:]), reads=["vs"], writes=["bst"])
                    s.op("dve", lambda e: e.bn_aggr(out=bag[:], in_=bst[:]), reads=["bst"], writes=["bag"])
                    s.op("act", lambda e: e.activation(out=sd[:], in_=bag[:, 1:2], func=AF.Sqrt, scale=1.0, bias=EPS), reads=["bag"], writes=["sd"])
                    s.op("dve", lambda e: e.reciprocal(out=sd[:], in_=sd[:]), reads=["sd"], writes=["sd"])
                    s.op("dve", lambda e: e.tensor_scalar(out=vs[:], in0=vs[:], scalar1=bag[:, 0:1], scalar2=sd[:, 0:1],
                                                          op0=ALU.subtract, op1=ALU.mult), reads=["vs", "bag", "sd"], writes=["vs"])
                    s.op("pool", lambda e: e.tensor_tensor(out=vn[:], in0=vs[:], in1=gvbc[:], op=ALU.mult), reads=["vs", "gvbc"], writes=["vn"])
                    pc_ = pcur[i % 2]; pck = "pcur%d" % (i % 2); pp_ = pcur[(i + 1) % 2]; ppk = "pcur%d" % ((i + 1) % 2)
                    for k in range(8):
                        mm(psP[:, :], hT[:, k, :], Win[:, k, 1024:1536], k == 0, k == 7, ["Win", "hT"], ["psP"])
                    s.op("act", lambda e, pc_=pc_: e.copy(out=pc_[:], in_=psP[:]), reads=["psP"], writes=[pck])
                    for g in range(4):
                        mm(psS[:, g, :], vn[:, g * 128:(g + 1) * 128], wsT[:, g, :], True, False, ["vn", "wsT"], ["psS"])
                        mm(psS[:, g, :], ones_row[0:1, :], bsrow[0:1, g * 128:(g + 1) * 128], False, True, ["ones_row", "bsrow"], ["psS"])
                    s.op("dve", lambda e: e.tensor_tensor(out=yabT[:, 0:4, :], in0=psS[:], in1=uTs[:], op=ALU.mult),
                         reads=["psS", "uTs"], writes=["yabT"])
                    for g in range(4):
                        gi = (4 + g) if i == 0 else g
                        mm(psQ[:, g, :], pc_[:, g * 128:(g + 1) * 128], poolm[:, gi, :], True, i == 0, [pck, "poolm"], ["psQ"])
                        if i > 0:
                            mm(psQ[:, g, :], pp_[:, g * 128:(g + 1) * 128], poolm[:, 8 + g, :], False, True, [ppk, "poolm"], ["psQ"])
                    s.op("act", lambda e: e.copy(out=pooledT[:], in_=psQ[:]), reads=["psQ"], writes=["pooledT"])
                    for g in range(4):
                        mm(psU[:, g, :], wpool[:, g, :], pooledT[:, g, :], True, True, ["wpool", "pooledT"], ["psU"])
                    for g in range(4):
                        s.op("dve", lambda e, g=g: e.tensor_scalar(out=yabT[:, 4 + g, :], in0=psU[:, g, :], scalar1=lscol[:, g:g + 1],
                                                                   scalar2=None, op0=ALU.mult), reads=["psU", "lscol"], writes=["yabT"])
                    for half in range(2):
                        for fc in range(8):
                            mm(pso[half][:, :], yabT[:, fc, :], Wout[:, fc, half * 512:(half + 1) * 512], fc == 0, fc == 7,
                               ["yabT", "Wout"], ["pso%d" % half])
                    residual_out(pso, ["pso0", "pso1"], G1, xt, xk, ct["xo"], dst, i)
                s.flush()

        def odd_mixer(l, hsrc, rsrc, dst, hg):
            o_ = l // 2
            lam_init = 0.8 - 0.6 * math.exp(-0.3 * l)
            with contextlib.ExitStack() as ph:
                ct = common_tiles(ph)
                Wq = T("Wq_", [128, 8, 512], BF16, stack=ph)
                Wk = T("Wk_", [128, 8, 512], BF16, stack=ph)
                Wv = T("Wv_", [128, 8, 512], BF16, stack=ph)
                Wout = T("Wout", [128, 4, D], BF16, stack=ph)
                KT = T("KT", [128, 4, S], BF16, stack=ph)
                V = T("V", [128, NT, 4, 130], BF16, stack=ph)
                QT = T("QT", [128, 4, 128], BF16, stack=ph)
                lamt = T("lamt", [128, 4, 64], stack=ph)
                lj = T("lj", [128, 64], stack=ph); ld1 = T("ld1", [128, 2], stack=ph)
                nlam = T("nlam", [128, 1], stack=ph)
                gsub = T("gsub", [128, 128], stack=ph)
                xr = [T("xr%d" % i, [128, D], stack=ph) for i in range(2)] if rsrc is not hsrc else None
                ropeA = T("ropeA", [128, 512], stack=ph); ropeB = T("ropeB", [128, 512], stack=ph)
                qr = T("qr", [128, 512], BF16, stack=ph); kr = T("kr", [128, 512], BF16, stack=ph)
                PT = [T("PT%d" % i, [128, 4, 128], BF16, stack=ph) for i in range(3)]
                mhalf = T("mhalf", [128, 1], stack=ph)
                rz = T("rz", [128, 2], stack=ph); of = T("of", [128, 128], stack=ph); oj = T("oj", [128, 128], stack=ph)
                oss = T("oss", [128, 1], stack=ph)
                Oall = T("Oall", [128, 4, 128], BF16, stack=ph)
                oT = T("oT", [128, 4, 128], BF16, stack=ph)
                tp = PS("tp", [128, 8, 128], BF16, stack=ph)
                psq = PS("psq", [128, 512], stack=ph); psk = PS("psk", [128, 512], stack=ph); psv = PS("psv", [128, 4, 128], stack=ph)
                pss = [PS("pss%d" % i, [128, 4, 128], stack=ph) for i in range(2)]
                pss3 = [pss[0], pss[1], psv]; pss3k = ["pss0", "pss1", "psv"]
                s.op("dve", lambda e: e.memset(mhalf[:], -0.5), writes=["mhalf"])
                pso_ = [PS("psoh%d" % i, [128, 2, 130], stack=ph) for i in range(2)]

                c0 = hg * 512
                wload_bf16(Wq, "Wq_", IN["od_w_in"][o_][:, c0:c0 + 512])
                wload_bf16(Wk, "Wk_", IN["od_w_in"][o_][:, 1024 + c0:1024 + c0 + 512])
                wload_bf16(Wv, "Wv_", IN["od_w_in"][o_][:, 2048 + c0:2048 + c0 + 512])
                wload_bf16(Wout, "Wout", IN["od_w_out"][o_][hg * 512:(hg + 1) * 512, :], nk=4, parts=2)
                for j, nm in enumerate(("od_lam_q1", "od_lam_k1", "od_lam_q2", "od_lam_k2")):
                    ld(lamt[:, j, :], IN[nm][o_, :].partition_broadcast(128), ["lamt"])
                ld(gsub[:], IN["od_g_sub"][o_, :].partition_broadcast(128), ["gsub"])
                s.op("dve", lambda e: e.tensor_scalar(out=gsub[:], in0=gsub[:], scalar1=1.0 - lam_init, scalar2=None, op0=ALU.mult),
                     reads=["gsub"], writes=["gsub"])
                for j in range(2):
                    s.op("dve", lambda e, j=j: e.scalar_tensor_tensor(out=lj[:], in0=lamt[:, 2 * j, :], scalar=1.0, in1=lamt[:, 2 * j + 1, :],
                                                                      op0=ALU.mult, op1=ALU.mult, accum_out=ld1[:, j:j + 1]),
                         reads=["lamt"], writes=["lj", "ld1"])
                s.op("act", lambda e: e.activation(out=ld1[:], in_=ld1[:], func=AF.Exp), reads=["ld1"], writes=["ld1"])
                s.op("dve", lambda e: e.tensor_tensor(out=nlam[:], in0=ld1[:, 1:2], in1=ld1[:, 0:1], op=ALU.subtract), reads=["ld1"], writes=["nlam"])
                s.op("dve", lambda e: e.tensor_scalar(out=nlam[:], in0=nlam[:], scalar1=-lam_init, scalar2=None, op0=ALU.add), reads=["nlam"], writes=["nlam"])
                s.op("dve", lambda e: e.memset(V[:, :, :, 128:130], 1.0), writes=["V"])

                def rope(ps, pskey, out, outkey, i):
                    v4 = lambda ap: ap.rearrange("p (g two i) -> p g two i", g=8, two=2)
                    cb = cosT[:, i, :].unsqueeze(1).unsqueeze(1).to_broadcast([128, 8, 2, 32])
                    sb = sinT[:, i, :].unsqueeze(1).to_broadcast([128, 8, 32])
                    nsb = nsinT[:, i, :].unsqueeze(1).to_broadcast([128, 8, 32])
                    s.op("dve", lambda e: e.tensor_tensor(out=v4(ropeA[:]), in0=v4(ps[:, :]), in1=cb, op=ALU.mult),
                         reads=[pskey, "cosT"], writes=["ropeA"])
                    s.op("dve", lambda e: e.tensor_tensor(out=v4(ropeB[:])[:, :, 0, :], in0=v4(ps[:, :])[:, :, 1, :], in1=nsb, op=ALU.mult),
                         reads=[pskey, "nsinT"], writes=["ropeB"])
                    s.op("dve", lambda e: e.tensor_tensor(out=v4(ropeB[:])[:, :, 1, :], in0=v4(ps[:, :])[:, :, 0, :], in1=sb, op=ALU.mult),
                         reads=[pskey, "sinT"], writes=["ropeB"])
                    s.op("pool", lambda e: e.tensor_tensor(out=out[:], in0=ropeA[:], in1=ropeB[:], op=ALU.add),
                         reads=["ropeA", "ropeB"], writes=[outkey])

                load_x(ct["xt"][0], "xt0", hsrc, 0)
                if xr is not None:
                    load_x(xr[0], "xr0", rsrc, 0)
                for i in range(NT):
                    xt = ct["xt"][i % 2]; xk = "xt%d" % (i % 2)
                    if i + 1 < NT:
                        load_x(ct["xt"][(i + 1) % 2], "xt%d" % ((i + 1) % 2), hsrc, i + 1)
                        if xr is not None:
                            load_x(xr[(i + 1) % 2], "xr%d" % ((i + 1) % 2), rsrc, i + 1)
                    norm_mod(ct, xt, xk, A1, B1)
                    hb, hT = ct["hb"], ct["hT"]
                    transpose8(hb, "hb", tp, hT)
                    for k in range(8):
                        mm(psq[:, :], hT[:, k, :], Wq[:, k, :], k == 0, k == 7, ["Wq_", "hT"], ["psq"])
                    for k in range(8):
                        mm(psk[:, :], hT[:, k, :], Wk[:, k, :], k == 0, k == 7, ["Wk_", "hT"], ["psk"])
                    for k in range(8):
                        mm(psv[:].rearrange("p a b -> p (a b)"), hT[:, k, :], Wv[:, k, :], k == 0, k == 7, ["Wv_", "hT"], ["psv"])
                    rope(psq, "psq", qr, "qr", i)
                    rope(psk, "psk", kr, "kr", i)
                    s.op("act", lambda e, i=i: e.copy(out=V[:, i, :, 0:128], in_=psv[:]),
                         reads=["psv"], writes=["V"])
                    for hh in range(4):
                        tr(tp[:, hh, :], qr[:, hh * 128:(hh + 1) * 128], ident_b[:], ["qr", "ident_b"], ["tp"])
                    for hh in range(4):
                        tr(tp[:, 4 + hh, :], kr[:, hh * 128:(hh + 1) * 128], ident_b[:], ["kr", "ident_b"], ["tp"])
                    s.op("act", lambda e: e.copy(out=QT[:], in_=tp[:, 0:4, :]), reads=["tp"], writes=["QT"])
                    s.op("act", lambda e, i=i: e.copy(out=KT[:, :, i * 128:(i + 1) * 128], in_=tp[:, 4:8, :]), reads=["tp"], writes=["KT"])
                    items = []
                    for hh in range(4):
                        for m in range(2):
                            for g0 in range(0, i + 1, 4):
                                items.append((hh, m, g0, list(range(g0, min(g0 + 4, i + 1)))))

                    def emitS(it, sl_):
                        hh, m, g0, kbs = it
                        rows = slice(m * 64, (m + 1) * 64)
                        bank = pss3[sl_]; bk = pss3k[sl_]
                        pt = PT[sl_]; ptk = "PT%d" % sl_
                        for j, kb in enumerate(kbs):
                            mm(bank[:, j, :], KT[rows, hh, kb * 128:(kb + 1) * 128], QT[rows, hh, :], True, True, ["KT", "QT"], [bk])
                        n = len(kbs)
                        s.op("act", lambda e, pt=pt, bank=bank, n=n: e.activation(out=pt[:, 0:n, :], in_=bank[:, 0:n, :], func=AF.Exp, scale=0.125),
                             reads=[bk], writes=[ptk])
                        if i in kbs:
                            jd = i - g0
                            s.op("pool", lambda e, pt=pt, jd=jd: e.tensor_tensor(out=pt[:, jd, :], in0=pt[:, jd, :], in1=triu_b[:], op=ALU.mult),
                                 reads=[ptk, "triu_b"], writes=[ptk])

                    def emitAV(it, sl_):
                        hh, m, g0, kbs = it
                        po = pso_[hh % 2]; pok = "psoh%d" % (hh % 2)
                        pt = PT[sl_]; ptk = "PT%d" % sl_
                        for j, kb in enumerate(kbs):
                            mm(po[:, m, 0:129], pt[:, j, :], V[:, kb, hh, 0:129], kb == 0, kb == i, [ptk, "V"], [pok])
                        if m == 1 and kbs[-1] == i:
                            s.op("dve", lambda e, po=po: e.reciprocal(out=rz[:], in_=po[:, :, 128]), reads=[pok], writes=["rz"])
                            s.op("dve", lambda e: e.tensor_tensor(out=rz[:, 1:2], in0=rz[:, 1:2], in1=nlam[:], op=ALU.mult), reads=["rz", "nlam"], writes=["rz"])
                            s.op("dve", lambda e, po=po: e.tensor_scalar(out=of[:], in0=po[:, 0, 0:128], scalar1=rz[:, 0:1], scalar2=None, op0=ALU.mult),
                                 reads=[pok, "rz"], writes=["of"])
                            s.op("dve", lambda e, po=po: e.scalar_tensor_tensor(out=of[:], in0=po[:, 1, 0:128], scalar=rz[:, 1:2], in1=of[:], op0=ALU.mult, op1=ALU.add),
                                 reads=[pok, "rz", "of"], writes=["of"])
                            s.op("dve", lambda e: e.scalar_tensor_tensor(out=oj[:], in0=of[:], scalar=1.0, in1=of[:], op0=ALU.mult, op1=ALU.mult, accum_out=oss[:]),
                                 reads=["of"], writes=["oj", "oss"])
                            s.op("dve", lambda e: e.tensor_scalar(out=oss[:], in0=oss[:], scalar1=1.0 / 128, scalar2=EPS, op0=ALU.mult, op1=ALU.add),
                                 reads=["oss"], writes=["oss"])
                            s.op("pool", lambda e: e.tensor_tensor(out=oss[:], in0=oss[:], in1=mhalf[:], op=ALU.pow), reads=["oss", "mhalf"], writes=["oss"])
                            s.op("dve", lambda e, hh=hh: e.scalar_tensor_tensor(out=Oall[:, hh, :], in0=of[:], scalar=oss[:, 0:1], in1=gsub[:], op0=ALU.mult, op1=ALU.mult),
                                 reads=["of", "oss", "gsub"], writes=["Oall"])

                    prev = None
                    for n_, it in enumerate(items):
                        emitS(it, n_ % 3)
                        if prev is not None:
                            emitAV(*prev)
                        prev = (it, n_ % 3)
                    emitAV(*prev)
                    for hh in range(4):
                        tr(tp[:, hh, :], Oall[:, hh, :], ident_b[:], ["Oall", "ident_b"], ["tp"])
                    s.op("act", lambda e: e.copy(out=oT[:], in_=tp[:, 0:4, :]), reads=["tp"], writes=["oT"])
                    pso = [psq, psk]
                    for half in range(2):
                        for hh in range(4):
                            mm(pso[half][:, :], oT[:, hh, :], Wout[:, hh, half * 512:(half + 1) * 512], hh == 0, hh == 3,
                               ["oT", "Wout"], [["psq", "psk"][half]])
                    if xr is not None:
                        residual_out(pso, ["psq", "psk"], G1, xr[i % 2], "xr%d" % (i % 2), ct["xo"], dst, i)
                    else:
                        residual_out(pso, ["psq", "psk"], G1, xt, xk, ct["xo"], dst, i)
                s.flush()

        def peer_pass(l, src, dst, fin):
            with contextlib.ExitStack() as ph:
                junk = T("junk", [128, D], BF16, stack=ph)
                ss = T("ss", [128, 1], stack=ph); rs = T("rs", [128, 1], stack=ph)
                hf = T("hf", [128, D], stack=ph); hb = T("hb", [128, D], BF16, stack=ph)
                hT = T("hT", [128, 8, 128], BF16, stack=ph)
                xo = T("xo", [128, D], stack=ph)
                xts = [T("xt%d" % i, [128, D], stack=ph) for i in range(2)]
                Wq = T("Wqp", [128, 8, 2048], BF16, stack=ph)
                skT = T("skT", [128, 16, 128], stack=ph)
                qT = T("qT", [128, 16, 128], stack=ph)
                sc = T("sc", [128, 16, 128], stack=ph)
                sc2 = T("sc2", [128, 16, 128], stack=ph)
                skraw = sc2
                sv = T("sv", [128, 16, 16], stack=ph)
                si = T("si", [128, 16, 16], U32, stack=ph)
                sif = T("sif", [128, 16, 16], stack=ph)
                cand = sc[:].rearrange("p (h a) n -> p h (a n)", h=8)
                cand2 = sc2[:].rearrange("p (h a) n -> p h (a n)", h=8)
                fv = T("fv", [128, 8, 16], stack=ph)
                fpos = T("fpos", [128, 8, 16], U32, stack=ph)
                fa = T("fa", [128, 8, 16], U32, stack=ph); fb = T("fb", [128, 8, 16], U32, stack=ph)
                faf = T("faf", [128, 8, 16], stack=ph); fbf = T("fbf", [128, 8, 16], stack=ph)
                oh = sc2[:].rearrange("p (h a) (b c) -> p h (a b) c", h=8, c=16)
                Ii = T("Ii", [128, 8, 16], stack=ph); Jj = T("Jj", [128, 8, 16], stack=ph)
                eif = T("eif", [128, 128], stack=ph)
                eidxs = [T("eidx%d" % i, [128, 128], U32, stack=ph) for i in range(2)]
                ge = T("ge", [128, 8, 16], stack=ph); gz = T("gz", [128, 8], stack=ph)
                gates = [T("gate%d" % i, [128, 128], stack=ph) for i in range(2)]
                act = T("act", [128, 128], stack=ph)
                wgt = T("wgt", [128, 128], stack=ph)
                NB = 12
                UV = [T("UV%d" % i, [128, 2 * D], BF16, stack=ph) for i in range(NB)]
                dj = T("dj", [128, D], BF16, stack=ph)
                diag = [T("diag%d" % i, [128, 128], BF16, stack=ph) for i in range(2)]
                fin_t = None
                if fin:
                    fnbc = T("fnbc", [128, D], stack=ph)
                    yo = T("yo", [128, D], stack=ph)
                    fj = T("fj", [128, D], BF16, stack=ph); fss = T("fss", [128, 1], stack=ph); frs = T("frs", [128, 1], stack=ph)
                    ld(fnbc[:], IN["final_norm"][0, :].partition_broadcast(128), ["fnbc"])
                    fin_t = (fj, fss, frs, fnbc, yo)
                tp = PS("tp", [128, 8, 128], BF16, stack=ph)
                psx = [PS("psx%d" % i, [128, 4, 128], stack=ph) for i in range(2)]
                pso = [PS("psop%d" % i, [128, 512], stack=ph) for i in range(2)]
                hps = [PS("hps%d" % i, [128, D], BF16, stack=ph) for i in range(2)]

                wload_bf16(Wq, "Wqp", IN["peer_w_q"][l])
                ld(skraw[:], IN["peer_sub_keys"][l].rearrange("g n k -> n g k"), ["sc2"])
                for r in range(4):
                    for c_ in range(4):
                        tr(psx[r % 2][:, c_, :], skraw[:, r * 4 + c_, :], ident_f[:], ["sc2", "ident_f"], ["psx%d" % (r % 2)])
                    S_().op("act", lambda e, r=r: e.copy(out=skT[:, r * 4:(r + 1) * 4, :], in_=psx[r % 2][:]), reads=["psx%d" % (r % 2)], writes=["skT"])

                def stageA(i):
                    xt = xts[i % 2]; xk = "xt%d" % (i % 2)
                    eidx = eidxs[i % 2]; ek = "eidx%d" % (i % 2)
                    gate = gates[i % 2]; gk = "gate%d" % (i % 2)
                    hp_ = hps[i % 2]; hpk = "hps%d" % (i % 2)
                    o = S_()
                    load_x(xt, xk, src, i)
                    o.op("act", lambda e: e.activation(out=junk[:], in_=xt[:], func=AF.Square, accum_out=ss[:]), reads=[xk], writes=["junk", "ss"])
                    o.op("act", lambda e: e.activation(out=rs[:], in_=ss[:], func=AF.Sqrt, scale=1.0 / D, bias=EPS), reads=["ss"], writes=["rs"])
                    o.op("dve", lambda e: e.reciprocal(out=rs[:], in_=rs[:]), reads=["rs"], writes=["rs"])
                    o.op("dve", lambda e: e.scalar_tensor_tensor(out=hf[:], in0=xt[:], scalar=rs[:, 0:1], in1=A2[:], op0=ALU.mult, op1=ALU.mult),
                         reads=[xk, "rs", A2.name], writes=["hf"])
                    o.op("pool", lambda e: e.tensor_tensor(out=hb[:], in0=hf[:], in1=B2[:], op=ALU.add), reads=["hf", B2.name], writes=["hb"])
                    for k in range(8):
                        tr(tp[:, k, :], hb[:, k * 128:(k + 1) * 128], ident_b[:], ["hb", "ident_b"], ["tp"])
                    o.op("act", lambda e: e.copy(out=hT[:], in_=tp[:]), reads=["tp"], writes=["hT"])
                    for k in range(8):
                        tr(hp_[:, k * 128:(k + 1) * 128], hT[:, k, :], ident_b[:], ["hT", "ident_b"], [hpk])
                    for r in range(4):
                        pq = psx[r % 2]; pqk = "psx%d" % (r % 2)
                        for c_ in range(4):
                            hp = r * 4 + c_
                            for k in range(8):
                                mm(pq[:, c_, :], Wq[:, k, hp * 128:(hp + 1) * 128], hT[:, k, :], k == 0, k == 7, ["Wqp", "hT"], [pqk])
                        o.op("act", lambda e, r=r, pq=pq: e.copy(out=qT[:, r * 4:(r + 1) * 4, :], in_=pq[:]), reads=[pqk], writes=["qT"])
                    for r in range(4):
                        pz = psx[r % 2]; pzk = "psx%d" % (r % 2)
                        for c_ in range(4):
                            hp = r * 4 + c_
                            mm(pz[:, c_, :], qT[:, hp, :], skT[:, hp, :], True, True, ["qT", "skT"], [pzk])
                        o.op("act", lambda e, r=r, pz=pz: e.copy(out=sc[:, r * 4:(r + 1) * 4, :], in_=pz[:]), reads=[pzk], writes=["sc"])
                    for g in range(16):
                        o.op("dve", lambda e, g=g: e.max(out=sv[:, g, 0:8], in_=sc[:, g, :]), reads=["sc"], writes=["sv"])
                        o.op("dve", lambda e, g=g: e.max_index(out=si[:, g, 0:8], in_max=sv[:, g, 0:8], in_values=sc[:, g, :]), reads=["sc", "sv"], writes=["si"])
                        o.op("dve", lambda e, g=g: e.match_replace(out=sc2[:, g, :], in_to_replace=sv[:, g, 0:8], in_values=sc[:, g, :], imm_value=-1e30),
                             reads=["sc", "sv"], writes=["sc2"])
                        o.op("dve", lambda e, g=g: e.max(out=sv[:, g, 8:16], in_=sc2[:, g, :]), reads=["sc2"], writes=["sv"])
                        o.op("dve", lambda e, g=g: e.max_index(out=si[:, g, 8:16], in_max=sv[:, g, 8:16], in_values=sc2[:, g, :]), reads=["sc2", "sv"], writes=["si"])
                    o.op("dve", lambda e: e.tensor_copy(out=sif[:], in_=si[:]), reads=["si"], writes=["sif"])
                    sv4 = sv[:].rearrange("p (h two) k -> p h two k", two=2)
                    sif4 = sif[:].rearrange("p (h two) k -> p h two k", two=2)
                    o.op("dve", lambda e: e.tensor_tensor(out=cand.rearrange("p h (a b) -> p h a b", a=16),
                                                          in0=sv4[:, :, 0, :].unsqueeze(3).to_broadcast([128, 8, 16, 16]),
                                                          in1=sv4[:, :, 1, :].unsqueeze(2).to_broadcast([128, 8, 16, 16]), op=ALU.add),
                         reads=["sv"], writes=["sc"])
                    for h in range(8):
                        o.op("dve", lambda e, h=h: e.max(out=fv[:, h, 0:8], in_=cand[:, h, :]), reads=["sc"], writes=["fv"])
                        o.op("dve", lambda e, h=h: e.max_index(out=fpos[:, h, 0:8], in_max=fv[:, h, 0:8], in_values=cand[:, h, :]), reads=["sc", "fv"], writes=["fpos"])
                        o.op("dve", lambda e, h=h: e.match_replace(out=cand2[:, h, :], in_to_replace=fv[:, h, 0:8], in_values=cand[:, h, :], imm_value=-1e30),
                             reads=["sc", "fv"], writes=["sc2"])
                        o.op("dve", lambda e, h=h: e.max(out=fv[:, h, 8:16], in_=cand2[:, h, :]), reads=["sc2"], writes=["fv"])
                        o.op("dve", lambda e, h=h: e.max_index(out=fpos[:, h, 8:16], in_max=fv[:, h, 8:16], in_values=cand2[:, h, :]), reads=["sc2", "fv"], writes=["fpos"])
                    o.op("dve", lambda e: e.tensor_tensor(out=ge[:], in0=fv[:], in1=fv[:, :, 0:1].to_broadcast([128, 8, 16]), op=ALU.subtract),
                         reads=["fv"], writes=["ge"])
                    o.op("act", lambda e: e.activation(out=ge[:], in_=ge[:], func=AF.Exp), reads=["ge"], writes=["ge"])
                    o.op("dve", lambda e: e.tensor_reduce(out=gz[:], in_=ge[:], axis=AX.X, op=ALU.add), reads=["ge"], writes=["gz"])
                    o.op("dve", lambda e: e.reciprocal(out=gz[:], in_=gz[:]), reads=["gz"], writes=["gz"])
                    o.op("dve", lambda e: e.tensor_tensor(out=gate[:].rearrange("p (h k) -> p h k", h=8), in0=ge[:],
                                                          in1=gz[:].unsqueeze(2).to_broadcast([128, 8, 16]), op=ALU.mult),
                         reads=["ge", "gz"], writes=[gk])
                    o.op("dve", lambda e: e.tensor_single_scalar(out=fa[:], in_=fpos[:], scalar=4, op=ALU.logical_shift_right), reads=["fpos"], writes=["fa"])
                    o.op("dve", lambda e: e.tensor_single_scalar(out=fb[:], in_=fpos[:], scalar=15, op=ALU.bitwise_and), reads=["fpos"], writes=["fb"])
                    o.op("dve", lambda e: e.tensor_copy(out=faf[:], in_=fa[:]), reads=["fa"], writes=["faf"])
                    o.op("dve", lambda e: e.tensor_copy(out=fbf[:], in_=fb[:]), reads=["fb"], writes=["fbf"])
                    io4 = iota16[:].unsqueeze(1).unsqueeze(1).to_broadcast([128, 8, 16, 16])
                    for (srcf, sk_, half, dstt, dk) in ((faf, "faf", 0, Ii, "Ii"), (fbf, "fbf", 1, Jj, "Jj")):
                        o.op("dve", lambda e, srcf=srcf: e.tensor_tensor(out=oh, in0=srcf[:].unsqueeze(3).to_broadcast([128, 8, 16, 16]), in1=io4, op=ALU.is_equal),
                             reads=[sk_, "iota16"], writes=["sc2"])
                        o.op("dve", lambda e, half=half: e.tensor_tensor(out=oh, in0=oh, in1=sif4[:, :, half, :].unsqueeze(2).to_broadcast([128, 8, 16, 16]), op=ALU.mult),
                             reads=["sc2", "sif"], writes=["sc2"])
                        o.op("dve", lambda e, dstt=dstt: e.tensor_reduce(out=dstt[:], in_=oh, axis=AX.X, op=ALU.add), reads=["sc2"], writes=[dk])
                    o.op("dve", lambda e: e.scalar_tensor_tensor(out=eif[:].rearrange("p (h k) -> p h k", h=8), in0=Ii[:], scalar=128.0, in1=Jj[:], op0=ALU.mult, op1=ALU.add),
                         reads=["Ii", "Jj"], writes=["eif"])
                    if l > 0:
                        o.op("dve", lambda e: e.tensor_scalar(out=eif[:], in0=eif[:], scalar1=float(l * NEXP), scalar2=None, op0=ALU.add),
                             reads=["eif"], writes=["eif"])
                    o.op("dve", lambda e: e.tensor_copy(out=eidx[:], in_=eif[:]), reads=["eif"], writes=[ek])

                nuv = [0]

                def stageB(i, filler):
                    xt = xts[i % 2]; xk = "xt%d" % (i % 2)
                    eidx = eidxs[i % 2]; ek = "eidx%d" % (i % 2)
                    gate = gates[i % 2]; gk = "gate%d" % (i % 2)
                    hp_ = hps[i % 2]; hpk = "hps%d" % (i % 2)
                    o = S_()
                    LOOK = 8
                    order = list(range(128))
                    bufs = {}

                    def issue(sl):
                        b = nuv[0] % NB; nuv[0] += 1
                        bufs[sl] = b
                        ub = UV[b]
                        o.dma("pool", lambda e, ub=ub, sl=sl: e.indirect_dma_start(
                            out=ub[:], out_offset=None, in_=uvb[:, :],
                            in_offset=bass.IndirectOffsetOnAxis(ap=eidx[:, sl:sl + 1], axis=0)), reads=[ek, ("uvb", l)], writes=["UV%d" % b])

                    for sl in range(min(LOOK, 128)):
                        issue(sl)
                    GS = 4
                    for g0 in range(0, 128, GS):
                        for sl in range(g0, g0 + GS):
                            if sl + LOOK < 128:
                                issue(sl + LOOK)
                            b = bufs[sl]; ub = UV[b]; ubk = "UV%d" % b
                            o.op("dve", lambda e, ub=ub, sl=sl: e.scalar_tensor_tensor(out=dj[:], in0=ub[:, 0:D], scalar=1.0, in1=hp_[:], op0=ALU.mult, op1=ALU.mult,
                                                                                       accum_out=act[:, sl:sl + 1]),
                                 reads=[ubk, hpk], writes=["dj", "act"])
                            filler()
                        hs = slice(g0, g0 + GS)
                        o.op("act", lambda e, hs=hs: e.activation(out=wgt[:, hs], in_=act[:, hs], func=AF.Gelu), reads=["act"], writes=["wgt"])
                        o.op("dve", lambda e, hs=hs: e.tensor_tensor(out=wgt[:, hs], in0=wgt[:, hs], in1=gate[:, hs], op=ALU.mult), reads=["wgt", gk], writes=["wgt"])
                        for sl in range(g0, g0 + GS):
                            b = bufs[sl]; ub = UV[b]; ubk = "UV%d" % b
                            dg = diag[sl % 2]; dgk = "diag%d" % (sl % 2)
                            o.op("act", lambda e, dg=dg, sl=sl: e.activation(out=dg[:], in_=ident_f[:], func=AF.Copy, scale=wgt[:, sl:sl + 1]),
                                 reads=["ident_f", "wgt"], writes=[dgk])
                            for half in range(2):
                                mm(pso[half][:, :], dg[:], ub[:, D + half * 512: D + (half + 1) * 512], sl == 0, sl == 127, [dgk, ubk], ["psop%d" % half])
                    residual_out(pso, ["psop0", "psop1"], G2, xt, xk, xo, dst, i, fin=fin_t)

                stageA(0)
                for i in range(NT):
                    nxt = Deferred()
                    if i + 1 < NT:
                        sink[0] = nxt
                        stageA(i + 1)
                        sink[0] = s
                    per = (len(nxt.q) + 127) // 128
                    stageB(i, lambda: nxt.run(s, per))
                    nxt.run(s, None)
                s.flush()

        cur = IN["x"]
        free = [0, 1, 2]

        def take(exclude):
            for b in free:
                if xs[b] is not exclude and all(xs[b] is not e_ for e_ in exclude if e_ is not None):
                    return xs[b]
            raise RuntimeError("no buffer")

        nl = len(layers)
        for li, l in enumerate(layers):
            compute_mod(l)
            if do_mixer:
                if l % 2 == 0:
                    d1 = [b for b in xs if b is not cur][0]
                    even_mixer(l, cur, d1)
                    cur = d1
                else:
                    others = [b for b in xs if b is not cur]
                    odd_mixer(l, cur, cur, others[0], 0)
                    odd_mixer(l, cur, others[0], others[1], 1)
                    cur = others[1]
            if do_peer:
                last = (li == nl - 1)
                d2 = y_out if last else [b for b in xs if b is not cur][0]
                peer_pass(l, cur, d2, fin=(last and final_norm))
                cur = d2
        if cur is not y_out:
            with contextlib.ExitStack() as ph:
                tt = T("cp", [128, D], stack=ph)
                for i in range(NT):
                    ld(tt[:], cur[i * 128:(i + 1) * 128, :], ["cp"], r=[(cur.tensor.name, i)])
                    ld(y_out[i * 128:(i + 1) * 128, :], tt[:], [("y", i)], r=["cp"])
                s.flush()
        s.wait_all("sp")
        s.flush()
        build.n_instr = s.n_instr
    return nc


def make_in_maps(inputs, n_cores, S=4096):
    consts = host_consts()
    shared = {}
    for k in IN_SHAPES:
        if k in consts:
            shared[k] = consts[k]
        else:
            shared[k] = np.ascontiguousarray(np.asarray(inputs[k], dtype=np.float32)).reshape(IN_SHAPES[k][0])
    maps = []
    x = np.asarray(inputs["x"], dtype=np.float32)
    c = np.asarray(inputs["c"], dtype=np.float32)
    pos = np.asarray(inputs["positions"]).astype(np.int32)
    for b in range(n_cores):
        m = dict(shared)
        m["x"] = np.ascontiguousarray(x[b, :S])
        m["c"] = np.ascontiguousarray(c[b:b + 1])
        m["positions"] = np.ascontiguousarray(pos[b:b + 1, :S])
        maps.append(m)
    return maps


def kernel(**inputs):
    n = 8
    nc = build(4096)
    maps = make_in_maps(inputs, n)
    res = run_bass_kernel_spmd(nc, maps, core_ids=list(range(n)))
    return np.stack([np.asarray(r["y"], dtype=np.float32) for r in res.results], axis=0)
```

```python
import contextlib
import math
import numpy as np
import concourse.bass as bass
import concourse.mybir as mybir
from concourse.bass_utils import run_bass_kernel_spmd

F32 = mybir.dt.float32
BF16 = mybir.dt.bfloat16
U32 = mybir.dt.uint32
I32 = mybir.dt.int32
AF = mybir.ActivationFunctionType
ALU = mybir.AluOpType
AX = mybir.AxisListType

D = 1024
EPS = 1e-6
NEXP = 16384
ENGS = ("pe", "act", "dve", "pool", "sp")
SAME_ENGINE_SYNC = True
NO_SAME_SYNC = set()


class Sched:
    def __init__(self, nc, st, n_dma_sems=12):
        self.nc = nc
        self.ops = {e: [] for e in ENGS}
        self.cnt = {e: 0 for e in ENGS}
        self.known = {e: {} for e in ENGS}
        self.lastw = {}
        self.reads = {}
        self.n_dma_sems = n_dma_sems
        self.dma_cnt = {}
        self.dma_rr = {e: 0 for e in ENGS}
        self.semh = {}
        for e in ENGS:
            self.semh[e] = st.enter_context(nc.semaphore("s_" + e))
        for q in ("sp", "pool", "act"):
            for i in range(n_dma_sems):
                self.semh[(q, i)] = st.enter_context(nc.semaphore("d_%s%d" % (q, i)))
        self.n_instr = 0

    def _deps(self, eng, reads, writes, skip_same):
        waits = {}

        def need(ev):
            if ev is None:
                return
            k, v = ev
            if k == eng and (skip_same or not SAME_ENGINE_SYNC or eng in NO_SAME_SYNC):
                return
            if self.known[eng].get(k, 0) >= v:
                return
            if waits.get(k, 0) < v:
                waits[k] = v

        for b in reads:
            need(self.lastw.get(b))
        for b in writes:
            need(self.lastw.get(b))
            for ev in self.reads.get(b, ()):
                need(ev)
        for k, v in waits.items():
            self.known[eng][k] = v
        return waits

    def _record(self, ev, reads, writes):
        for b in reads:
            lst = self.reads.setdefault(b, [])
            lst[:] = [x for x in lst if x[0] != ev[0]]
            lst.append(ev)
        for b in writes:
            self.lastw[b] = ev
            self.reads[b] = []

    def op(self, eng, fn, reads=(), writes=(), skip_same=False):
        waits = self._deps(eng, reads, writes, skip_same)
        self.cnt[eng] += 1
        ev = (eng, self.cnt[eng])
        self.ops[eng].append(("op", fn, waits, ev))
        self._record(ev, reads, writes)
        return ev

    def dma(self, queue, fn, reads=(), writes=()):
        i = self.dma_rr[queue]
        self.dma_rr[queue] = (i + 1) % self.n_dma_sems
        key = (queue, i)
        prev = self.dma_cnt.get(key, 0)
        waits = self._deps(queue, reads, writes, False)
        if prev > 0 and self.known[queue].get(key, 0) < prev * 16:
            waits[key] = prev * 16
            self.known[queue][key] = prev * 16
        self.dma_cnt[key] = prev + 1
        ev = (key, (prev + 1) * 16)
        self.ops[queue].append(("dma", fn, waits, ev))
        self._record(ev, reads, writes)
        return ev

    def wait_all(self, eng):
        waits = {}
        for e in ENGS:
            if e != eng and self.cnt[e] > 0:
                waits[e] = self.cnt[e]
        for key, c in self.dma_cnt.items():
            waits[key] = c * 16
        self.ops[eng].append(("wait", None, waits, None))

    def flush(self):
        nc = self.nc
        if not any(self.ops[e] for e in ENGS):
            return
        with nc.Block() as block:
            regs = {"pe": block.tensor, "act": block.scalar, "dve": block.vector,
                    "pool": block.gpsimd, "sp": block.sync}

            def make(e, lst):
                def body(engobj):
                    for kind, fn, waits, ev in lst:
                        for k, v in waits.items():
                            engobj.wait_ge(self.semh[k], v)
                        if kind == "wait":
                            continue
                        ins = fn(engobj)
                        if kind == "dma":
                            ins.then_inc(self.semh[ev[0]], 16)
                        else:
                            ins.then_inc(self.semh[e], 1)
                return body

            for e in ENGS:
                lst = self.ops[e]
                self.n_instr += len(lst)
                if lst:
                    regs[e](make(e, lst))
        self.ops = {e: [] for e in ENGS}


class Deferred:
    def __init__(self):
        self.q = []

    def op(self, *a, **k):
        self.q.append(("op", a, k))

    def dma(self, *a, **k):
        self.q.append(("dma", a, k))

    def run(self, s, n):
        m = len(self.q) if n is None else min(n, len(self.q))
        for kind, a, k in self.q[:m]:
            getattr(s, kind)(*a, **k)
        del self.q[:m]


def host_consts():
    ident = np.eye(128, dtype=np.float32)
    p = np.arange(128)[:, None]
    j = np.arange(128)[None, :]
    triu = (j >= p).astype(np.float32)
    poolm = np.zeros((12, 128, 128), np.float32)
    for g, w in enumerate((2, 4, 8, 16)):
        band = ((j - p >= 0) & (j - p <= w - 1)).astype(np.float32)
        poolm[g] = band / w - ident
        cnt = np.minimum(np.arange(128) + 1, w).astype(np.float32)[None, :]
        poolm[4 + g] = band / cnt - ident
        poolm[8 + g] = ((j - p + 128) <= (w - 1)).astype(np.float32) / w
    iota16 = np.tile(np.arange(16, dtype=np.float32)[None, :], (128, 1))
    inv_freq = 1.0 / (10000.0 ** (np.arange(0, 64, 2, dtype=np.float32) / 64.0))
    invf = np.tile((inv_freq / (2.0 * np.pi)).astype(np.float32)[None, :], (128, 1))
    return {"k_ident": ident, "k_triu": triu, "k_poolm": poolm.transpose(1, 0, 2).copy(),
            "k_iota16": iota16, "k_invf": invf}


IN_SHAPES = {
    "ada_w": ([4, 1024, 6144], F32), "ada_b": ([4, 6144], F32),
    "norm_mix": ([4, 1024], F32), "norm_ffn": ([4, 1024], F32),
    "ev_w_in": ([2, 1024, 1536], F32), "ev_g_v": ([2, 512], F32),
    "ev_w_s": ([2, 4, 128, 128], F32), "ev_b_s": ([2, 512], F32),
    "ev_w_pool": ([2, 4, 128, 128], F32), "ev_pool_scale": ([2, 512], F32),
    "ev_w_out": ([2, 1024, 1024], F32), "od_w_in": ([2, 1024, 3072], F32),
    "od_lam_q1": ([2, 64], F32), "od_lam_k1": ([2, 64], F32),
    "od_lam_q2": ([2, 64], F32), "od_lam_k2": ([2, 64], F32),
    "od_g_sub": ([2, 128], F32), "od_w_out": ([2, 1024, 1024], F32),
    "peer_w_q": ([4, 1024, 2048], F32), "peer_sub_keys": ([4, 16, 128, 128], F32),
    "peer_u": ([4, NEXP, 1024], F32), "peer_v": ([4, NEXP, 1024], F32),
    "final_norm": ([1, 1024], F32),
    "k_ident": ([128, 128], F32), "k_triu": ([128, 128], F32), "k_poolm": ([128, 12, 128], F32),
    "k_iota16": ([128, 16], F32), "k_invf": ([128, 32], F32),
}


def build(S=4096, layers=(0, 1, 2, 3), dbg=False, do_mixer=True, do_peer=True, final_norm=True):
    NT = S // 128
    nc = bass.Bass("TRN2", target_bir_lowering=False)
    IN = {}
    IN["x"] = nc.dram_tensor("x", [S, D], F32, kind="ExternalInput").ap()
    IN["c"] = nc.dram_tensor("c", [1, D], F32, kind="ExternalInput").ap()
    IN["positions"] = nc.dram_tensor("positions", [1, S], I32, kind="ExternalInput").ap()
    for k, (shp, dt) in IN_SHAPES.items():
        IN[k] = nc.dram_tensor(k, shp, dt, kind="ExternalInput").ap()
    y_out = nc.dram_tensor("y", [S, D], F32, kind="ExternalOutput").ap()
    xs = [nc.dram_tensor("xs%d" % i, [S, D], F32,
                         kind=("ExternalOutput" if dbg else "Internal")).ap() for i in range(3)]

    uvb = nc.dram_tensor("uvb", [4 * NEXP, 2 * D], BF16, kind="Internal").ap()

    with contextlib.ExitStack() as st:
        s = Sched(nc, st)
        for h in s.semh.values():
            nc.gpsimd.sem_clear(h)
        nc.all_engine_barrier()

        uid = [0]

        def T(name, shape, dt=F32, stack=st):
            uid[0] += 1
            return stack.enter_context(nc.sbuf_tensor("%s_%d" % (name, uid[0]), shape, dt))

        def PS(name, shape, dt=F32, stack=st):
            uid[0] += 1
            return stack.enter_context(nc.psum_tensor("%s_%d" % (name, uid[0]), shape, dt))

        sink = [s]

        def S_():
            return sink[0]

        def mm(out, lhsT, rhs, start, stop, r, w):
            sink[0].op("pe", lambda e: e.matmul(out, lhsT=lhsT, rhs=rhs, start=start, stop=stop),
                 reads=r, writes=w, skip_same=True)

        def tr(out, in_, ident, r, w):
            sink[0].op("pe", lambda e: e.transpose(out=out, in_=in_, identity=ident),
                 reads=r, writes=w, skip_same=True)

        def ld(out, in_, w, r=(), q="sp", slow=False):
            if slow:
                sink[0].dma(q, lambda e: e.dma_start(out=out, in_=in_, allow_slow_non_contiguous=True), reads=r, writes=w)
            else:
                sink[0].dma(q, lambda e: e.dma_start(out=out, in_=in_), reads=r, writes=w)

        ident_f = T("ident_f", [128, 128]); ident_b = T("ident_b", [128, 128], BF16)
        triu_b = T("triu_b", [128, 128], BF16); triu_f = T("triu_f", [128, 128])
        iota16 = T("iota16", [128, 16])
        ones_row = T("ones_row", [1, 128])
        modbc = [T("modbc%d" % i, [128, D]) for i in range(6)]
        A1, B1, G1, A2, B2, G2 = modbc
        cosT = T("cosT", [128, NT, 32]); sinT = T("sinT", [128, NT, 32]); nsinT = T("nsinT", [128, NT, 32])
        c_act = T("c_act", [128, 8])

        ld(ident_f[:], IN["k_ident"][:, :], ["ident_f"])
        ld(triu_f[:], IN["k_triu"][:, :], ["triu_f"])
        ld(iota16[:], IN["k_iota16"][:, :], ["iota16"])
        s.op("dve", lambda e: e.tensor_copy(out=ident_b[:], in_=ident_f[:]), reads=["ident_f"], writes=["ident_b"])
        s.op("dve", lambda e: e.tensor_copy(out=triu_b[:], in_=triu_f[:]), reads=["triu_f"], writes=["triu_b"])
        s.op("dve", lambda e: e.memset(ones_row[:], 1.0), writes=["ones_row"])

        if do_peer:
            for l_ in layers:
                for c_ in range(16):
                    r0 = l_ * NEXP + c_ * 1024
                    s.dma("pool", lambda e, l_=l_, c_=c_, r0=r0: e.dma_start(out=uvb[r0:r0 + 1024, 0:D], in_=IN["peer_u"][l_, c_ * 1024:(c_ + 1) * 1024, :]),
                          writes=[("uvb", l_)])
                    s.dma("pool", lambda e, l_=l_, c_=c_, r0=r0: e.dma_start(out=uvb[r0:r0 + 1024, D:2 * D], in_=IN["peer_v"][l_, c_ * 1024:(c_ + 1) * 1024, :]),
                          writes=[("uvb", l_)])

        with contextlib.ExitStack() as ph:
            c_row = T("c_row", [1, D], stack=ph)
            one11 = T("one11", [1, 1], stack=ph)
            pc = PS("pc", [128, 8], stack=ph)
            ld(c_row[:], IN["c"][:, :], ["c_row"])
            s.op("dve", lambda e: e.memset(one11[:], 1.0), writes=["one11"])
            for k in range(8):
                mm(pc[:, k:k + 1], c_row[0:1, k * 128:(k + 1) * 128], one11[0:1, 0:1], True, True,
                   ["c_row", "one11"], ["pc"])
            s.op("act", lambda e: e.activation(out=c_act[:], in_=pc[:], func=AF.Silu), reads=["pc"], writes=["c_act"])
            pos_i = T("pos_i", [128, NT], I32, stack=ph)
            pos_f = T("pos_f", [128, NT], stack=ph)
            invf = T("invf", [128, 32], stack=ph)
            yy = T("yy", [128, NT, 32], stack=ph); y2 = T("y2", [128, NT, 32], stack=ph)
            ki = T("ki", [128, NT, 32], I32, stack=ph); kf = T("kf", [128, NT, 32], stack=ph)
            ld(pos_i[:], IN["positions"][0, :].rearrange("(n p) -> p n", p=128), ["pos_i"], slow=True)
            ld(invf[:], IN["k_invf"][:, :], ["invf"])
            s.op("dve", lambda e: e.tensor_copy(out=pos_f[:], in_=pos_i[:]), reads=["pos_i"], writes=["pos_f"])
            s.op("dve", lambda e: e.tensor_tensor(out=yy[:], in0=pos_f[:].unsqueeze(2).to_broadcast([128, NT, 32]),
                                                  in1=invf[:].unsqueeze(1).to_broadcast([128, NT, 32]), op=ALU.mult),
                 reads=["pos_f", "invf"], writes=["yy"])
            s.op("dve", lambda e: e.tensor_copy(out=ki[:], in_=yy[:]), reads=["yy"], writes=["ki"])
            s.op("dve", lambda e: e.tensor_copy(out=kf[:], in_=ki[:]), reads=["ki"], writes=["kf"])
            s.op("dve", lambda e: e.tensor_tensor(out=y2[:], in0=yy[:], in1=kf[:], op=ALU.subtract), reads=["yy", "kf"], writes=["y2"])
            s.op("act", lambda e: e.activation(out=sinT[:], in_=y2[:], func=AF.Sin, scale=2.0 * math.pi), reads=["y2"], writes=["sinT"])
            s.op("dve", lambda e: e.tensor_scalar(out=nsinT[:], in0=sinT[:], scalar1=-1.0, scalar2=None, op0=ALU.mult), reads=["sinT"], writes=["nsinT"])
            s.op("dve", lambda e: e.tensor_scalar(out=yy[:], in0=yy[:], scalar1=0.25, scalar2=None, op0=ALU.add), reads=["yy"], writes=["yy"])
            s.op("dve", lambda e: e.tensor_copy(out=ki[:], in_=yy[:]), reads=["yy"], writes=["ki"])
            s.op("dve", lambda e: e.tensor_copy(out=kf[:], in_=ki[:]), reads=["ki"], writes=["kf"])
            s.op("dve", lambda e: e.tensor_tensor(out=y2[:], in0=yy[:], in1=kf[:], op=ALU.subtract), reads=["yy", "kf"], writes=["y2"])
            s.op("act", lambda e: e.activation(out=cosT[:], in_=y2[:], func=AF.Sin, scale=2.0 * math.pi), reads=["y2"], writes=["cosT"])
            s.flush()

        def compute_mod(l):
            with contextlib.ExitStack() as ph:
                wt = [T("adaw%d" % i, [128, 8, 512], stack=ph) for i in range(2)]
                brow = T("adab", [1, 6 * D], stack=ph)
                mrow = T("mrow", [1, 6 * D], stack=ph)
                nbc = [T("nbc%d" % i, [128, D], stack=ph) for i in range(2)]
                pm = [PS("pm%d" % i, [1, 512], stack=ph) for i in range(2)]
                pb = [PS("pb%d" % i, [128, 512], stack=ph) for i in range(2)]
                ld(brow[:], IN["ada_b"][l:l + 1, :], ["adab"])
                ld(nbc[0][:], IN["norm_mix"][l, :].partition_broadcast(128), ["nbc0"])
                ld(nbc[1][:], IN["norm_ffn"][l, :].partition_broadcast(128), ["nbc1"])
                for cb in range(12):
                    w = wt[cb % 2]; wk = "adaw%d" % (cb % 2); pk = "pm%d" % (cb % 2)
                    ld(w[:], IN["ada_w"][l, :, cb * 512:(cb + 1) * 512].rearrange("(k p) f -> p k f", p=128), [wk])
                    for k in range(8):
                        mm(pm[cb % 2][:, :], c_act[:, k:k + 1], w[:, k, :], k == 0, k == 7, ["c_act", wk], [pk])
                    s.op("dve", lambda e, cb=cb: e.tensor_tensor(out=mrow[0:1, cb * 512:(cb + 1) * 512], in0=pm[cb % 2][:, :],
                                                                 in1=brow[0:1, cb * 512:(cb + 1) * 512], op=ALU.add),
                         reads=[pk, "adab"], writes=["mrow"])
                dst = {0: (B1, None), 1: (A1, 0), 2: (G1, None), 3: (B2, None), 4: (A2, 1), 5: (G2, None)}
                n = 0
                for i in range(6):
                    tgt, nb = dst[i]
                    for half in range(2):
                        pbk = "pb%d" % (n % 2); pbt = pb[n % 2]; n += 1
                        mm(pbt[:, :], ones_row[0:1, :], mrow[0:1, i * D + half * 512: i * D + (half + 1) * 512], True, True,
                           ["ones_row", "mrow"], [pbk])
                        o = tgt[:, half * 512:(half + 1) * 512]
                        if nb is None:
                            s.op("act", lambda e, o=o, pbt=pbt: e.copy(out=o, in_=pbt[:, :]), reads=[pbk], writes=[tgt.name])
                        else:
                            nbt = nbc[nb][:, half * 512:(half + 1) * 512]
                            s.op("dve", lambda e, o=o, pbt=pbt, nbt=nbt: e.scalar_tensor_tensor(
                                out=o, in0=pbt[:, :], scalar=1.0, in1=nbt, op0=ALU.add, op1=ALU.mult),
                                reads=[pbk, "nbc%d" % nb], writes=[tgt.name])
                s.flush()

        def load_x(xt, key, src, i):
            ld(xt[:], src[i * 128:(i + 1) * 128, :], [key], r=[(src.tensor.name, i)])

        def norm_mod(ph_t, xt, xkey, A, B, want_f32=False):
            junk, ss, rs, hf, hb = ph_t["junk"], ph_t["ss"], ph_t["rs"], ph_t["hf"], ph_t["hb"]
            s.op("act", lambda e: e.activation(out=junk[:], in_=xt[:], func=AF.Square, accum_out=ss[:]),
                 reads=[xkey], writes=["junk", "ss"])
            s.op("act", lambda e: e.activation(out=rs[:], in_=ss[:], func=AF.Sqrt, scale=1.0 / D, bias=EPS),
                 reads=["ss"], writes=["rs"])
            s.op("dve", lambda e: e.reciprocal(out=rs[:], in_=rs[:]), reads=["rs"], writes=["rs"])
            s.op("dve", lambda e: e.scalar_tensor_tensor(out=hf[:], in0=xt[:], scalar=rs[:, 0:1], in1=A[:],
                                                         op0=ALU.mult, op1=ALU.mult),
                 reads=[xkey, "rs", A.name], writes=["hf"])
            if want_f32:
                s.op("pool", lambda e: e.tensor_tensor(out=hf[:], in0=hf[:], in1=B[:], op=ALU.add),
                     reads=["hf", B.name], writes=["hf"])
                s.op("act", lambda e: e.copy(out=hb[:], in_=hf[:]), reads=["hf"], writes=["hb"])
            else:
                s.op("pool", lambda e: e.tensor_tensor(out=hb[:], in0=hf[:], in1=B[:], op=ALU.add),
                     reads=["hf", B.name], writes=["hb"])

        def transpose8(hb, hbkey, tp, hT):
            for k in range(8):
                tr(tp[:, k, :], hb[:, k * 128:(k + 1) * 128], ident_b[:], [hbkey, "ident_b"], ["tp"])
            s.op("act", lambda e: e.copy(out=hT[:], in_=tp[:]), reads=["tp"], writes=["hT"])

        def wload_bf16(dst, key, src, nk=8, parts=4):
            step = nk // parts
            for a in range(parts):
                ld(dst[:, a * step:(a + 1) * step, :],
                   src[a * step * 128:(a + 1) * step * 128, :].rearrange("(k p) f -> p k f", p=128), [key], q="pool")

        def residual_out(pso, psk, Gt, xres, xreskey, xo, dst, i, fin=None):
            for half in range(2):
                sl = slice(half * 512, (half + 1) * 512)
                s.op("dve", lambda e, half=half, sl=sl: e.tensor_tensor(out=xo[:, sl], in0=pso[half][:, :], in1=Gt[:, sl], op=ALU.mult),
                     reads=[psk[half], Gt.name], writes=["xo"])
            s.op("pool", lambda e: e.tensor_tensor(out=xo[:], in0=xo[:], in1=xres[:], op=ALU.add),
                 reads=["xo", xreskey], writes=["xo"])
            if fin is None:
                ld(dst[i * 128:(i + 1) * 128, :], xo[:], [(dst.tensor.name, i)], r=["xo"])
            else:
                junk, ss, rs, fnbc, yo = fin
                s.op("act", lambda e: e.activation(out=junk[:], in_=xo[:], func=AF.Square, accum_out=ss[:]),
                     reads=["xo"], writes=["fss"])
                s.op("act", lambda e: e.activation(out=rs[:], in_=ss[:], func=AF.Sqrt, scale=1.0 / D, bias=EPS),
                     reads=["fss"], writes=["frs"])
                s.op("dve", lambda e: e.reciprocal(out=rs[:], in_=rs[:]), reads=["frs"], writes=["frs"])
                s.op("dve", lambda e: e.scalar_tensor_tensor(out=yo[:], in0=xo[:], scalar=rs[:, 0:1], in1=fnbc[:],
                                                             op0=ALU.mult, op1=ALU.mult),
                     reads=["xo", "frs", "fnbc"], writes=["xo"])
                ld(dst[i * 128:(i + 1) * 128, :], yo[:], [(dst.tensor.name, i)], r=["xo"])

        def common_tiles(ph):
            d = {}
            d["junk"] = T("junk", [128, D], BF16, stack=ph)
            d["ss"] = T("ss", [128, 1], stack=ph)
            d["rs"] = T("rs", [128, 1], stack=ph)
            d["hf"] = T("hf", [128, D], stack=ph)
            d["hb"] = T("hb", [128, D], BF16, stack=ph)
            d["hT"] = T("hT", [128, 8, 128], BF16, stack=ph)
            d["xo"] = T("xo", [128, D], stack=ph)
            d["xt"] = [T("xt%d" % i, [128, D], stack=ph) for i in range(2)]
            return d

        def even_mixer(l, src, dst):
            e_ = l // 2
            with contextlib.ExitStack() as ph:
                ct = common_tiles(ph)
                Win = T("Win", [128, 8, 1536], BF16, stack=ph)
                Wout = T("Wout", [128, 8, D], BF16, stack=ph)
                wsraw = T("wsraw", [128, 4, 128], stack=ph)
                wsT = T("wsT", [128, 4, 128], BF16, stack=ph)
                bsrow = T("bsrow", [1, 512], stack=ph)
                wpool = T("wpool", [128, 4, 128], BF16, stack=ph)
                gvbc = T("gvbc", [128, 512], stack=ph)
                lscol = T("lscol", [128, 4], stack=ph)
                poolm = T("poolm", [128, 12, 128], stack=ph)
                uTs = T("uTs", [128, 4, 128], stack=ph)
                vs = T("vs", [128, 512], stack=ph)
                vn = T("vn", [128, 512], BF16, stack=ph)
                bst = T("bst", [128, 6], stack=ph); bag = T("bag", [128, 2], stack=ph); sd = T("sd", [128, 1], stack=ph)
                pcur = [T("pcur%d" % i, [128, 512], stack=ph) for i in range(2)]
                pooledT = T("pooledT", [128, 4, 128], BF16, stack=ph)
                yabT = T("yabT", [128, 8, 128], BF16, stack=ph)
                tp = PS("tp", [128, 8, 128], BF16, stack=ph)
                psU = PS("psU", [128, 4, 128], stack=ph)
                psV = PS("psV", [128, 512], stack=ph)
                psP = PS("psP", [128, 512], stack=ph)
                psS = PS("psS", [128, 4, 128], stack=ph)
                psQ = PS("psQ", [128, 4, 128], stack=ph)
                pso = [PS("pso%d" % i, [128, 512], stack=ph) for i in range(2)]

                wload_bf16(Win, "Win", IN["ev_w_in"][e_])
                wload_bf16(Wout, "Wout", IN["ev_w_out"][e_])
                ld(wsraw[:], IN["ev_w_s"][e_].rearrange("g t s -> t g s"), ["wsraw"])
                ld(bsrow[:], IN["ev_b_s"][e_:e_ + 1, :], ["bsrow"])
                ld(wpool[:], IN["ev_w_pool"][e_].rearrange("g c d -> c g d"), ["wpool"], q="pool")
                ld(gvbc[:], IN["ev_g_v"][e_, :].partition_broadcast(128), ["gvbc"])
                ld(lscol[:], IN["ev_pool_scale"][e_, :].rearrange("(g d) -> d g", d=128), ["lscol"], slow=True)
                ld(poolm[:], IN["k_poolm"][:, :, :], ["poolm"])
                for g in range(4):
                    tr(psS[:, g, :], wsraw[:, g, :], ident_f[:], ["wsraw", "ident_f"], ["psS"])
                s.op("dve", lambda e: e.tensor_tensor(out=wsT[:], in0=psS[:], in1=triu_f[:].unsqueeze(1).to_broadcast([128, 4, 128]), op=ALU.mult),
                     reads=["psS", "triu_f"], writes=["wsT"])

                load_x(ct["xt"][0], "xt0", src, 0)
                for i in range(NT):
                    xt = ct["xt"][i % 2]; xk = "xt%d" % (i % 2)
                    if i + 1 < NT:
                        load_x(ct["xt"][(i + 1) % 2], "xt%d" % ((i + 1) % 2), src, i + 1)
                    norm_mod(ct, xt, xk, A1, B1)
                    hb, hT = ct["hb"], ct["hT"]
                    transpose8(hb, "hb", tp, hT)
                    for fc in range(4):
                        for k in range(8):
                            mm(psU[:, fc, :], Win[:, k, fc * 128:(fc + 1) * 128], hT[:, k, :], k == 0, k == 7, ["Win", "hT"], ["psU"])
                    s.op("act", lambda e: e.activation(out=uTs[:], in_=psU[:], func=AF.Gelu), reads=["psU"], writes=["uTs"])
                    for k in range(8):
                        mm(psV[:, :], hT[:, k, :], Win[:, k, 512:1024], k == 0, k == 7, ["Win", "hT"], ["psV"])
                    s.op("act", lambda e: e.activation(out=vs[:], in_=psV[:], func=AF.Gelu), reads=["psV"], writes=["vs"])
                    s.op("dve", lambda e: e.bn_stats(out=bst[:], in_=vs[:]), reads=["vs"], writes=["bst"])
                    s.op("dve", lambda e: e.bn_aggr(out=bag[:], in_=bst[:]), reads=["bst"], writes=["bag"])
                    s.op("act", lambda e: e.activation(out=sd[:], in_=bag[:, 1:2], func=AF.Sqrt, scale=1.0, bias=EPS), reads=["bag"], writes=["sd"])
                    s.op("dve", lambda e: e.reciprocal(out=sd[:], in_=sd[:]), reads=["sd"], writes=["sd"])
                    s.op("dve", lambda e: e.tensor_scalar(out=vs[:], in0=vs[:], scalar1=bag[:, 0:1], scalar2=sd[:, 0:1],
                                                          op0=ALU.subtract, op1=ALU.mult), reads=["vs", "bag", "sd"], writes=["vs"])
                    s.op("pool", lambda e: e.tensor_tensor(out=vn[:], in0=vs[:], in1=gvbc[:], op=ALU.mult), reads=["vs", "gvbc"], writes=["vn"])
                    pc_ = pcur[i % 2]; pck = "pcur%d" % (i % 2); pp_ = pcur[(i + 1) % 2]; ppk = "pcur%d" % ((i + 1) % 2)
                    for k in range(8):
                        mm(psP[:, :], hT[:, k, :], Win[:, k, 1024:1536], k == 0, k == 7, ["Win", "hT"], ["psP"])
                    s.op("act", lambda e, pc_=pc_: e.copy(out=pc_[:], in_=psP[:]), reads=["psP"], writes=[pck])
                    for g in range(4):
                        mm(psS[:, g, :], vn[:, g * 128:(g + 1) * 128], wsT[:, g, :], True, False, ["vn", "wsT"], ["psS"])
                        mm(psS[:, g, :], ones_row[0:1, :], bsrow[0:1, g * 128:(g + 1) * 128], False, True, ["ones_row", "bsrow"], ["psS"])
                    s.op("dve", lambda e: e.tensor_tensor(out=yabT[:, 0:4, :], in0=psS[:], in1=uTs[:], op=ALU.mult),
                         reads=["psS", "uTs"], writes=["yabT"])
                    for g in range(4):
                        gi = (4 + g) if i == 0 else g
                        mm(psQ[:, g, :], pc_[:, g * 128:(g + 1) * 128], poolm[:, gi, :], True, i == 0, [pck, "poolm"], ["psQ"])
                        if i > 0:
                            mm(psQ[:, g, :], pp_[:, g * 128:(g + 1) * 128], poolm[:, 8 + g, :], False, True, [ppk, "poolm"], ["psQ"])
                    s.op("act", lambda e: e.copy(out=pooledT[:], in_=psQ[:]), reads=["psQ"], writes=["pooledT"])
                    for g in range(4):
                        mm(psU[:, g, :], wpool[:, g, :], pooledT[:, g, :], True, True, ["wpool", "pooledT"], ["psU"])
                    for g in range(4):
                        s.op("dve", lambda e, g=g: e.tensor_scalar(out=yabT[:, 4 + g, :], in0=psU[:, g, :], scalar1=lscol[:, g:g + 1],
                                                                   scalar2=None, op0=ALU.mult), reads=["psU", "lscol"], writes=["yabT"])
                    for half in range(2):
                        for fc in range(8):
                            mm(pso[half][:, :], yabT[:, fc, :], Wout[:, fc, half * 512:(half + 1) * 512], fc == 0, fc == 7,
                               ["yabT", "Wout"], ["pso%d" % half])
                    residual_out(pso, ["pso0", "pso1"], G1, xt, xk, ct["xo"], dst, i)
                s.flush()

        def odd_mixer(l, hsrc, rsrc, dst, hg):
            o_ = l // 2
            lam_init = 0.8 - 0.6 * math.exp(-0.3 * l)
            with contextlib.ExitStack() as ph:
                ct = common_tiles(ph)
                Wq = T("Wq_", [128, 8, 512], BF16, stack=ph)
                Wk = T("Wk_", [128, 8, 512], BF16, stack=ph)
                Wv = T("Wv_", [128, 8, 512], BF16, stack=ph)
                Wout = T("Wout", [128, 4, D], BF16, stack=ph)
                KT = T("KT", [128, 4, S], BF16, stack=ph)
                V = T("V", [128, NT, 4, 130], BF16, stack=ph)
                QT = T("QT", [128, 4, 128], BF16, stack=ph)
                lamt = T("lamt", [128, 4, 64], stack=ph)
                lj = T("lj", [128, 64], stack=ph); ld1 = T("ld1", [128, 2], stack=ph)
                nlam = T("nlam", [128, 1], stack=ph)
                gsub = T("gsub", [128, 128], stack=ph)
                xr = [T("xr%d" % i, [128, D], stack=ph) for i in range(2)] if rsrc is not hsrc else None
                ropeA = T("ropeA", [128, 512], stack=ph); ropeB = T("ropeB", [128, 512], stack=ph)
                qr = T("qr", [128, 512], BF16, stack=ph); kr = T("kr", [128, 512], BF16, stack=ph)
                PT = [T("PT%d" % i, [128, 4, 128], BF16, stack=ph) for i in range(3)]
                mhalf = T("mhalf", [128, 1], stack=ph)
                rz = T("rz", [128, 2], stack=ph); of = T("of", [128, 128], stack=ph); oj = T("oj", [128, 128], stack=ph)
                oss = T("oss", [128, 1], stack=ph)
                Oall = T("Oall", [128, 4, 128], BF16, stack=ph)
                oT = T("oT", [128, 4, 128], BF16, stack=ph)
                tp = PS("tp", [128, 8, 128], BF16, stack=ph)
                psq = PS("psq", [128, 512], stack=ph); psk = PS("psk", [128, 512], stack=ph); psv = PS("psv", [128, 4, 128], stack=ph)
                pss = [PS("pss%d" % i, [128, 4, 128], stack=ph) for i in range(2)]
                pss3 = [pss[0], pss[1], psv]; pss3k = ["pss0", "pss1", "psv"]
                s.op("dve", lambda e: e.memset(mhalf[:], -0.5), writes=["mhalf"])
                pso_ = [PS("psoh%d" % i, [128, 2, 130], stack=ph) for i in range(2)]

                c0 = hg * 512
                wload_bf16(Wq, "Wq_", IN["od_w_in"][o_][:, c0:c0 + 512])
                wload_bf16(Wk, "Wk_", IN["od_w_in"][o_][:, 1024 + c0:1024 + c0 + 512])
                wload_bf16(Wv, "Wv_", IN["od_w_in"][o_][:, 2048 + c0:2048 + c0 + 512])
                wload_bf16(Wout, "Wout", IN["od_w_out"][o_][hg * 512:(hg + 1) * 512, :], nk=4, parts=2)
                for j, nm in enumerate(("od_lam_q1", "od_lam_k1", "od_lam_q2", "od_lam_k2")):
                    ld(lamt[:, j, :], IN[nm][o_, :].partition_broadcast(128), ["lamt"])
                ld(gsub[:], IN["od_g_sub"][o_, :].partition_broadcast(128), ["gsub"])
                s.op("dve", lambda e: e.tensor_scalar(out=gsub[:], in0=gsub[:], scalar1=1.0 - lam_init, scalar2=None, op0=ALU.mult),
                     reads=["gsub"], writes=["gsub"])
                for j in range(2):
                    s.op("dve", lambda e, j=j: e.scalar_tensor_tensor(out=lj[:], in0=lamt[:, 2 * j, :], scalar=1.0, in1=lamt[:, 2 * j + 1, :],
                                                                      op0=ALU.mult, op1=ALU.mult, accum_out=ld1[:, j:j + 1]),
                         reads=["lamt"], writes=["lj", "ld1"])
                s.op("act", lambda e: e.activation(out=ld1[:], in_=ld1[:], func=AF.Exp), reads=["ld1"], writes=["ld1"])
                s.op("dve", lambda e: e.tensor_tensor(out=nlam[:], in0=ld1[:, 1:2], in1=ld1[:, 0:1], op=ALU.subtract), reads=["ld1"], writes=["nlam"])
                s.op("dve", lambda e: e.tensor_scalar(out=nlam[:], in0=nlam[:], scalar1=-lam_init, scalar2=None, op0=ALU.add), reads=["nlam"], writes=["nlam"])
                s.op("dve", lambda e: e.memset(V[:, :, :, 128:130], 1.0), writes=["V"])

                def rope(ps, pskey, out, outkey, i):
                    v4 = lambda ap: ap.rearrange("p (g two i) -> p g two i", g=8, two=2)
                    cb = cosT[:, i, :].unsqueeze(1).unsqueeze(1).to_broadcast([128, 8, 2, 32])
                    sb = sinT[:, i, :].unsqueeze(1).to_broadcast([128, 8, 32])
                    nsb = nsinT[:, i, :].unsqueeze(1).to_broadcast([128, 8, 32])
                    s.op("dve", lambda e: e.tensor_tensor(out=v4(ropeA[:]), in0=v4(ps[:, :]), in1=cb, op=ALU.mult),
                         reads=[pskey, "cosT"], writes=["ropeA"])
                    s.op("dve", lambda e: e.tensor_tensor(out=v4(ropeB[:])[:, :, 0, :], in0=v4(ps[:, :])[:, :, 1, :], in1=nsb, op=ALU.mult),
                         reads=[pskey, "nsinT"], writes=["ropeB"])
                    s.op("dve", lambda e: e.tensor_tensor(out=v4(ropeB[:])[:, :, 1, :], in0=v4(ps[:, :])[:, :, 0, :], in1=sb, op=ALU.mult),
                         reads=[pskey, "sinT"], writes=["ropeB"])
                    s.op("pool", lambda e: e.tensor_tensor(out=out[:], in0=ropeA[:], in1=ropeB[:], op=ALU.add),
                         reads=["ropeA", "ropeB"], writes=[outkey])

                load_x(ct["xt"][0], "xt0", hsrc, 0)
                if xr is not None:
                    load_x(xr[0], "xr0", rsrc, 0)
                for i in range(NT):
                    xt = ct["xt"][i % 2]; xk = "xt%d" % (i % 2)
                    if i + 1 < NT:
                        load_x(ct["xt"][(i + 1) % 2], "xt%d" % ((i + 1) % 2), hsrc, i + 1)
                        if xr is not None:
                            load_x(xr[(i + 1) % 2], "xr%d" % ((i + 1) % 2), rsrc, i + 1)
                    norm_mod(ct, xt, xk, A1, B1)
                    hb, hT = ct["hb"], ct["hT"]
                    transpose8(hb, "hb", tp, hT)
                    for k in range(8):
                        mm(psq[:, :], hT[:, k, :], Wq[:, k, :], k == 0, k == 7, ["Wq_", "hT"], ["psq"])
                    for k in range(8):
                        mm(psk[:, :], hT[:, k, :], Wk[:, k, :], k == 0, k == 7, ["Wk_", "hT"], ["psk"])
                    for k in range(8):
                        mm(psv[:].rearrange("p a b -> p (a b)"), hT[:, k, :], Wv[:, k, :], k == 0, k == 7, ["Wv_", "hT"], ["psv"])
                    rope(psq, "psq", qr, "qr", i)
                    rope(psk, "psk", kr, "kr", i)
                    s.op("act", lambda e, i=i: e.copy(out=V[:, i, :, 0:128], in_=psv[:]),
                         reads=["psv"], writes=["V"])
                    for hh in range(4):
                        tr(tp[:, hh, :], qr[:, hh * 128:(hh + 1) * 128], ident_b[:], ["qr", "ident_b"], ["tp"])
                    for hh in range(4):
                        tr(tp[:, 4 + hh, :], kr[:, hh * 128:(hh + 1) * 128], ident_b[:], ["kr", "ident_b"], ["tp"])
                    s.op("act", lambda e: e.copy(out=QT[:], in_=tp[:, 0:4, :]), reads=["tp"], writes=["QT"])
                    s.op("act", lambda e, i=i: e.copy(out=KT[:, :, i * 128:(i + 1) * 128], in_=tp[:, 4:8, :]), reads=["tp"], writes=["KT"])
                    items = []
                    for hh in range(4):
                        for m in range(2):
                            for g0 in range(0, i + 1, 4):
                                items.append((hh, m, g0, list(range(g0, min(g0 + 4, i + 1)))))

                    def emitS(it, sl_):
                        hh, m, g0, kbs = it
                        rows = slice(m * 64, (m + 1) * 64)
                        bank = pss3[sl_]; bk = pss3k[sl_]
                        pt = PT[sl_]; ptk = "PT%d" % sl_
                        for j, kb in enumerate(kbs):
                            mm(bank[:, j, :], KT[rows, hh, kb * 128:(kb + 1) * 128], QT[rows, hh, :], True, True, ["KT", "QT"], [bk])
                        n = len(kbs)
                        s.op("act", lambda e, pt=pt, bank=bank, n=n: e.activation(out=pt[:, 0:n, :], in_=bank[:, 0:n, :], func=AF.Exp, scale=0.125),
                             reads=[bk], writes=[ptk])
                        if i in kbs:
                            jd = i - g0
                            s.op("pool", lambda e, pt=pt, jd=jd: e.tensor_tensor(out=pt[:, jd, :], in0=pt[:, jd, :], in1=triu_b[:], op=ALU.mult),
                                 reads=[ptk, "triu_b"], writes=[ptk])

                    def emitAV(it, sl_):
                        hh, m, g0, kbs = it
                        po = pso_[hh % 2]; pok = "psoh%d" % (hh % 2)
                        pt = PT[sl_]; ptk = "PT%d" % sl_
                        for j, kb in enumerate(kbs):
                            mm(po[:, m, 0:129], pt[:, j, :], V[:, kb, hh, 0:129], kb == 0, kb == i, [ptk, "V"], [pok])
                        if m == 1 and kbs[-1] == i:
                            s.op("dve", lambda e, po=po: e.reciprocal(out=rz[:], in_=po[:, :, 128]), reads=[pok], writes=["rz"])
                            s.op("dve", lambda e: e.tensor_tensor(out=rz[:, 1:2], in0=rz[:, 1:2], in1=nlam[:], op=ALU.mult), reads=["rz", "nlam"], writes=["rz"])
                            s.op("dve", lambda e, po=po: e.tensor_scalar(out=of[:], in0=po[:, 0, 0:128], scalar1=rz[:, 0:1], scalar2=None, op0=ALU.mult),
                                 reads=[pok, "rz"], writes=["of"])
                            s.op("dve", lambda e, po=po: e.scalar_tensor_tensor(out=of[:], in0=po[:, 1, 0:128], scalar=rz[:, 1:2], in1=of[:], op0=ALU.mult, op1=ALU.add),
                                 reads=[pok, "rz", "of"], writes=["of"])
                            s.op("dve", lambda e: e.scalar_tensor_tensor(out=oj[:], in0=of[:], scalar=1.0, in1=of[:], op0=ALU.mult, op1=ALU.mult, accum_out=oss[:]),
                                 reads=["of"], writes=["oj", "oss"])
                            s.op("dve", lambda e: e.tensor_scalar(out=oss[:], in0=oss[:], scalar1=1.0 / 128, scalar2=EPS, op0=ALU.mult, op1=ALU.add),
                                 reads=["oss"], writes=["oss"])
                            s.op("pool", lambda e: e.tensor_tensor(out=oss[:], in0=oss[:], in1=mhalf[:], op=ALU.pow), reads=["oss", "mhalf"], writes=["oss"])
                            s.op("dve", lambda e, hh=hh: e.scalar_tensor_tensor(out=Oall[:, hh, :], in0=of[:], scalar=oss[:, 0:1], in1=gsub[:], op0=ALU.mult, op1=ALU.mult),
                                 reads=["of", "oss", "gsub"], writes=["Oall"])

                    prev = None
                    for n_, it in enumerate(items):
                        emitS(it, n_ % 3)
                        if prev is not None:
                            emitAV(*prev)
                        prev = (it, n_ % 3)
                    emitAV(*prev)
                    for hh in range(4):
                        tr(tp[:, hh, :], Oall[:, hh, :], ident_b[:], ["Oall", "ident_b"], ["tp"])
                    s.op("act", lambda e: e.copy(out=oT[:], in_=tp[:, 0:4, :]), reads=["tp"], writes=["oT"])
                    pso = [psq, psk]
                    for half in range(2):
                        for hh in range(4):
                            mm(pso[half][:, :], oT[:, hh, :], Wout[:, hh, half * 512:(half + 1) * 512], hh == 0, hh == 3,
                               ["oT", "Wout"], [["psq", "psk"][half]])
                    if xr is not None:
                        residual_out(pso, ["psq", "psk"], G1, xr[i % 2], "xr%d" % (i % 2), ct["xo"], dst, i)
                    else:
                        residual_out(pso, ["psq", "psk"], G1, xt, xk, ct["xo"], dst, i)
                s.flush()

        def peer_pass(l, src, dst, fin):
            with contextlib.ExitStack() as ph:
                junk = T("junk", [128, D], BF16, stack=ph)
                ss = T("ss", [128, 1], stack=ph); rs = T("rs", [128, 1], stack=ph)
                hf = T("hf", [128, D], stack=ph); hb = T("hb", [128, D], BF16, stack=ph)
                hT = T("hT", [128, 8, 128], BF16, stack=ph)
                xo = T("xo", [128, D], stack=ph)
                xts = [T("xt%d" % i, [128, D], stack=ph) for i in range(2)]
                Wq = T("Wqp", [128, 8, 2048], BF16, stack=ph)
                skT = T("skT", [128, 16, 128], stack=ph)
                qT = T("qT", [128, 16, 128], stack=ph)
                sc = T("sc", [128, 16, 128], stack=ph)
                sc2 = T("sc2", [128, 16, 128], stack=ph)
                skraw = sc2
                sv = T("sv", [128, 16, 16], stack=ph)
                si = T("si", [128, 16, 16], U32, stack=ph)
                sif = T("sif", [128, 16, 16], stack=ph)
                cand = sc[:].rearrange("p (h a) n -> p h (a n)", h=8)
                cand2 = sc2[:].rearrange("p (h a) n -> p h (a n)", h=8)
                fv = T("fv", [128, 8, 16], stack=ph)
                fpos = T("fpos", [128, 8, 16], U32, stack=ph)
                fa = T("fa", [128, 8, 16], U32, stack=ph); fb = T("fb", [128, 8, 16], U32, stack=ph)
                faf = T("faf", [128, 8, 16], stack=ph); fbf = T("fbf", [128, 8, 16], stack=ph)
                oh = sc2[:].rearrange("p (h a) (b c) -> p h (a b) c", h=8, c=16)
                Ii = T("Ii", [128, 8, 16], stack=ph); Jj = T("Jj", [128, 8, 16], stack=ph)
                eif = T("eif", [128, 128], stack=ph)
                eidxs = [T("eidx%d" % i, [128, 128], U32, stack=ph) for i in range(2)]
                ge = T("ge", [128, 8, 16], stack=ph); gz = T("gz", [128, 8], stack=ph)
                gates = [T("gate%d" % i, [128, 128], stack=ph) for i in range(2)]
                act = T("act", [128, 128], stack=ph)
                wgt = T("wgt", [128, 128], stack=ph)
                NB = 16
                UV = [T("UV%d" % i, [128, 2 * D], BF16, stack=ph) for i in range(NB)]
                diag = [T("diag%d" % i, [128, 128], BF16, stack=ph) for i in range(2)]
                dj = T("dj", [128, D], BF16, stack=ph)
                fin_t = None
                if fin:
                    fnbc = T("fnbc", [128, D], stack=ph)
                    fss = T("fss", [128, 1], stack=ph); frs = T("frs", [128, 1], stack=ph)
                    ld(fnbc[:], IN["final_norm"][0, :].partition_broadcast(128), ["fnbc"])
                    fin_t = (dj, fss, frs, fnbc, xo)
                tp = PS("tp", [128, 8, 128], BF16, stack=ph)
                psx = [PS("psx%d" % i, [128, 4, 128], stack=ph) for i in range(2)]
                pso = [PS("psop%d" % i, [128, 512], stack=ph) for i in range(2)]
                hps = [PS("hps%d" % i, [128, D], BF16, stack=ph) for i in range(2)]

                wload_bf16(Wq, "Wqp", IN["peer_w_q"][l])
                ld(skraw[:], IN["peer_sub_keys"][l].rearrange("g n k -> n g k"), ["sc2_%d" % g for g in range(16)])
                for r in range(4):
                    for c_ in range(4):
                        tr(psx[r % 2][:, c_, :], skraw[:, r * 4 + c_, :], ident_f[:], ["sc2_%d" % (r * 4 + c_), "ident_f"], ["psx%d" % (r % 2)])
                    S_().op("act", lambda e, r=r: e.copy(out=skT[:, r * 4:(r + 1) * 4, :], in_=psx[r % 2][:]), reads=["psx%d" % (r % 2)], writes=["skT"])

                def stageA(i):
                    xt = xts[i % 2]; xk = "xt%d" % (i % 2)
                    eidx = eidxs[i % 2]; ek = "eidx%d" % (i % 2)
                    gate = gates[i % 2]; gk = "gate%d" % (i % 2)
                    hp_ = hps[i % 2]; hpk = "hps%d" % (i % 2)
                    o = S_()
                    load_x(xt, xk, src, i)
                    o.op("act", lambda e: e.activation(out=junk[:], in_=xt[:], func=AF.Square, accum_out=ss[:]), reads=[xk], writes=["junk", "ss"])
                    o.op("act", lambda e: e.activation(out=rs[:], in_=ss[:], func=AF.Sqrt, scale=1.0 / D, bias=EPS), reads=["ss"], writes=["rs"])
                    o.op("dve", lambda e: e.reciprocal(out=rs[:], in_=rs[:]), reads=["rs"], writes=["rs"])
                    o.op("dve", lambda e: e.scalar_tensor_tensor(out=hf[:], in0=xt[:], scalar=rs[:, 0:1], in1=A2[:], op0=ALU.mult, op1=ALU.mult),
                         reads=[xk, "rs", A2.name], writes=["hf"])
                    o.op("pool", lambda e: e.tensor_tensor(out=hb[:], in0=hf[:], in1=B2[:], op=ALU.add), reads=["hf", B2.name], writes=["hb"])
                    for k in range(8):
                        tr(tp[:, k, :], hb[:, k * 128:(k + 1) * 128], ident_b[:], ["hb", "ident_b"], ["tp"])
                    o.op("act", lambda e: e.copy(out=hT[:], in_=tp[:]), reads=["tp"], writes=["hT"])
                    for k in range(8):
                        tr(hp_[:, k * 128:(k + 1) * 128], hT[:, k, :], ident_b[:], ["hT", "ident_b"], [hpk])
                    for r in range(4):
                        pq = psx[r % 2]; pqk = "psx%d" % (r % 2)
                        for c_ in range(4):
                            hp = r * 4 + c_
                            for k in range(8):
                                mm(pq[:, c_, :], Wq[:, k, hp * 128:(hp + 1) * 128], hT[:, k, :], k == 0, k == 7, ["Wqp", "hT"], [pqk])
                        o.op("act", lambda e, r=r, pq=pq: e.copy(out=qT[:, r * 4:(r + 1) * 4, :], in_=pq[:]), reads=[pqk], writes=["qT"])
                    for r in range(4):
                        pz = psx[r % 2]; pzk = "psx%d" % (r % 2)
                        for c_ in range(4):
                            hp = r * 4 + c_
                            mm(pz[:, c_, :], qT[:, hp, :], skT[:, hp, :], True, True, ["qT", "skT"], [pzk])
                        o.op("act", lambda e, r=r, pz=pz: e.copy(out=sc[:, r * 4:(r + 1) * 4, :], in_=pz[:]), reads=[pzk],
                             writes=["sc_%d" % g for g in range(r * 4, r * 4 + 4)])
                    G16 = range(16)
                    SCK = ["sc_%d" % g for g in G16]; SC2K = ["sc2_%d" % g for g in G16]
                    SVA = ["sva%d" % g for g in G16]; SVB = ["svb%d" % g for g in G16]
                    SIA = ["sia%d" % g for g in G16]; SIB = ["sib%d" % g for g in G16]
                    for g in G16:
                        o.op("dve", lambda e, g=g: e.max(out=sv[:, g, 0:8], in_=sc[:, g, :]), reads=[SCK[g]], writes=[SVA[g]])
                    for g in G16:
                        o.op("dve", lambda e, g=g: e.max_index(out=si[:, g, 0:8], in_max=sv[:, g, 0:8], in_values=sc[:, g, :]), reads=[SCK[g], SVA[g]], writes=[SIA[g]])
                    for g in G16:
                        o.op("dve", lambda e, g=g: e.match_replace(out=sc2[:, g, :], in_to_replace=sv[:, g, 0:8], in_values=sc[:, g, :], imm_value=-1e30),
                             reads=[SCK[g], SVA[g]], writes=[SC2K[g]])
                    for g in G16:
                        o.op("dve", lambda e, g=g: e.max(out=sv[:, g, 8:16], in_=sc2[:, g, :]), reads=[SC2K[g]], writes=[SVB[g]])
                    for g in G16:
                        o.op("dve", lambda e, g=g: e.max_index(out=si[:, g, 8:16], in_max=sv[:, g, 8:16], in_values=sc2[:, g, :]), reads=[SC2K[g], SVB[g]], writes=[SIB[g]])
                    o.op("dve", lambda e: e.tensor_copy(out=sif[:], in_=si[:]), reads=SIA + SIB, writes=["sif"])
                    sv4 = sv[:].rearrange("p (h two) k -> p h two k", two=2)
                    sif4 = sif[:].rearrange("p (h two) k -> p h two k", two=2)
                    o.op("dve", lambda e: e.tensor_tensor(out=cand.rearrange("p h (a b) -> p h a b", a=16),
                                                          in0=sv4[:, :, 0, :].unsqueeze(3).to_broadcast([128, 8, 16, 16]),
                                                          in1=sv4[:, :, 1, :].unsqueeze(2).to_broadcast([128, 8, 16, 16]), op=ALU.add),
                         reads=SVA + SVB, writes=SCK)
                    H8 = range(8)
                    CK = [[SCK[2 * h], SCK[2 * h + 1]] for h in H8]; C2K = [[SC2K[2 * h], SC2K[2 * h + 1]] for h in H8]
                    FVA = ["fva%d" % h for h in H8]; FVB = ["fvb%d" % h for h in H8]
                    FPA = ["fpa%d" % h for h in H8]; FPB = ["fpb%d" % h for h in H8]
                    for h in H8:
                        o.op("dve", lambda e, h=h: e.max(out=fv[:, h, 0:8], in_=cand[:, h, :]), reads=CK[h], writes=[FVA[h]])
                    for h in H8:
                        o.op("dve", lambda e, h=h: e.max_index(out=fpos[:, h, 0:8], in_max=fv[:, h, 0:8], in_values=cand[:, h, :]), reads=CK[h] + [FVA[h]], writes=[FPA[h]])
                    for h in H8:
                        o.op("dve", lambda e, h=h: e.match_replace(out=cand2[:, h, :], in_to_replace=fv[:, h, 0:8], in_values=cand[:, h, :], imm_value=-1e30),
                             reads=CK[h] + [FVA[h]], writes=C2K[h])
                    for h in H8:
                        o.op("dve", lambda e, h=h: e.max(out=fv[:, h, 8:16], in_=cand2[:, h, :]), reads=C2K[h], writes=[FVB[h]])
                    for h in H8:
                        o.op("dve", lambda e, h=h: e.max_index(out=fpos[:, h, 8:16], in_max=fv[:, h, 8:16], in_values=cand2[:, h, :]), reads=C2K[h] + [FVB[h]], writes=[FPB[h]])
                    FV = FVA + FVB; FP = FPA + FPB
                    o.op("dve", lambda e: e.tensor_tensor(out=ge[:], in0=fv[:], in1=fv[:, :, 0:1].to_broadcast([128, 8, 16]), op=ALU.subtract),
                         reads=FV, writes=["ge"])
                    o.op("act", lambda e: e.activation(out=ge[:], in_=ge[:], func=AF.Exp), reads=["ge"], writes=["ge"])
                    o.op("dve", lambda e: e.tensor_reduce(out=gz[:], in_=ge[:], axis=AX.X, op=ALU.add), reads=["ge"], writes=["gz"])
                    o.op("dve", lambda e: e.reciprocal(out=gz[:], in_=gz[:]), reads=["gz"], writes=["gz"])
                    o.op("dve", lambda e: e.tensor_tensor(out=gate[:].rearrange("p (h k) -> p h k", h=8), in0=ge[:],
                                                          in1=gz[:].unsqueeze(2).to_broadcast([128, 8, 16]), op=ALU.mult),
                         reads=["ge", "gz"], writes=[gk])
                    o.op("dve", lambda e: e.tensor_single_scalar(out=fa[:], in_=fpos[:], scalar=4, op=ALU.logical_shift_right), reads=FP, writes=["fa"])
                    o.op("dve", lambda e: e.tensor_single_scalar(out=fb[:], in_=fpos[:], scalar=15, op=ALU.bitwise_and), reads=FP, writes=["fb"])
                    o.op("dve", lambda e: e.tensor_copy(out=faf[:], in_=fa[:]), reads=["fa"], writes=["faf"])
                    o.op("dve", lambda e: e.tensor_copy(out=fbf[:], in_=fb[:]), reads=["fb"], writes=["fbf"])
                    io4 = iota16[:].unsqueeze(1).unsqueeze(1).to_broadcast([128, 8, 16, 16])
                    for (srcf, sk_, half, dstt, dk) in ((faf, "faf", 0, Ii, "Ii"), (fbf, "fbf", 1, Jj, "Jj")):
                        o.op("dve", lambda e, srcf=srcf: e.tensor_tensor(out=oh, in0=srcf[:].unsqueeze(3).to_broadcast([128, 8, 16, 16]), in1=io4, op=ALU.is_equal),
                             reads=[sk_, "iota16"], writes=SC2K)
                        o.op("dve", lambda e, half=half: e.tensor_tensor(out=oh, in0=oh, in1=sif4[:, :, half, :].unsqueeze(2).to_broadcast([128, 8, 16, 16]), op=ALU.mult),
                             reads=SC2K + ["sif"], writes=SC2K)
                        o.op("dve", lambda e, dstt=dstt: e.tensor_reduce(out=dstt[:], in_=oh, axis=AX.X, op=ALU.add), reads=SC2K, writes=[dk])
                    o.op("dve", lambda e: e.scalar_tensor_tensor(out=eif[:].rearrange("p (h k) -> p h k", h=8), in0=Ii[:], scalar=128.0, in1=Jj[:], op0=ALU.mult, op1=ALU.add),
                         reads=["Ii", "Jj"], writes=["eif"])
                    if l > 0:
                        o.op("dve", lambda e: e.tensor_scalar(out=eif[:], in0=eif[:], scalar1=float(l * NEXP), scalar2=None, op0=ALU.add),
                             reads=["eif"], writes=["eif"])
                    o.op("dve", lambda e: e.tensor_copy(out=eidx[:], in_=eif[:]), reads=["eif"], writes=[ek])

                nuv = [0]

                def stageB(i, filler):
                    xt = xts[i % 2]; xk = "xt%d" % (i % 2)
                    eidx = eidxs[i % 2]; ek = "eidx%d" % (i % 2)
                    gate = gates[i % 2]; gk = "gate%d" % (i % 2)
                    hp_ = hps[i % 2]; hpk = "hps%d" % (i % 2)
                    o = S_()
                    LOOK = 8
                    order = list(range(128))
                    bufs = {}

                    def issue(sl):
                        b = nuv[0] % NB; nuv[0] += 1
                        bufs[sl] = b
                        ub = UV[b]
                        o.dma("pool", lambda e, ub=ub, sl=sl: e.indirect_dma_start(
                            out=ub[:], out_offset=None, in_=uvb[:, :],
                            in_offset=bass.IndirectOffsetOnAxis(ap=eidx[:, sl:sl + 1], axis=0)), reads=[ek, ("uvb", l)], writes=["UV%d" % b])

                    for sl in range(min(LOOK, 128)):
                        issue(sl)
                    GS = 4

                    def finish(g0):
                        hs = slice(g0, g0 + GS)
                        wk = "wgt%d" % (g0 // GS)
                        o.op("dve", lambda e, hs=hs: e.tensor_tensor(out=wgt[:, hs], in0=wgt[:, hs], in1=gate[:, hs], op=ALU.mult), reads=[wk, gk], writes=[wk])
                        for sl in range(g0, g0 + GS):
                            b = bufs[sl]; ub = UV[b]; ubk = "UV%d" % b
                            dg = diag[sl % 2]; dgk = "diag%d" % (sl % 2)
                            o.op("act", lambda e, dg=dg, sl=sl: e.activation(out=dg[:], in_=ident_f[:], func=AF.Copy, scale=wgt[:, sl:sl + 1]),
                                 reads=["ident_f", wk], writes=[dgk])
                            for half in range(2):
                                mm(pso[half][:, :], dg[:], ub[:, D + half * 512: D + (half + 1) * 512], sl == 0, sl == 127, [dgk, ubk], ["psop%d" % half])

                    for g0 in range(0, 128, GS):
                        ak = "act%d" % (g0 // GS); wk = "wgt%d" % (g0 // GS)
                        for sl in range(g0, g0 + GS):
                            if sl + LOOK < 128:
                                issue(sl + LOOK)
                            b = bufs[sl]; ub = UV[b]; ubk = "UV%d" % b
                            o.op("dve", lambda e, ub=ub, sl=sl: e.scalar_tensor_tensor(out=dj[:], in0=ub[:, 0:D], scalar=1.0, in1=hp_[:], op0=ALU.mult, op1=ALU.mult,
                                                                                       accum_out=act[:, sl:sl + 1]),
                                 reads=[ubk, hpk], writes=[ak])
                            filler()
                        hs = slice(g0, g0 + GS)
                        o.op("act", lambda e, hs=hs: e.activation(out=wgt[:, hs], in_=act[:, hs], func=AF.Gelu), reads=[ak], writes=[wk])
                        if g0 > 0:
                            finish(g0 - GS)
                    finish(128 - GS)
                    residual_out(pso, ["psop0", "psop1"], G2, xt, xk, xo, dst, i, fin=fin_t)

                stageA(0)
                for i in range(NT):
                    nxt = Deferred()
                    if i + 1 < NT:
                        sink[0] = nxt
                        stageA(i + 1)
                        sink[0] = s
                    per = (len(nxt.q) + 127) // 128
                    stageB(i, lambda: nxt.run(s, per))
                    nxt.run(s, None)
                s.flush()

        cur = IN["x"]
        free = [0, 1, 2]

        def take(exclude):
            for b in free:
                if xs[b] is not exclude and all(xs[b] is not e_ for e_ in exclude if e_ is not None):
                    return xs[b]
            raise RuntimeError("no buffer")

        nl = len(layers)
        for li, l in enumerate(layers):
            compute_mod(l)
            if do_mixer:
                if l % 2 == 0:
                    d1 = [b for b in xs if b is not cur][0]
                    even_mixer(l, cur, d1)
                    cur = d1
                else:
                    others = [b for b in xs if b is not cur]
                    odd_mixer(l, cur, cur, others[0], 0)
                    odd_mixer(l, cur, others[0], others[1], 1)
                    cur = others[1]
            if do_peer:
                last = (li == nl - 1)
                d2 = y_out if last else [b for b in xs if b is not cur][0]
                peer_pass(l, cur, d2, fin=(last and final_norm))
                cur = d2
        if cur is not y_out:
            with contextlib.ExitStack() as ph:
                tt = T("cp", [128, D], stack=ph)
                for i in range(NT):
                    ld(tt[:], cur[i * 128:(i + 1) * 128, :], ["cp"], r=[(cur.tensor.name, i)])
                    ld(y_out[i * 128:(i + 1) * 128, :], tt[:], [("y", i)], r=["cp"])
                s.flush()
        s.wait_all("sp")
        s.flush()
        build.n_instr = s.n_instr
    return nc


def make_in_maps(inputs, n_cores, S=4096):
    consts = host_consts()
    shared = {}
    for k in IN_SHAPES:
        if k in consts:
            shared[k] = consts[k]
        else:
            shared[k] = np.ascontiguousarray(np.asarray(inputs[k], dtype=np.float32)).reshape(IN_SHAPES[k][0])
    maps = []
    x = np.asarray(inputs["x"], dtype=np.float32)
    c = np.asarray(inputs["c"], dtype=np.float32)
    pos = np.asarray(inputs["positions"]).astype(np.int32)
    for b in range(n_cores):
        m = dict(shared)
        m["x"] = np.ascontiguousarray(x[b, :S])
        m["c"] = np.ascontiguousarray(c[b:b + 1])
        m["positions"] = np.ascontiguousarray(pos[b:b + 1, :S])
        maps.append(m)
    return maps


def kernel(**inputs):
    n = 8
    nc = build(4096)
    maps = make_in_maps(inputs, n)
    res = run_bass_kernel_spmd(nc, maps, core_ids=list(range(n)))
    return np.stack([np.asarray(r["y"], dtype=np.float32) for r in res.results], axis=0)
```

```python
import contextlib
import math
import numpy as np
import concourse.bass as bass
import concourse.mybir as mybir
from concourse.bass_utils import run_bass_kernel_spmd

F32 = mybir.dt.float32
BF16 = mybir.dt.bfloat16
U32 = mybir.dt.uint32
I32 = mybir.dt.int32
AF = mybir.ActivationFunctionType
ALU = mybir.AluOpType
AX = mybir.AxisListType

D = 1024
EPS = 1e-6
NEXP = 16384
ENGS = ("pe", "act", "dve", "pool", "sp")
SAME_ENGINE_SYNC = True
NO_SAME_SYNC = set()
FILL_MODE = 0


class Sched:
    def __init__(self, nc, st, n_dma_sems=12):
        self.nc = nc
        self.ops = {e: [] for e in ENGS}
        self.cnt = {e: 0 for e in ENGS}
        self.known = {e: {} for e in ENGS}
        self.lastw = {}
        self.reads = {}
        self.n_dma_sems = n_dma_sems
        self.dma_cnt = {}
        self.dma_rr = {e: 0 for e in ENGS}
        self.semh = {}
        for e in ENGS:
            self.semh[e] = st.enter_context(nc.semaphore("s_" + e))
        for q in ("sp", "pool", "act"):
            for i in range(n_dma_sems):
                self.semh[(q, i)] = st.enter_context(nc.semaphore("d_%s%d" % (q, i)))
        self.n_instr = 0

    def _deps(self, eng, reads, writes, skip_same):
        waits = {}

        def need(ev):
            if ev is None:
                return
            k, v = ev
            if k == eng and (skip_same or not SAME_ENGINE_SYNC or eng in NO_SAME_SYNC):
                return
            if self.known[eng].get(k, 0) >= v:
                return
            if waits.get(k, 0) < v:
                waits[k] = v

        for b in reads:
            need(self.lastw.get(b))
        for b in writes:
            need(self.lastw.get(b))
            for ev in self.reads.get(b, ()):
                need(ev)
        for k, v in waits.items():
            self.known[eng][k] = v
        return waits

    def _record(self, ev, reads, writes):
        for b in reads:
            lst = self.reads.setdefault(b, [])
            lst[:] = [x for x in lst if x[0] != ev[0]]
            lst.append(ev)
        for b in writes:
            self.lastw[b] = ev
            self.reads[b] = []

    def op(self, eng, fn, reads=(), writes=(), skip_same=False, nostop=False):
        waits = self._deps(eng, reads, writes, skip_same)
        self.cnt[eng] += 1
        ev = (eng, self.cnt[eng])
        self.ops[eng].append(("op", fn, waits, ev))
        self._record(ev, reads, writes)
        return ev

    def dma(self, queue, fn, reads=(), writes=()):
        i = self.dma_rr[queue]
        self.dma_rr[queue] = (i + 1) % self.n_dma_sems
        key = (queue, i)
        prev = self.dma_cnt.get(key, 0)
        waits = self._deps(queue, reads, writes, False)
        if prev > 0 and self.known[queue].get(key, 0) < prev * 16:
            waits[key] = prev * 16
            self.known[queue][key] = prev * 16
        self.dma_cnt[key] = prev + 1
        ev = (key, (prev + 1) * 16)
        self.ops[queue].append(("dma", fn, waits, ev))
        self._record(ev, reads, writes)
        return ev

    def wait_all(self, eng):
        waits = {}
        for e in ENGS:
            if e != eng and self.cnt[e] > 0:
                waits[e] = self.cnt[e]
        for key, c in self.dma_cnt.items():
            waits[key] = c * 16
        self.ops[eng].append(("wait", None, waits, None))

    def flush(self):
        nc = self.nc
        if not any(self.ops[e] for e in ENGS):
            return
        with nc.Block() as block:
            regs = {"pe": block.tensor, "act": block.scalar, "dve": block.vector,
                    "pool": block.gpsimd, "sp": block.sync}

            def make(e, lst):
                def body(engobj):
                    for kind, fn, waits, ev in lst:
                        for k, v in waits.items():
                            engobj.wait_ge(self.semh[k], v)
                        if kind == "wait":
                            continue
                        ins = fn(engobj)
                        if kind == "dma":
                            ins.then_inc(self.semh[ev[0]], 16)
                        else:
                            ins.then_inc(self.semh[e], 1)
                return body

            for e in ENGS:
                lst = self.ops[e]
                self.n_instr += len(lst)
                if lst:
                    regs[e](make(e, lst))
        self.ops = {e: [] for e in ENGS}


class Deferred:
    def __init__(self):
        self.q = []

    def op(self, *a, **k):
        self.q.append(("op", a, k))

    def dma(self, *a, **k):
        self.q.append(("dma", a, k))

    def run(self, s, n):
        m = len(self.q) if n is None else min(n, len(self.q))
        while 0 < m < len(self.q) and self.q[m - 1][2].get("nostop"):
            m += 1
        for kind, a, k in self.q[:m]:
            getattr(s, kind)(*a, **k)
        del self.q[:m]


def host_consts():
    ident = np.eye(128, dtype=np.float32)
    p = np.arange(128)[:, None]
    j = np.arange(128)[None, :]
    triu = (j >= p).astype(np.float32)
    poolm = np.zeros((12, 128, 128), np.float32)
    for g, w in enumerate((2, 4, 8, 16)):
        band = ((j - p >= 0) & (j - p <= w - 1)).astype(np.float32)
        poolm[g] = band / w - ident
        cnt = np.minimum(np.arange(128) + 1, w).astype(np.float32)[None, :]
        poolm[4 + g] = band / cnt - ident
        poolm[8 + g] = ((j - p + 128) <= (w - 1)).astype(np.float32) / w
    iota16 = np.tile(np.arange(16, dtype=np.float32)[None, :], (128, 1))
    inv_freq = 1.0 / (10000.0 ** (np.arange(0, 64, 2, dtype=np.float32) / 64.0))
    invf = np.tile((inv_freq / (2.0 * np.pi)).astype(np.float32)[None, :], (128, 1))
    return {"k_ident": ident, "k_triu": triu, "k_poolm": poolm.transpose(1, 0, 2).copy(),
            "k_iota16": iota16, "k_invf": invf}


IN_SHAPES = {
    "ada_w": ([4, 1024, 6144], F32), "ada_b": ([4, 6144], F32),
    "norm_mix": ([4, 1024], F32), "norm_ffn": ([4, 1024], F32),
    "ev_w_in": ([2, 1024, 1536], F32), "ev_g_v": ([2, 512], F32),
    "ev_w_s": ([2, 4, 128, 128], F32), "ev_b_s": ([2, 512], F32),
    "ev_w_pool": ([2, 4, 128, 128], F32), "ev_pool_scale": ([2, 512], F32),
    "ev_w_out": ([2, 1024, 1024], F32), "od_w_in": ([2, 1024, 3072], F32),
    "od_lam_q1": ([2, 64], F32), "od_lam_k1": ([2, 64], F32),
    "od_lam_q2": ([2, 64], F32), "od_lam_k2": ([2, 64], F32),
    "od_g_sub": ([2, 128], F32), "od_w_out": ([2, 1024, 1024], F32),
    "peer_w_q": ([4, 1024, 2048], F32), "peer_sub_keys": ([4, 16, 128, 128], F32),
    "peer_u": ([4, NEXP, 1024], F32), "peer_v": ([4, NEXP, 1024], F32),
    "final_norm": ([1, 1024], F32),
    "k_ident": ([128, 128], F32), "k_triu": ([128, 128], F32), "k_poolm": ([128, 12, 128], F32),
    "k_iota16": ([128, 16], F32), "k_invf": ([128, 32], F32),
}


def build(S=4096, layers=(0, 1, 2, 3), dbg=False, do_mixer=True, do_peer=True, final_norm=True):
    NT = S // 128
    nc = bass.Bass("TRN2", target_bir_lowering=False)
    IN = {}
    IN["x"] = nc.dram_tensor("x", [S, D], F32, kind="ExternalInput").ap()
    IN["c"] = nc.dram_tensor("c", [1, D], F32, kind="ExternalInput").ap()
    IN["positions"] = nc.dram_tensor("positions", [1, S], I32, kind="ExternalInput").ap()
    for k, (shp, dt) in IN_SHAPES.items():
        IN[k] = nc.dram_tensor(k, shp, dt, kind="ExternalInput").ap()
    y_out = nc.dram_tensor("y", [S, D], F32, kind="ExternalOutput").ap()
    xs = [nc.dram_tensor("xs%d" % i, [S, D], F32,
                         kind=("ExternalOutput" if dbg else "Internal")).ap() for i in range(3)]

    uvb = nc.dram_tensor("uvb", [4 * NEXP, 2 * D], BF16, kind="Internal").ap()

    with contextlib.ExitStack() as st:
        s = Sched(nc, st)
        for h in s.semh.values():
            nc.gpsimd.sem_clear(h)
        nc.all_engine_barrier()

        uid = [0]

        def T(name, shape, dt=F32, stack=st):
            uid[0] += 1
            return stack.enter_context(nc.sbuf_tensor("%s_%d" % (name, uid[0]), shape, dt))

        def PS(name, shape, dt=F32, stack=st):
            uid[0] += 1
            return stack.enter_context(nc.psum_tensor("%s_%d" % (name, uid[0]), shape, dt))

        sink = [s]

        def S_():
            return sink[0]

        def mm(out, lhsT, rhs, start, stop, r, w):
            sink[0].op("pe", lambda e: e.matmul(out, lhsT=lhsT, rhs=rhs, start=start, stop=stop),
                 reads=r, writes=w, skip_same=True, nostop=(not stop))

        def tr(out, in_, ident, r, w):
            sink[0].op("pe", lambda e: e.transpose(out=out, in_=in_, identity=ident),
                 reads=r, writes=w, skip_same=True)

        def ld(out, in_, w, r=(), q="sp", slow=False):
            if slow:
                sink[0].dma(q, lambda e: e.dma_start(out=out, in_=in_, allow_slow_non_contiguous=True), reads=r, writes=w)
            else:
                sink[0].dma(q, lambda e: e.dma_start(out=out, in_=in_), reads=r, writes=w)

        ident_f = T("ident_f", [128, 128]); ident_b = T("ident_b", [128, 128], BF16)
        triu_b = T("triu_b", [128, 128], BF16); triu_f = T("triu_f", [128, 128])
        iota16 = T("iota16", [128, 16])
        ones_row = T("ones_row", [1, 128])
        modbc = [T("modbc%d" % i, [128, D]) for i in range(6)]
        A1, B1, G1, A2, B2, G2 = modbc
        cosT = T("cosT", [128, NT, 32]); sinT = T("sinT", [128, NT, 32]); nsinT = T("nsinT", [128, NT, 32])
        c_act = T("c_act", [128, 8])

        ld(ident_f[:], IN["k_ident"][:, :], ["ident_f"])
        ld(triu_f[:], IN["k_triu"][:, :], ["triu_f"])
        ld(iota16[:], IN["k_iota16"][:, :], ["iota16"])
        S_().op("dve", lambda e: e.tensor_copy(out=ident_b[:], in_=ident_f[:]), reads=["ident_f"], writes=["ident_b"])
        S_().op("dve", lambda e: e.tensor_copy(out=triu_b[:], in_=triu_f[:]), reads=["triu_f"], writes=["triu_b"])
        S_().op("dve", lambda e: e.memset(ones_row[:], 1.0), writes=["ones_row"])

        if do_peer:
            for l_ in layers:
                for c_ in range(16):
                    r0 = l_ * NEXP + c_ * 1024
                    S_().dma("pool", lambda e, l_=l_, c_=c_, r0=r0: e.dma_start(out=uvb[r0:r0 + 1024, 0:D], in_=IN["peer_u"][l_, c_ * 1024:(c_ + 1) * 1024, :]),
                          writes=[("uvb", l_)])
                    S_().dma("pool", lambda e, l_=l_, c_=c_, r0=r0: e.dma_start(out=uvb[r0:r0 + 1024, D:2 * D], in_=IN["peer_v"][l_, c_ * 1024:(c_ + 1) * 1024, :]),
                          writes=[("uvb", l_)])

        with contextlib.ExitStack() as ph:
            c_row = T("c_row", [1, D], stack=ph)
            one11 = T("one11", [1, 1], stack=ph)
            pc = PS("pc", [128, 8], stack=ph)
            ld(c_row[:], IN["c"][:, :], ["c_row"])
            S_().op("dve", lambda e: e.memset(one11[:], 1.0), writes=["one11"])
            for k in range(8):
                mm(pc[:, k:k + 1], c_row[0:1, k * 128:(k + 1) * 128], one11[0:1, 0:1], True, True,
                   ["c_row", "one11"], ["pc"])
            S_().op("act", lambda e: e.activation(out=c_act[:], in_=pc[:], func=AF.Silu), reads=["pc"], writes=["c_act"])
            pos_i = T("pos_i", [128, NT], I32, stack=ph)
            pos_f = T("pos_f", [128, NT], stack=ph)
            invf = T("invf", [128, 32], stack=ph)
            yy = T("yy", [128, NT, 32], stack=ph); y2 = T("y2", [128, NT, 32], stack=ph)
            ki = T("ki", [128, NT, 32], I32, stack=ph); kf = T("kf", [128, NT, 32], stack=ph)
            ld(pos_i[:], IN["positions"][0, :].rearrange("(n p) -> p n", p=128), ["pos_i"], slow=True)
            ld(invf[:], IN["k_invf"][:, :], ["invf"])
            S_().op("dve", lambda e: e.tensor_copy(out=pos_f[:], in_=pos_i[:]), reads=["pos_i"], writes=["pos_f"])
            S_().op("dve", lambda e: e.tensor_tensor(out=yy[:], in0=pos_f[:].unsqueeze(2).to_broadcast([128, NT, 32]),
                                                  in1=invf[:].unsqueeze(1).to_broadcast([128, NT, 32]), op=ALU.mult),
                 reads=["pos_f", "invf"], writes=["yy"])
            S_().op("dve", lambda e: e.tensor_copy(out=ki[:], in_=yy[:]), reads=["yy"], writes=["ki"])
            S_().op("dve", lambda e: e.tensor_copy(out=kf[:], in_=ki[:]), reads=["ki"], writes=["kf"])
            S_().op("dve", lambda e: e.tensor_tensor(out=y2[:], in0=yy[:], in1=kf[:], op=ALU.subtract), reads=["yy", "kf"], writes=["y2"])
            S_().op("act", lambda e: e.activation(out=sinT[:], in_=y2[:], func=AF.Sin, scale=2.0 * math.pi), reads=["y2"], writes=["sinT"])
            S_().op("dve", lambda e: e.tensor_scalar(out=nsinT[:], in0=sinT[:], scalar1=-1.0, scalar2=None, op0=ALU.mult), reads=["sinT"], writes=["nsinT"])
            S_().op("dve", lambda e: e.tensor_scalar(out=yy[:], in0=yy[:], scalar1=0.25, scalar2=None, op0=ALU.add), reads=["yy"], writes=["yy"])
            S_().op("dve", lambda e: e.tensor_copy(out=ki[:], in_=yy[:]), reads=["yy"], writes=["ki"])
            S_().op("dve", lambda e: e.tensor_copy(out=kf[:], in_=ki[:]), reads=["ki"], writes=["kf"])
            S_().op("dve", lambda e: e.tensor_tensor(out=y2[:], in0=yy[:], in1=kf[:], op=ALU.subtract), reads=["yy", "kf"], writes=["y2"])
            S_().op("act", lambda e: e.activation(out=cosT[:], in_=y2[:], func=AF.Sin, scale=2.0 * math.pi), reads=["y2"], writes=["cosT"])
            s.flush()

        def compute_mod(l):
            with contextlib.ExitStack() as ph:
                wt = [T("adaw%d" % i, [128, 8, 512], stack=ph) for i in range(2)]
                brow = T("adab", [1, 6 * D], stack=ph)
                mrow = T("mrow", [1, 6 * D], stack=ph)
                nbc = [T("nbc%d" % i, [128, D], stack=ph) for i in range(2)]
                pm = [PS("pm%d" % i, [1, 512], stack=ph) for i in range(2)]
                pb = [PS("pb%d" % i, [128, 512], stack=ph) for i in range(2)]
                ld(brow[:], IN["ada_b"][l:l + 1, :], ["adab"])
                ld(nbc[0][:], IN["norm_mix"][l, :].partition_broadcast(128), ["nbc0"])
                ld(nbc[1][:], IN["norm_ffn"][l, :].partition_broadcast(128), ["nbc1"])
                for cb in range(12):
                    w = wt[cb % 2]; wk = "adaw%d" % (cb % 2); pk = "pm%d" % (cb % 2)
                    ld(w[:], IN["ada_w"][l, :, cb * 512:(cb + 1) * 512].rearrange("(k p) f -> p k f", p=128), [wk])
                    for k in range(8):
                        mm(pm[cb % 2][:, :], c_act[:, k:k + 1], w[:, k, :], k == 0, k == 7, ["c_act", wk], [pk])
                    S_().op("dve", lambda e, cb=cb: e.tensor_tensor(out=mrow[0:1, cb * 512:(cb + 1) * 512], in0=pm[cb % 2][:, :],
                                                                 in1=brow[0:1, cb * 512:(cb + 1) * 512], op=ALU.add),
                         reads=[pk, "adab"], writes=["mrow"])
                dst = {0: (B1, None), 1: (A1, 0), 2: (G1, None), 3: (B2, None), 4: (A2, 1), 5: (G2, None)}
                n = 0
                for i in range(6):
                    tgt, nb = dst[i]
                    for half in range(2):
                        pbk = "pb%d" % (n % 2); pbt = pb[n % 2]; n += 1
                        mm(pbt[:, :], ones_row[0:1, :], mrow[0:1, i * D + half * 512: i * D + (half + 1) * 512], True, True,
                           ["ones_row", "mrow"], [pbk])
                        o = tgt[:, half * 512:(half + 1) * 512]
                        if nb is None:
                            S_().op("act", lambda e, o=o, pbt=pbt: e.copy(out=o, in_=pbt[:, :]), reads=[pbk], writes=[tgt.name])
                        else:
                            nbt = nbc[nb][:, half * 512:(half + 1) * 512]
                            S_().op("dve", lambda e, o=o, pbt=pbt, nbt=nbt: e.scalar_tensor_tensor(
                                out=o, in0=pbt[:, :], scalar=1.0, in1=nbt, op0=ALU.add, op1=ALU.mult),
                                reads=[pbk, "nbc%d" % nb], writes=[tgt.name])
                s.flush()

        def load_x(xt, key, src, i):
            ld(xt[:], src[i * 128:(i + 1) * 128, :], [key], r=[(src.tensor.name, i)])

        def norm_mod(ph_t, xt, xkey, A, B, want_f32=False):
            junk, ss, rs, hf, hb = ph_t["junk"], ph_t["ss"], ph_t["rs"], ph_t["hf"], ph_t["hb"]
            S_().op("act", lambda e: e.activation(out=junk[:], in_=xt[:], func=AF.Square, accum_out=ss[:]),
                 reads=[xkey], writes=["junk", "ss"])
            S_().op("act", lambda e: e.activation(out=rs[:], in_=ss[:], func=AF.Sqrt, scale=1.0 / D, bias=EPS),
                 reads=["ss"], writes=["rs"])
            S_().op("dve", lambda e: e.reciprocal(out=rs[:], in_=rs[:]), reads=["rs"], writes=["rs"])
            S_().op("dve", lambda e: e.scalar_tensor_tensor(out=hf[:], in0=xt[:], scalar=rs[:, 0:1], in1=A[:],
                                                         op0=ALU.mult, op1=ALU.mult),
                 reads=[xkey, "rs", A.name], writes=["hf"])
            if want_f32:
                S_().op("pool", lambda e: e.tensor_tensor(out=hf[:], in0=hf[:], in1=B[:], op=ALU.add),
                     reads=["hf", B.name], writes=["hf"])
                S_().op("act", lambda e: e.copy(out=hb[:], in_=hf[:]), reads=["hf"], writes=["hb"])
            else:
                S_().op("pool", lambda e: e.tensor_tensor(out=hb[:], in0=hf[:], in1=B[:], op=ALU.add),
                     reads=["hf", B.name], writes=["hb"])

        def transpose8(hb, hbkey, tp, hT):
            for k in range(8):
                tr(tp[:, k, :], hb[:, k * 128:(k + 1) * 128], ident_b[:], [hbkey, "ident_b"], ["tp"])
            S_().op("act", lambda e: e.copy(out=hT[:], in_=tp[:]), reads=["tp"], writes=["hT"])

        def wload_bf16(dst, key, src, nk=8, parts=4):
            step = nk // parts
            for a in range(parts):
                ld(dst[:, a * step:(a + 1) * step, :],
                   src[a * step * 128:(a + 1) * step * 128, :].rearrange("(k p) f -> p k f", p=128), [key], q="pool")

        def residual_out(pso, psk, Gt, xres, xreskey, xo, dst, i, fin=None):
            for half in range(2):
                sl = slice(half * 512, (half + 1) * 512)
                S_().op("dve", lambda e, half=half, sl=sl: e.tensor_tensor(out=xo[:, sl], in0=pso[half][:, :], in1=Gt[:, sl], op=ALU.mult),
                     reads=[psk[half], Gt.name], writes=["xo"])
            S_().op("pool", lambda e: e.tensor_tensor(out=xo[:], in0=xo[:], in1=xres[:], op=ALU.add),
                 reads=["xo", xreskey], writes=["xo"])
            if fin is None:
                ld(dst[i * 128:(i + 1) * 128, :], xo[:], [(dst.tensor.name, i)], r=["xo"])
            else:
                junk, ss, rs, fnbc, yo = fin
                S_().op("act", lambda e: e.activation(out=junk[:], in_=xo[:], func=AF.Square, accum_out=ss[:]),
                     reads=["xo"], writes=["fss"])
                S_().op("act", lambda e: e.activation(out=rs[:], in_=ss[:], func=AF.Sqrt, scale=1.0 / D, bias=EPS),
                     reads=["fss"], writes=["frs"])
                S_().op("dve", lambda e: e.reciprocal(out=rs[:], in_=rs[:]), reads=["frs"], writes=["frs"])
                S_().op("dve", lambda e: e.scalar_tensor_tensor(out=yo[:], in0=xo[:], scalar=rs[:, 0:1], in1=fnbc[:],
                                                             op0=ALU.mult, op1=ALU.mult),
                     reads=["xo", "frs", "fnbc"], writes=["xo"])
                ld(dst[i * 128:(i + 1) * 128, :], yo[:], [(dst.tensor.name, i)], r=["xo"])

        def common_tiles(ph):
            d = {}
            d["junk"] = T("junk", [128, D], BF16, stack=ph)
            d["ss"] = T("ss", [128, 1], stack=ph)
            d["rs"] = T("rs", [128, 1], stack=ph)
            d["hf"] = T("hf", [128, D], stack=ph)
            d["hb"] = T("hb", [128, D], BF16, stack=ph)
            d["hT"] = T("hT", [128, 8, 128], BF16, stack=ph)
            d["xo"] = T("xo", [128, D], stack=ph)
            d["xt"] = [T("xt%d" % i, [128, D], stack=ph) for i in range(2)]
            return d

        def even_mixer(l, src, dst):
            e_ = l // 2
            with contextlib.ExitStack() as ph:
                ct = common_tiles(ph)
                Win = T("Win", [128, 8, 1536], BF16, stack=ph)
                Wout = T("Wout", [128, 8, D], BF16, stack=ph)
                wsraw = T("wsraw", [128, 4, 128], stack=ph)
                wsT = T("wsT", [128, 4, 128], BF16, stack=ph)
                bsrow = T("bsrow", [1, 512], stack=ph)
                wpool = T("wpool", [128, 4, 128], BF16, stack=ph)
                gvbc = T("gvbc", [128, 512], stack=ph)
                lscol = T("lscol", [128, 4], stack=ph)
                poolm = T("poolm", [128, 12, 128], stack=ph)
                uTs = T("uTs", [128, 4, 128], stack=ph)
                vs = T("vs", [128, 512], stack=ph)
                vn = T("vn", [128, 512], BF16, stack=ph)
                bst = T("bst", [128, 6], stack=ph); bag = T("bag", [128, 2], stack=ph); sd = T("sd", [128, 1], stack=ph)
                pcur = [T("pcur%d" % i, [128, 512], stack=ph) for i in range(2)]
                pooledT = T("pooledT", [128, 4, 128], BF16, stack=ph)
                yabT = T("yabT", [128, 8, 128], BF16, stack=ph)
                tp = PS("tp", [128, 8, 128], BF16, stack=ph)
                psU = PS("psU", [128, 4, 128], stack=ph)
                psV = PS("psV", [128, 512], stack=ph)
                psP = PS("psP", [128, 512], stack=ph)
                psS = PS("psS", [128, 4, 128], stack=ph)
                psQ = PS("psQ", [128, 4, 128], stack=ph)
                pso = [PS("pso%d" % i, [128, 512], stack=ph) for i in range(2)]

                wload_bf16(Win, "Win", IN["ev_w_in"][e_])
                wload_bf16(Wout, "Wout", IN["ev_w_out"][e_])
                ld(wsraw[:], IN["ev_w_s"][e_].rearrange("g t s -> t g s"), ["wsraw"])
                ld(bsrow[:], IN["ev_b_s"][e_:e_ + 1, :], ["bsrow"])
                ld(wpool[:], IN["ev_w_pool"][e_].rearrange("g c d -> c g d"), ["wpool"], q="pool")
                ld(gvbc[:], IN["ev_g_v"][e_, :].partition_broadcast(128), ["gvbc"])
                ld(lscol[:], IN["ev_pool_scale"][e_, :].rearrange("(g d) -> d g", d=128), ["lscol"], slow=True)
                ld(poolm[:], IN["k_poolm"][:, :, :], ["poolm"])
                for g in range(4):
                    tr(psS[:, g, :], wsraw[:, g, :], ident_f[:], ["wsraw", "ident_f"], ["psS"])
                S_().op("dve", lambda e: e.tensor_tensor(out=wsT[:], in0=psS[:], in1=triu_f[:].unsqueeze(1).to_broadcast([128, 4, 128]), op=ALU.mult),
                     reads=["psS", "triu_f"], writes=["wsT"])

                load_x(ct["xt"][0], "xt0", src, 0)
                for i in range(NT):
                    xt = ct["xt"][i % 2]; xk = "xt%d" % (i % 2)
                    if i + 1 < NT:
                        load_x(ct["xt"][(i + 1) % 2], "xt%d" % ((i + 1) % 2), src, i + 1)
                    norm_mod(ct, xt, xk, A1, B1)
                    hb, hT = ct["hb"], ct["hT"]
                    transpose8(hb, "hb", tp, hT)
                    for fc in range(4):
                        for k in range(8):
                            mm(psU[:, fc, :], Win[:, k, fc * 128:(fc + 1) * 128], hT[:, k, :], k == 0, k == 7, ["Win", "hT"], ["psU"])
                    S_().op("act", lambda e: e.activation(out=uTs[:], in_=psU[:], func=AF.Gelu), reads=["psU"], writes=["uTs"])
                    for k in range(8):
                        mm(psV[:, :], hT[:, k, :], Win[:, k, 512:1024], k == 0, k == 7, ["Win", "hT"], ["psV"])
                    S_().op("act", lambda e: e.activation(out=vs[:], in_=psV[:], func=AF.Gelu), reads=["psV"], writes=["vs"])
                    S_().op("dve", lambda e: e.bn_stats(out=bst[:], in_=vs[:]), reads=["vs"], writes=["bst"])
                    S_().op("dve", lambda e: e.bn_aggr(out=bag[:], in_=bst[:]), reads=["bst"], writes=["bag"])
                    S_().op("act", lambda e: e.activation(out=sd[:], in_=bag[:, 1:2], func=AF.Sqrt, scale=1.0, bias=EPS), reads=["bag"], writes=["sd"])
                    S_().op("dve", lambda e: e.reciprocal(out=sd[:], in_=sd[:]), reads=["sd"], writes=["sd"])
                    S_().op("dve", lambda e: e.tensor_scalar(out=vs[:], in0=vs[:], scalar1=bag[:, 0:1], scalar2=sd[:, 0:1],
                                                          op0=ALU.subtract, op1=ALU.mult), reads=["vs", "bag", "sd"], writes=["vs"])
                    S_().op("pool", lambda e: e.tensor_tensor(out=vn[:], in0=vs[:], in1=gvbc[:], op=ALU.mult), reads=["vs", "gvbc"], writes=["vn"])
                    pc_ = pcur[i % 2]; pck = "pcur%d" % (i % 2); pp_ = pcur[(i + 1) % 2]; ppk = "pcur%d" % ((i + 1) % 2)
                    for k in range(8):
                        mm(psP[:, :], hT[:, k, :], Win[:, k, 1024:1536], k == 0, k == 7, ["Win", "hT"], ["psP"])
                    S_().op("act", lambda e, pc_=pc_: e.copy(out=pc_[:], in_=psP[:]), reads=["psP"], writes=[pck])
                    for g in range(4):
                        mm(psS[:, g, :], vn[:, g * 128:(g + 1) * 128], wsT[:, g, :], True, False, ["vn", "wsT"], ["psS"])
                        mm(psS[:, g, :], ones_row[0:1, :], bsrow[0:1, g * 128:(g + 1) * 128], False, True, ["ones_row", "bsrow"], ["psS"])
                    S_().op("dve", lambda e: e.tensor_tensor(out=yabT[:, 0:4, :], in0=psS[:], in1=uTs[:], op=ALU.mult),
                         reads=["psS", "uTs"], writes=["yabT"])
                    for g in range(4):
                        gi = (4 + g) if i == 0 else g
                        mm(psQ[:, g, :], pc_[:, g * 128:(g + 1) * 128], poolm[:, gi, :], True, i == 0, [pck, "poolm"], ["psQ"])
                        if i > 0:
                            mm(psQ[:, g, :], pp_[:, g * 128:(g + 1) * 128], poolm[:, 8 + g, :], False, True, [ppk, "poolm"], ["psQ"])
                    S_().op("act", lambda e: e.copy(out=pooledT[:], in_=psQ[:]), reads=["psQ"], writes=["pooledT"])
                    for g in range(4):
                        mm(psU[:, g, :], wpool[:, g, :], pooledT[:, g, :], True, True, ["wpool", "pooledT"], ["psU"])
                    for g in range(4):
                        S_().op("dve", lambda e, g=g: e.tensor_scalar(out=yabT[:, 4 + g, :], in0=psU[:, g, :], scalar1=lscol[:, g:g + 1],
                                                                   scalar2=None, op0=ALU.mult), reads=["psU", "lscol"], writes=["yabT"])
                    for half in range(2):
                        for fc in range(8):
                            mm(pso[half][:, :], yabT[:, fc, :], Wout[:, fc, half * 512:(half + 1) * 512], fc == 0, fc == 7,
                               ["yabT", "Wout"], ["pso%d" % half])
                    residual_out(pso, ["pso0", "pso1"], G1, xt, xk, ct["xo"], dst, i)
                s.flush()

        def odd_mixer(l, hsrc, rsrc, dst, hg):
            o_ = l // 2
            lam_init = 0.8 - 0.6 * math.exp(-0.3 * l)
            with contextlib.ExitStack() as ph:
                ct = common_tiles(ph)
                Wq = T("Wq_", [128, 8, 512], BF16, stack=ph)
                Wk = T("Wk_", [128, 8, 512], BF16, stack=ph)
                Wv = T("Wv_", [128, 8, 512], BF16, stack=ph)
                Wout = T("Wout", [128, 4, D], BF16, stack=ph)
                KT = T("KT", [128, 4, S], BF16, stack=ph)
                V = T("V", [128, NT, 4, 130], BF16, stack=ph)
                QT = T("QT", [128, 4, 128], BF16, stack=ph)
                lamt = T("lamt", [128, 4, 64], stack=ph)
                lj = T("lj", [128, 64], stack=ph); ld1 = T("ld1", [128, 2], stack=ph)
                nlam = T("nlam", [128, 1], stack=ph)
                gsub = T("gsub", [128, 128], stack=ph)
                xr = [T("xr%d" % i, [128, D], stack=ph) for i in range(2)] if rsrc is not hsrc else None
                ropeA = T("ropeA", [128, 512], stack=ph); ropeB = T("ropeB", [128, 512], stack=ph)
                qr = T("qr", [128, 512], BF16, stack=ph); kr = T("kr", [128, 512], BF16, stack=ph)
                PT = [T("PT%d" % i, [128, 4, 128], BF16, stack=ph) for i in range(3)]
                mhalf = T("mhalf", [128, 1], stack=ph)
                rz = T("rz", [128, 2], stack=ph); of = T("of", [128, 128], stack=ph); oj = T("oj", [128, 128], stack=ph)
                oss = T("oss", [128, 1], stack=ph)
                Oall = T("Oall", [128, 4, 128], BF16, stack=ph)
                oT = T("oT", [128, 4, 128], BF16, stack=ph)
                tp = PS("tp", [128, 8, 128], BF16, stack=ph)
                psq = PS("psq", [128, 512], stack=ph); psk = PS("psk", [128, 512], stack=ph); psv = PS("psv", [128, 4, 128], stack=ph)
                pss = [PS("pss%d" % i, [128, 4, 128], stack=ph) for i in range(2)]
                pss3 = [pss[0], pss[1], psv]; pss3k = ["pss0", "pss1", "psv"]
                S_().op("dve", lambda e: e.memset(mhalf[:], -0.5), writes=["mhalf"])
                pso_ = [PS("psoh%d" % i, [128, 2, 130], stack=ph) for i in range(2)]

                c0 = hg * 512
                wload_bf16(Wq, "Wq_", IN["od_w_in"][o_][:, c0:c0 + 512])
                wload_bf16(Wk, "Wk_", IN["od_w_in"][o_][:, 1024 + c0:1024 + c0 + 512])
                wload_bf16(Wv, "Wv_", IN["od_w_in"][o_][:, 2048 + c0:2048 + c0 + 512])
                wload_bf16(Wout, "Wout", IN["od_w_out"][o_][hg * 512:(hg + 1) * 512, :], nk=4, parts=2)
                for j, nm in enumerate(("od_lam_q1", "od_lam_k1", "od_lam_q2", "od_lam_k2")):
                    ld(lamt[:, j, :], IN[nm][o_, :].partition_broadcast(128), ["lamt"])
                ld(gsub[:], IN["od_g_sub"][o_, :].partition_broadcast(128), ["gsub"])
                S_().op("dve", lambda e: e.tensor_scalar(out=gsub[:], in0=gsub[:], scalar1=1.0 - lam_init, scalar2=None, op0=ALU.mult),
                     reads=["gsub"], writes=["gsub"])
                for j in range(2):
                    S_().op("dve", lambda e, j=j: e.scalar_tensor_tensor(out=lj[:], in0=lamt[:, 2 * j, :], scalar=1.0, in1=lamt[:, 2 * j + 1, :],
                                                                      op0=ALU.mult, op1=ALU.mult, accum_out=ld1[:, j:j + 1]),
                         reads=["lamt"], writes=["lj", "ld1"])
                S_().op("act", lambda e: e.activation(out=ld1[:], in_=ld1[:], func=AF.Exp), reads=["ld1"], writes=["ld1"])
                S_().op("dve", lambda e: e.tensor_tensor(out=nlam[:], in0=ld1[:, 1:2], in1=ld1[:, 0:1], op=ALU.subtract), reads=["ld1"], writes=["nlam"])
                S_().op("dve", lambda e: e.tensor_scalar(out=nlam[:], in0=nlam[:], scalar1=-lam_init, scalar2=None, op0=ALU.add), reads=["nlam"], writes=["nlam"])
                S_().op("dve", lambda e: e.memset(V[:, :, :, 128:130], 1.0), writes=[("V", i_) for i_ in range(NT)])

                def rope(ps, pskey, out, outkey, i):
                    v4 = lambda ap: ap.rearrange("p (g two i) -> p g two i", g=8, two=2)
                    cb = cosT[:, i, :].unsqueeze(1).unsqueeze(1).to_broadcast([128, 8, 2, 32])
                    sb = sinT[:, i, :].unsqueeze(1).to_broadcast([128, 8, 32])
                    nsb = nsinT[:, i, :].unsqueeze(1).to_broadcast([128, 8, 32])
                    S_().op("dve", lambda e: e.tensor_tensor(out=v4(ropeA[:]), in0=v4(ps[:, :]), in1=cb, op=ALU.mult),
                         reads=[pskey, "cosT"], writes=["ropeA"])
                    S_().op("dve", lambda e: e.tensor_tensor(out=v4(ropeB[:])[:, :, 0, :], in0=v4(ps[:, :])[:, :, 1, :], in1=nsb, op=ALU.mult),
                         reads=[pskey, "nsinT"], writes=["ropeB"])
                    S_().op("dve", lambda e: e.tensor_tensor(out=v4(ropeB[:])[:, :, 1, :], in0=v4(ps[:, :])[:, :, 0, :], in1=sb, op=ALU.mult),
                         reads=[pskey, "sinT"], writes=["ropeB"])
                    S_().op("pool", lambda e: e.tensor_tensor(out=out[:], in0=ropeA[:], in1=ropeB[:], op=ALU.add),
                         reads=["ropeA", "ropeB"], writes=[outkey])

                QTs = [QT, T("QTb", [128, 4, 128], BF16, stack=ph)]

                def stageA(i):
                    o = S_()
                    xt = ct["xt"][i % 2]; xk = "xt%d" % (i % 2)
                    load_x(xt, xk, hsrc, i)
                    if xr is not None:
                        load_x(xr[i % 2], "xr%d" % (i % 2), rsrc, i)
                    norm_mod(ct, xt, xk, A1, B1)
                    hb, hT = ct["hb"], ct["hT"]
                    transpose8(hb, "hb", tp, hT)
                    for k in range(8):
                        mm(psq[:, :], hT[:, k, :], Wq[:, k, :], k == 0, k == 7, ["Wq_", "hT"], ["psq"])
                    for k in range(8):
                        mm(psk[:, :], hT[:, k, :], Wk[:, k, :], k == 0, k == 7, ["Wk_", "hT"], ["psk"])
                    rope(psq, "psq", qr, "qr", i)
                    for k in range(8):
                        mm(psq[:, :], hT[:, k, :], Wv[:, k, :], k == 0, k == 7, ["Wv_", "hT"], ["psq"])
                    rope(psk, "psk", kr, "kr", i)
                    o.op("act", lambda e, i=i: e.copy(out=V[:, i, :, 0:128], in_=psq[:, :].rearrange("p (h d) -> p h d", h=4)), reads=["psq"], writes=[("V", i)])
                    for hh in range(4):
                        tr(tp[:, hh, :], qr[:, hh * 128:(hh + 1) * 128], ident_b[:], ["qr", "ident_b"], ["tp"])
                    for hh in range(4):
                        tr(tp[:, 4 + hh, :], kr[:, hh * 128:(hh + 1) * 128], ident_b[:], ["kr", "ident_b"], ["tp"])
                    qt_ = QTs[i % 2]
                    o.op("act", lambda e: e.copy(out=qt_[:], in_=tp[:, 0:4, :]), reads=["tp"], writes=["QT%d" % (i % 2)])
                    o.op("act", lambda e, i=i: e.copy(out=KT[:, :, i * 128:(i + 1) * 128], in_=tp[:, 4:8, :]), reads=["tp"], writes=[("KT", i)])

                def stageB(i, filler):
                    o = S_()
                    xt = ct["xt"][i % 2]; xk = "xt%d" % (i % 2)
                    qt_ = QTs[i % 2]; qtk = "QT%d" % (i % 2)
                    items = []
                    for hh in range(4):
                        for m in range(2):
                            for g0 in range(0, i + 1, 4):
                                items.append((hh, m, g0, list(range(g0, min(g0 + 4, i + 1)))))

                    def emitS(it, sl_):
                        hh, m, g0, kbs = it
                        rows = slice(m * 64, (m + 1) * 64)
                        bank = pss3[sl_]; bk = pss3k[sl_]
                        pt = PT[sl_]; ptk = "PT%d" % sl_
                        for j, kb in enumerate(kbs):
                            mm(bank[:, j, :], KT[rows, hh, kb * 128:(kb + 1) * 128], qt_[rows, hh, :], True, True, [("KT", kb), qtk], [bk])
                        n = len(kbs)
                        o.op("act", lambda e, pt=pt, bank=bank, n=n: e.activation(out=pt[:, 0:n, :], in_=bank[:, 0:n, :], func=AF.Exp, scale=0.125),
                             reads=[bk], writes=[ptk])
                        if i in kbs:
                            jd = i - g0
                            o.op("pool", lambda e, pt=pt, jd=jd: e.tensor_tensor(out=pt[:, jd, :], in0=pt[:, jd, :], in1=triu_b[:], op=ALU.mult),
                                 reads=[ptk, "triu_b"], writes=[ptk])

                    def emitAV(it, sl_):
                        hh, m, g0, kbs = it
                        po = pso_[hh % 2]; pok = "psoh%d" % (hh % 2)
                        pt = PT[sl_]; ptk = "PT%d" % sl_
                        for j, kb in enumerate(kbs):
                            mm(po[:, m, 0:129], pt[:, j, :], V[:, kb, hh, 0:129], kb == 0, kb == i, [ptk, ("V", kb)], [pok])
                        if m == 1 and kbs[-1] == i:
                            o.op("dve", lambda e, po=po: e.reciprocal(out=rz[:], in_=po[:, :, 128]), reads=[pok], writes=["rz"])
                            o.op("dve", lambda e: e.tensor_tensor(out=rz[:, 1:2], in0=rz[:, 1:2], in1=nlam[:], op=ALU.mult), reads=["rz", "nlam"], writes=["rz"])
                            o.op("dve", lambda e, po=po: e.tensor_scalar(out=of[:], in0=po[:, 0, 0:128], scalar1=rz[:, 0:1], scalar2=None, op0=ALU.mult),
                                 reads=[pok, "rz"], writes=["of"])
                            o.op("dve", lambda e, po=po: e.scalar_tensor_tensor(out=of[:], in0=po[:, 1, 0:128], scalar=rz[:, 1:2], in1=of[:], op0=ALU.mult, op1=ALU.add),
                                 reads=[pok, "rz", "of"], writes=["of"])
                            o.op("dve", lambda e: e.scalar_tensor_tensor(out=oj[:], in0=of[:], scalar=1.0, in1=of[:], op0=ALU.mult, op1=ALU.mult, accum_out=oss[:]),
                                 reads=["of"], writes=["oj", "oss"])
                            o.op("dve", lambda e: e.tensor_scalar(out=oss[:], in0=oss[:], scalar1=1.0 / 128, scalar2=EPS, op0=ALU.mult, op1=ALU.add),
                                 reads=["oss"], writes=["oss"])
                            o.op("pool", lambda e: e.tensor_tensor(out=oss[:], in0=oss[:], in1=mhalf[:], op=ALU.pow), reads=["oss", "mhalf"], writes=["oss"])
                            o.op("dve", lambda e, hh=hh: e.scalar_tensor_tensor(out=Oall[:, hh, :], in0=of[:], scalar=oss[:, 0:1], in1=gsub[:], op0=ALU.mult, op1=ALU.mult),
                                 reads=["of", "oss", "gsub"], writes=["Oall"])

                    prev = None
                    for n_, it in enumerate(items):
                        emitS(it, n_ % 3)
                        if prev is not None:
                            emitAV(*prev)
                        prev = (it, n_ % 3)
                        if FILL_MODE == 0 or it[3][-1] == i:
                            filler(len(items) if FILL_MODE == 0 else 8)
                    emitAV(*prev)
                    for hh in range(4):
                        tr(tp[:, hh, :], Oall[:, hh, :], ident_b[:], ["Oall", "ident_b"], ["tp"])
                    o.op("act", lambda e: e.copy(out=oT[:], in_=tp[:, 0:4, :]), reads=["tp"], writes=["oT"])
                    pso = [psq, psk]
                    for half in range(2):
                        for hh in range(4):
                            mm(pso[half][:, :], oT[:, hh, :], Wout[:, hh, half * 512:(half + 1) * 512], hh == 0, hh == 3,
                               ["oT", "Wout"], [["psq", "psk"][half]])
                    if xr is not None:
                        residual_out(pso, ["psq", "psk"], G1, xr[i % 2], "xr%d" % (i % 2), ct["xo"], dst, i)
                    else:
                        residual_out(pso, ["psq", "psk"], G1, xt, xk, ct["xo"], dst, i)

                stageA(0)
                for i in range(NT):
                    nxt = Deferred()
                    if i + 1 < NT:
                        sink[0] = nxt
                        stageA(i + 1)
                        sink[0] = s
                    tot = len(nxt.q)
                    if FILL_MODE == 2:
                        import os
                        NPRE = int(os.environ.get("NPRE", "0"))
                        done = [False]

                        def f2(nit):
                            if not done[0]:
                                nxt.run(s, NPRE); done[0] = True
                        stageB(i, f2)
                    else:
                        stageB(i, lambda nit: nxt.run(s, (tot + nit - 1) // nit))
                    nxt.run(s, None)
                s.flush()

        def peer_pass(l, src, dst, fin):
            with contextlib.ExitStack() as ph:
                junk = T("junk", [128, D], BF16, stack=ph)
                ss = T("ss", [128, 1], stack=ph); rs = T("rs", [128, 1], stack=ph)
                hf = T("hf", [128, D], stack=ph); hb = T("hb", [128, D], BF16, stack=ph)
                hT = T("hT", [128, 8, 128], BF16, stack=ph)
                xo = T("xo", [128, D], stack=ph)
                xts = [T("xt%d" % i, [128, D], stack=ph) for i in range(2)]
                Wq = T("Wqp", [128, 8, 2048], BF16, stack=ph)
                skT = T("skT", [128, 16, 128], stack=ph)
                qT = T("qT", [128, 16, 128], stack=ph)
                sc = T("sc", [128, 16, 128], stack=ph)
                sc2 = T("sc2", [128, 16, 128], stack=ph)
                skraw = sc2
                sv = T("sv", [128, 16, 16], stack=ph)
                si = T("si", [128, 16, 16], U32, stack=ph)
                sif = T("sif", [128, 16, 16], stack=ph)
                cand = sc[:].rearrange("p (h a) n -> p h (a n)", h=8)
                cand2 = sc2[:].rearrange("p (h a) n -> p h (a n)", h=8)
                fv = T("fv", [128, 8, 16], stack=ph)
                fpos = T("fpos", [128, 8, 16], U32, stack=ph)
                fa = T("fa", [128, 8, 16], U32, stack=ph); fb = T("fb", [128, 8, 16], U32, stack=ph)
                faf = T("faf", [128, 8, 16], stack=ph); fbf = T("fbf", [128, 8, 16], stack=ph)
                oh = sc2[:].rearrange("p (h a) (b c) -> p h (a b) c", h=8, c=16)
                Ii = T("Ii", [128, 8, 16], stack=ph); Jj = T("Jj", [128, 8, 16], stack=ph)
                eif = T("eif", [128, 128], stack=ph)
                eidxs = [T("eidx%d" % i, [128, 128], U32, stack=ph) for i in range(2)]
                ge = T("ge", [128, 8, 16], stack=ph); gz = T("gz", [128, 8], stack=ph)
                gates = [T("gate%d" % i, [128, 128], stack=ph) for i in range(2)]
                act = T("act", [128, 128], stack=ph)
                wgt = T("wgt", [128, 128], stack=ph)
                NB = 16
                UV = [T("UV%d" % i, [128, 2 * D], BF16, stack=ph) for i in range(NB)]
                diag = [T("diag%d" % i, [128, 128], BF16, stack=ph) for i in range(2)]
                dj = T("dj", [128, D], BF16, stack=ph)
                fin_t = None
                if fin:
                    fnbc = T("fnbc", [128, D], stack=ph)
                    fss = T("fss", [128, 1], stack=ph); frs = T("frs", [128, 1], stack=ph)
                    ld(fnbc[:], IN["final_norm"][0, :].partition_broadcast(128), ["fnbc"])
                    fin_t = (dj, fss, frs, fnbc, xo)
                tp = PS("tp", [128, 8, 128], BF16, stack=ph)
                psx = [PS("psx%d" % i, [128, 4, 128], stack=ph) for i in range(2)]
                pso = [PS("psop%d" % i, [128, 512], stack=ph) for i in range(2)]
                hps = [PS("hps%d" % i, [128, D], BF16, stack=ph) for i in range(2)]

                wload_bf16(Wq, "Wqp", IN["peer_w_q"][l])
                ld(skraw[:], IN["peer_sub_keys"][l].rearrange("g n k -> n g k"), ["sc2_%d" % g for g in range(16)])
                for r in range(4):
                    for c_ in range(4):
                        tr(psx[r % 2][:, c_, :], skraw[:, r * 4 + c_, :], ident_f[:], ["sc2_%d" % (r * 4 + c_), "ident_f"], ["psx%d" % (r % 2)])
                    S_().op("act", lambda e, r=r: e.copy(out=skT[:, r * 4:(r + 1) * 4, :], in_=psx[r % 2][:]), reads=["psx%d" % (r % 2)], writes=["skT"])

                def stageA(i):
                    xt = xts[i % 2]; xk = "xt%d" % (i % 2)
                    eidx = eidxs[i % 2]; ek = "eidx%d" % (i % 2)
                    gate = gates[i % 2]; gk = "gate%d" % (i % 2)
                    hp_ = hps[i % 2]; hpk = "hps%d" % (i % 2)
                    o = S_()
                    load_x(xt, xk, src, i)
                    o.op("act", lambda e: e.activation(out=junk[:], in_=xt[:], func=AF.Square, accum_out=ss[:]), reads=[xk], writes=["junk", "ss"])
                    o.op("act", lambda e: e.activation(out=rs[:], in_=ss[:], func=AF.Sqrt, scale=1.0 / D, bias=EPS), reads=["ss"], writes=["rs"])
                    o.op("dve", lambda e: e.reciprocal(out=rs[:], in_=rs[:]), reads=["rs"], writes=["rs"])
                    o.op("dve", lambda e: e.scalar_tensor_tensor(out=hf[:], in0=xt[:], scalar=rs[:, 0:1], in1=A2[:], op0=ALU.mult, op1=ALU.mult),
                         reads=[xk, "rs", A2.name], writes=["hf"])
                    o.op("pool", lambda e: e.tensor_tensor(out=hb[:], in0=hf[:], in1=B2[:], op=ALU.add), reads=["hf", B2.name], writes=["hb"])
                    for k in range(8):
                        tr(tp[:, k, :], hb[:, k * 128:(k + 1) * 128], ident_b[:], ["hb", "ident_b"], ["tp"])
                    o.op("act", lambda e: e.copy(out=hT[:], in_=tp[:]), reads=["tp"], writes=["hT"])
                    for k in range(8):
                        tr(hp_[:, k * 128:(k + 1) * 128], hT[:, k, :], ident_b[:], ["hT", "ident_b"], [hpk])
                    for r in range(4):
                        pq = psx[r % 2]; pqk = "psx%d" % (r % 2)
                        for c_ in range(4):
                            hp = r * 4 + c_
                            for k in range(8):
                                mm(pq[:, c_, :], Wq[:, k, hp * 128:(hp + 1) * 128], hT[:, k, :], k == 0, k == 7, ["Wqp", "hT"], [pqk])
                        o.op("act", lambda e, r=r, pq=pq: e.copy(out=qT[:, r * 4:(r + 1) * 4, :], in_=pq[:]), reads=[pqk], writes=["qT"])
                    for r in range(4):
                        pz = psx[r % 2]; pzk = "psx%d" % (r % 2)
                        for c_ in range(4):
                            hp = r * 4 + c_
                            mm(pz[:, c_, :], qT[:, hp, :], skT[:, hp, :], True, True, ["qT", "skT"], [pzk])
                        o.op("act", lambda e, r=r, pz=pz: e.copy(out=sc[:, r * 4:(r + 1) * 4, :], in_=pz[:]), reads=[pzk],
                             writes=["sc_%d" % g for g in range(r * 4, r * 4 + 4)])
                    G16 = range(16)
                    SCK = ["sc_%d" % g for g in G16]; SC2K = ["sc2_%d" % g for g in G16]
                    SVA = ["sva%d" % g for g in G16]; SVB = ["svb%d" % g for g in G16]
                    SIA = ["sia%d" % g for g in G16]; SIB = ["sib%d" % g for g in G16]
                    for g in G16:
                        o.op("dve", lambda e, g=g: e.max(out=sv[:, g, 0:8], in_=sc[:, g, :]), reads=[SCK[g]], writes=[SVA[g]])
                    for g in G16:
                        o.op("dve", lambda e, g=g: e.max_index(out=si[:, g, 0:8], in_max=sv[:, g, 0:8], in_values=sc[:, g, :]), reads=[SCK[g], SVA[g]], writes=[SIA[g]])
                    for g in G16:
                        o.op("dve", lambda e, g=g: e.match_replace(out=sc2[:, g, :], in_to_replace=sv[:, g, 0:8], in_values=sc[:, g, :], imm_value=-1e30),
                             reads=[SCK[g], SVA[g]], writes=[SC2K[g]])
                    for g in G16:
                        o.op("dve", lambda e, g=g: e.max(out=sv[:, g, 8:16], in_=sc2[:, g, :]), reads=[SC2K[g]], writes=[SVB[g]])
                    for g in G16:
                        o.op("dve", lambda e, g=g: e.max_index(out=si[:, g, 8:16], in_max=sv[:, g, 8:16], in_values=sc2[:, g, :]), reads=[SC2K[g], SVB[g]], writes=[SIB[g]])
                    o.op("dve", lambda e: e.tensor_copy(out=sif[:], in_=si[:]), reads=SIA + SIB, writes=["sif"])
                    sv4 = sv[:].rearrange("p (h two) k -> p h two k", two=2)
                    sif4 = sif[:].rearrange("p (h two) k -> p h two k", two=2)
                    o.op("dve", lambda e: e.tensor_tensor(out=cand.rearrange("p h (a b) -> p h a b", a=16),
                                                          in0=sv4[:, :, 0, :].unsqueeze(3).to_broadcast([128, 8, 16, 16]),
                                                          in1=sv4[:, :, 1, :].unsqueeze(2).to_broadcast([128, 8, 16, 16]), op=ALU.add),
                         reads=SVA + SVB, writes=SCK)
                    H8 = range(8)
                    CK = [[SCK[2 * h], SCK[2 * h + 1]] for h in H8]; C2K = [[SC2K[2 * h], SC2K[2 * h + 1]] for h in H8]
                    FVA = ["fva%d" % h for h in H8]; FVB = ["fvb%d" % h for h in H8]
                    FPA = ["fpa%d" % h for h in H8]; FPB = ["fpb%d" % h for h in H8]
                    for h in H8:
                        o.op("dve", lambda e, h=h: e.max(out=fv[:, h, 0:8], in_=cand[:, h, :]), reads=CK[h], writes=[FVA[h]])
                    for h in H8:
                        o.op("dve", lambda e, h=h: e.max_index(out=fpos[:, h, 0:8], in_max=fv[:, h, 0:8], in_values=cand[:, h, :]), reads=CK[h] + [FVA[h]], writes=[FPA[h]])
                    for h in H8:
                        o.op("dve", lambda e, h=h: e.match_replace(out=cand2[:, h, :], in_to_replace=fv[:, h, 0:8], in_values=cand[:, h, :], imm_value=-1e30),
                             reads=CK[h] + [FVA[h]], writes=C2K[h])
                    for h in H8:
                        o.op("dve", lambda e, h=h: e.max(out=fv[:, h, 8:16], in_=cand2[:, h, :]), reads=C2K[h], writes=[FVB[h]])
                    for h in H8:
                        o.op("dve", lambda e, h=h: e.max_index(out=fpos[:, h, 8:16], in_max=fv[:, h, 8:16], in_values=cand2[:, h, :]), reads=C2K[h] + [FVB[h]], writes=[FPB[h]])
                    FV = FVA + FVB; FP = FPA + FPB
                    o.op("dve", lambda e: e.tensor_tensor(out=ge[:], in0=fv[:], in1=fv[:, :, 0:1].to_broadcast([128, 8, 16]), op=ALU.subtract),
                         reads=FV, writes=["ge"])
                    o.op("act", lambda e: e.activation(out=ge[:], in_=ge[:], func=AF.Exp), reads=["ge"], writes=["ge"])
                    o.op("dve", lambda e: e.tensor_reduce(out=gz[:], in_=ge[:], axis=AX.X, op=ALU.add), reads=["ge"], writes=["gz"])
                    o.op("dve", lambda e: e.reciprocal(out=gz[:], in_=gz[:]), reads=["gz"], writes=["gz"])
                    o.op("dve", lambda e: e.tensor_tensor(out=gate[:].rearrange("p (h k) -> p h k", h=8), in0=ge[:],
                                                          in1=gz[:].unsqueeze(2).to_broadcast([128, 8, 16]), op=ALU.mult),
                         reads=["ge", "gz"], writes=[gk])
                    o.op("dve", lambda e: e.tensor_single_scalar(out=fa[:], in_=fpos[:], scalar=4, op=ALU.logical_shift_right), reads=FP, writes=["fa"])
                    o.op("dve", lambda e: e.tensor_single_scalar(out=fb[:], in_=fpos[:], scalar=15, op=ALU.bitwise_and), reads=FP, writes=["fb"])
                    o.op("dve", lambda e: e.tensor_copy(out=faf[:], in_=fa[:]), reads=["fa"], writes=["faf"])
                    o.op("dve", lambda e: e.tensor_copy(out=fbf[:], in_=fb[:]), reads=["fb"], writes=["fbf"])
                    io4 = iota16[:].unsqueeze(1).unsqueeze(1).to_broadcast([128, 8, 16, 16])
                    for (srcf, sk_, half, dstt, dk) in ((faf, "faf", 0, Ii, "Ii"), (fbf, "fbf", 1, Jj, "Jj")):
                        o.op("dve", lambda e, srcf=srcf: e.tensor_tensor(out=oh, in0=srcf[:].unsqueeze(3).to_broadcast([128, 8, 16, 16]), in1=io4, op=ALU.is_equal),
                             reads=[sk_, "iota16"], writes=SC2K)
                        o.op("dve", lambda e, half=half: e.tensor_tensor(out=oh, in0=oh, in1=sif4[:, :, half, :].unsqueeze(2).to_broadcast([128, 8, 16, 16]), op=ALU.mult),
                             reads=SC2K + ["sif"], writes=SC2K)
                        o.op("dve", lambda e, dstt=dstt: e.tensor_reduce(out=dstt[:], in_=oh, axis=AX.X, op=ALU.add), reads=SC2K, writes=[dk])
                    o.op("dve", lambda e: e.scalar_tensor_tensor(out=eif[:].rearrange("p (h k) -> p h k", h=8), in0=Ii[:], scalar=128.0, in1=Jj[:], op0=ALU.mult, op1=ALU.add),
                         reads=["Ii", "Jj"], writes=["eif"])
                    if l > 0:
                        o.op("dve", lambda e: e.tensor_scalar(out=eif[:], in0=eif[:], scalar1=float(l * NEXP), scalar2=None, op0=ALU.add),
                             reads=["eif"], writes=["eif"])
                    o.op("dve", lambda e: e.tensor_copy(out=eidx[:], in_=eif[:]), reads=["eif"], writes=[ek])

                nuv = [0]

                def stageB(i, filler):
                    xt = xts[i % 2]; xk = "xt%d" % (i % 2)
                    eidx = eidxs[i % 2]; ek = "eidx%d" % (i % 2)
                    gate = gates[i % 2]; gk = "gate%d" % (i % 2)
                    hp_ = hps[i % 2]; hpk = "hps%d" % (i % 2)
                    o = S_()
                    LOOK = 8
                    order = list(range(128))
                    bufs = {}

                    def issue(sl):
                        b = nuv[0] % NB; nuv[0] += 1
                        bufs[sl] = b
                        ub = UV[b]
                        o.dma("pool", lambda e, ub=ub, sl=sl: e.indirect_dma_start(
                            out=ub[:], out_offset=None, in_=uvb[:, :],
                            in_offset=bass.IndirectOffsetOnAxis(ap=eidx[:, sl:sl + 1], axis=0)), reads=[ek, ("uvb", l)], writes=["UV%d" % b])

                    for sl in range(min(LOOK, 128)):
                        issue(sl)
                    GS = 4

                    def finish(g0):
                        hs = slice(g0, g0 + GS)
                        wk = "wgt%d" % (g0 // GS)
                        o.op("dve", lambda e, hs=hs: e.tensor_tensor(out=wgt[:, hs], in0=wgt[:, hs], in1=gate[:, hs], op=ALU.mult), reads=[wk, gk], writes=[wk])
                        for sl in range(g0, g0 + GS):
                            b = bufs[sl]; ub = UV[b]; ubk = "UV%d" % b
                            dg = diag[sl % 2]; dgk = "diag%d" % (sl % 2)
                            o.op("act", lambda e, dg=dg, sl=sl: e.activation(out=dg[:], in_=ident_f[:], func=AF.Copy, scale=wgt[:, sl:sl + 1]),
                                 reads=["ident_f", wk], writes=[dgk])
                            for half in range(2):
                                mm(pso[half][:, :], dg[:], ub[:, D + half * 512: D + (half + 1) * 512], sl == 0, sl == 127, [dgk, ubk], ["psop%d" % half])

                    for g0 in range(0, 128, GS):
                        ak = "act%d" % (g0 // GS); wk = "wgt%d" % (g0 // GS)
                        for sl in range(g0, g0 + GS):
                            if sl + LOOK < 128:
                                issue(sl + LOOK)
                            b = bufs[sl]; ub = UV[b]; ubk = "UV%d" % b
                            o.op("dve", lambda e, ub=ub, sl=sl: e.scalar_tensor_tensor(out=dj[:], in0=ub[:, 0:D], scalar=1.0, in1=hp_[:], op0=ALU.mult, op1=ALU.mult,
                                                                                       accum_out=act[:, sl:sl + 1]),
                                 reads=[ubk, hpk], writes=[ak])
                            filler()
                        hs = slice(g0, g0 + GS)
                        o.op("act", lambda e, hs=hs: e.activation(out=wgt[:, hs], in_=act[:, hs], func=AF.Gelu), reads=[ak], writes=[wk])
                        if g0 > 0:
                            finish(g0 - GS)
                    finish(128 - GS)
                    residual_out(pso, ["psop0", "psop1"], G2, xt, xk, xo, dst, i, fin=fin_t)

                stageA(0)
                for i in range(NT):
                    nxt = Deferred()
                    if i + 1 < NT:
                        sink[0] = nxt
                        stageA(i + 1)
                        sink[0] = s
                    per = (len(nxt.q) + 127) // 128
                    stageB(i, lambda: nxt.run(s, per))
                    nxt.run(s, None)
                s.flush()

        cur = IN["x"]
        free = [0, 1, 2]

        def take(exclude):
            for b in free:
                if xs[b] is not exclude and all(xs[b] is not e_ for e_ in exclude if e_ is not None):
                    return xs[b]
            raise RuntimeError("no buffer")

        nl = len(layers)
        for li, l in enumerate(layers):
            compute_mod(l)
            if do_mixer:
                if l % 2 == 0:
                    d1 = [b for b in xs if b is not cur][0]
                    even_mixer(l, cur, d1)
                    cur = d1
                else:
                    others = [b for b in xs if b is not cur]
                    odd_mixer(l, cur, cur, others[0], 0)
                    odd_mixer(l, cur, others[0], others[1], 1)
                    cur = others[1]
            if do_peer:
                last = (li == nl - 1)
                d2 = y_out if last else [b for b in xs if b is not cur][0]
                peer_pass(l, cur, d2, fin=(last and final_norm))
                cur = d2
        if cur is not y_out:
            with contextlib.ExitStack() as ph:
                tt = T("cp", [128, D], stack=ph)
                for i in range(NT):
                    ld(tt[:], cur[i * 128:(i + 1) * 128, :], ["cp"], r=[(cur.tensor.name, i)])
                    ld(y_out[i * 128:(i + 1) * 128, :], tt[:], [("y", i)], r=["cp"])
                s.flush()
        s.wait_all("sp")
        s.flush()
        build.n_instr = s.n_instr
    return nc


def make_in_maps(inputs, n_cores, S=4096):
    consts = host_consts()
    shared = {}
    for k in IN_SHAPES:
        if k in consts:
            shared[k] = consts[k]
        else:
            shared[k] = np.ascontiguousarray(np.asarray(inputs[k], dtype=np.float32)).reshape(IN_SHAPES[k][0])
    maps = []
    x = np.asarray(inputs["x"], dtype=np.float32)
    c = np.asarray(inputs["c"], dtype=np.float32)
    pos = np.asarray(inputs["positions"]).astype(np.int32)
    for b in range(n_cores):
        m = dict(shared)
        m["x"] = np.ascontiguousarray(x[b, :S])
        m["c"] = np.ascontiguousarray(c[b:b + 1])
        m["positions"] = np.ascontiguousarray(pos[b:b + 1, :S])
        maps.append(m)
    return maps


def kernel(**inputs):
    n = 8
    nc = build(4096)
    maps = make_in_maps(inputs, n)
    res = run_bass_kernel_spmd(nc, maps, core_ids=list(range(n)))
    return np.stack([np.asarray(r["y"], dtype=np.float32) for r in res.results], axis=0)
```

```python
import contextlib
import math
import numpy as np
import concourse.bass as bass
import concourse.mybir as mybir
from concourse.bass_utils import run_bass_kernel_spmd

F32 = mybir.dt.float32
BF16 = mybir.dt.bfloat16
U32 = mybir.dt.uint32
I32 = mybir.dt.int32
AF = mybir.ActivationFunctionType
ALU = mybir.AluOpType
AX = mybir.AxisListType

D = 1024
EPS = 1e-6
NEXP = 16384
ENGS = ("pe", "act", "dve", "pool", "sp")
SAME_ENGINE_SYNC = True
NO_SAME_SYNC = set()
FILL_MODE = 0


class Sched:
    def __init__(self, nc, st, n_dma_sems=12):
        self.nc = nc
        self.ops = {e: [] for e in ENGS}
        self.cnt = {e: 0 for e in ENGS}
        self.known = {e: {} for e in ENGS}
        self.lastw = {}
        self.reads = {}
        self.n_dma_sems = n_dma_sems
        self.dma_cnt = {}
        self.dma_rr = {e: 0 for e in ENGS}
        self.semh = {}
        for e in ENGS:
            self.semh[e] = st.enter_context(nc.semaphore("s_" + e))
        for q in ("sp", "pool", "act"):
            for i in range(n_dma_sems):
                self.semh[(q, i)] = st.enter_context(nc.semaphore("d_%s%d" % (q, i)))
        self.n_instr = 0

    def _deps(self, eng, reads, writes, skip_same):
        waits = {}

        def need(ev):
            if ev is None:
                return
            k, v = ev
            if k == eng and (skip_same or not SAME_ENGINE_SYNC or eng in NO_SAME_SYNC):
                return
            if self.known[eng].get(k, 0) >= v:
                return
            if waits.get(k, 0) < v:
                waits[k] = v

        for b in reads:
            need(self.lastw.get(b))
        for b in writes:
            need(self.lastw.get(b))
            for ev in self.reads.get(b, ()):
                need(ev)
        for k, v in waits.items():
            self.known[eng][k] = v
        return waits

    def _record(self, ev, reads, writes):
        for b in reads:
            lst = self.reads.setdefault(b, [])
            lst[:] = [x for x in lst if x[0] != ev[0]]
            lst.append(ev)
        for b in writes:
            self.lastw[b] = ev
            self.reads[b] = []

    def op(self, eng, fn, reads=(), writes=(), skip_same=False, nostop=False):
        waits = self._deps(eng, reads, writes, skip_same)
        self.cnt[eng] += 1
        ev = (eng, self.cnt[eng])
        self.ops[eng].append(("op", fn, waits, ev))
        self._record(ev, reads, writes)
        return ev

    def dma(self, queue, fn, reads=(), writes=()):
        i = self.dma_rr[queue]
        self.dma_rr[queue] = (i + 1) % self.n_dma_sems
        key = (queue, i)
        prev = self.dma_cnt.get(key, 0)
        waits = self._deps(queue, reads, writes, False)
        if prev > 0 and self.known[queue].get(key, 0) < prev * 16:
            waits[key] = prev * 16
            self.known[queue][key] = prev * 16
        self.dma_cnt[key] = prev + 1
        ev = (key, (prev + 1) * 16)
        self.ops[queue].append(("dma", fn, waits, ev))
        self._record(ev, reads, writes)
        return ev

    def wait_all(self, eng):
        waits = {}
        for e in ENGS:
            if e != eng and self.cnt[e] > 0:
                waits[e] = self.cnt[e]
        for key, c in self.dma_cnt.items():
            waits[key] = c * 16
        self.ops[eng].append(("wait", None, waits, None))

    def flush(self):
        nc = self.nc
        if not any(self.ops[e] for e in ENGS):
            return
        with nc.Block() as block:
            regs = {"pe": block.tensor, "act": block.scalar, "dve": block.vector,
                    "pool": block.gpsimd, "sp": block.sync}

            def make(e, lst):
                def body(engobj):
                    for kind, fn, waits, ev in lst:
                        for k, v in waits.items():
                            engobj.wait_ge(self.semh[k], v)
                        if kind == "wait":
                            continue
                        ins = fn(engobj)
                        if kind == "dma":
                            ins.then_inc(self.semh[ev[0]], 16)
                        else:
                            ins.then_inc(self.semh[e], 1)
                return body

            for e in ENGS:
                lst = self.ops[e]
                self.n_instr += len(lst)
                if lst:
                    regs[e](make(e, lst))
        self.ops = {e: [] for e in ENGS}


class Deferred:
    def __init__(self):
        self.q = []

    def op(self, *a, **k):
        self.q.append(("op", a, k))

    def dma(self, *a, **k):
        self.q.append(("dma", a, k))

    def run(self, s, n):
        m = len(self.q) if n is None else min(n, len(self.q))
        while 0 < m < len(self.q) and self.q[m - 1][2].get("nostop"):
            m += 1
        for kind, a, k in self.q[:m]:
            getattr(s, kind)(*a, **k)
        del self.q[:m]


def host_consts():
    ident = np.eye(128, dtype=np.float32)
    p = np.arange(128)[:, None]
    j = np.arange(128)[None, :]
    triu = (j >= p).astype(np.float32)
    poolm = np.zeros((12, 128, 128), np.float32)
    for g, w in enumerate((2, 4, 8, 16)):
        band = ((j - p >= 0) & (j - p <= w - 1)).astype(np.float32)
        poolm[g] = band / w - ident
        cnt = np.minimum(np.arange(128) + 1, w).astype(np.float32)[None, :]
        poolm[4 + g] = band / cnt - ident
        poolm[8 + g] = ((j - p + 128) <= (w - 1)).astype(np.float32) / w
    iota16 = np.tile(np.arange(16, dtype=np.float32)[None, :], (128, 1))
    inv_freq = 1.0 / (10000.0 ** (np.arange(0, 64, 2, dtype=np.float32) / 64.0))
    invf = np.tile((inv_freq / (2.0 * np.pi)).astype(np.float32)[None, :], (128, 1))
    return {"k_ident": ident, "k_triu": triu, "k_poolm": poolm.transpose(1, 0, 2).copy(),
            "k_iota16": iota16, "k_invf": invf}


IN_SHAPES = {
    "ada_w": ([4, 1024, 6144], F32), "ada_b": ([4, 6144], F32),
    "norm_mix": ([4, 1024], F32), "norm_ffn": ([4, 1024], F32),
    "ev_w_in": ([2, 1024, 1536], F32), "ev_g_v": ([2, 512], F32),
    "ev_w_s": ([2, 4, 128, 128], F32), "ev_b_s": ([2, 512], F32),
    "ev_w_pool": ([2, 4, 128, 128], F32), "ev_pool_scale": ([2, 512], F32),
    "ev_w_out": ([2, 1024, 1024], F32), "od_w_in": ([2, 1024, 3072], F32),
    "od_lam_q1": ([2, 64], F32), "od_lam_k1": ([2, 64], F32),
    "od_lam_q2": ([2, 64], F32), "od_lam_k2": ([2, 64], F32),
    "od_g_sub": ([2, 128], F32), "od_w_out": ([2, 1024, 1024], F32),
    "peer_w_q": ([4, 1024, 2048], F32), "peer_sub_keys": ([4, 16, 128, 128], F32),
    "peer_u": ([4, NEXP, 1024], F32), "peer_v": ([4, NEXP, 1024], F32),
    "final_norm": ([1, 1024], F32),
    "k_ident": ([128, 128], F32), "k_triu": ([128, 128], F32), "k_poolm": ([128, 12, 128], F32),
    "k_iota16": ([128, 16], F32), "k_invf": ([128, 32], F32),
}


def build(S=4096, layers=(0, 1, 2, 3), dbg=False, do_mixer=True, do_peer=True, final_norm=True):
    NT = S // 128
    nc = bass.Bass("TRN2", target_bir_lowering=False)
    IN = {}
    IN["x"] = nc.dram_tensor("x", [S, D], F32, kind="ExternalInput").ap()
    IN["c"] = nc.dram_tensor("c", [1, D], F32, kind="ExternalInput").ap()
    IN["positions"] = nc.dram_tensor("positions", [1, S], I32, kind="ExternalInput").ap()
    for k, (shp, dt) in IN_SHAPES.items():
        IN[k] = nc.dram_tensor(k, shp, dt, kind="ExternalInput").ap()
    y_out = nc.dram_tensor("y", [S, D], F32, kind="ExternalOutput").ap()
    xs = [nc.dram_tensor("xs%d" % i, [S, D], F32,
                         kind=("ExternalOutput" if dbg else "Internal")).ap() for i in range(3)]

    uvb = nc.dram_tensor("uvb", [4 * NEXP, 2 * D], BF16, kind="Internal").ap()
    modrows = nc.dram_tensor("modrows", [4, 6 * D], F32, kind="Internal").ap()

    with contextlib.ExitStack() as st:
        s = Sched(nc, st)
        for h in s.semh.values():
            nc.gpsimd.sem_clear(h)
        nc.all_engine_barrier()

        uid = [0]

        def T(name, shape, dt=F32, stack=st):
            uid[0] += 1
            return stack.enter_context(nc.sbuf_tensor("%s_%d" % (name, uid[0]), shape, dt))

        def PS(name, shape, dt=F32, stack=st):
            uid[0] += 1
            return stack.enter_context(nc.psum_tensor("%s_%d" % (name, uid[0]), shape, dt))

        sink = [s]

        def S_():
            return sink[0]

        def mm(out, lhsT, rhs, start, stop, r, w):
            sink[0].op("pe", lambda e: e.matmul(out, lhsT=lhsT, rhs=rhs, start=start, stop=stop),
                 reads=r, writes=w, skip_same=True, nostop=(not stop))

        def tr(out, in_, ident, r, w):
            sink[0].op("pe", lambda e: e.transpose(out=out, in_=in_, identity=ident),
                 reads=r, writes=w, skip_same=True)

        def ld(out, in_, w, r=(), q="sp", slow=False):
            if slow:
                sink[0].dma(q, lambda e: e.dma_start(out=out, in_=in_, allow_slow_non_contiguous=True), reads=r, writes=w)
            else:
                sink[0].dma(q, lambda e: e.dma_start(out=out, in_=in_), reads=r, writes=w)

        ident_f = T("ident_f", [128, 128]); ident_b = T("ident_b", [128, 128], BF16)
        triu_b = T("triu_b", [128, 128], BF16); triu_f = T("triu_f", [128, 128])
        iota16 = T("iota16", [128, 16])
        ones_row = T("ones_row", [1, 128])
        modbc = [T("modbc%d" % i, [128, D]) for i in range(6)]
        A1, B1, G1, A2, B2, G2 = modbc
        cosT = T("cosT", [128, NT, 32]); sinT = T("sinT", [128, NT, 32]); nsinT = T("nsinT", [128, NT, 32])
        c_act = T("c_act", [128, 8])

        ld(ident_f[:], IN["k_ident"][:, :], ["ident_f"])
        ld(triu_f[:], IN["k_triu"][:, :], ["triu_f"])
        ld(iota16[:], IN["k_iota16"][:, :], ["iota16"])
        S_().op("dve", lambda e: e.tensor_copy(out=ident_b[:], in_=ident_f[:]), reads=["ident_f"], writes=["ident_b"])
        S_().op("dve", lambda e: e.tensor_copy(out=triu_b[:], in_=triu_f[:]), reads=["triu_f"], writes=["triu_b"])
        S_().op("dve", lambda e: e.memset(ones_row[:], 1.0), writes=["ones_row"])

        if do_peer:
            for l_ in layers:
                for c_ in range(16):
                    r0 = l_ * NEXP + c_ * 1024
                    S_().dma("pool", lambda e, l_=l_, c_=c_, r0=r0: e.dma_start(out=uvb[r0:r0 + 1024, 0:D], in_=IN["peer_u"][l_, c_ * 1024:(c_ + 1) * 1024, :]),
                          writes=[("uvb", l_)])
                    S_().dma("pool", lambda e, l_=l_, c_=c_, r0=r0: e.dma_start(out=uvb[r0:r0 + 1024, D:2 * D], in_=IN["peer_v"][l_, c_ * 1024:(c_ + 1) * 1024, :]),
                          writes=[("uvb", l_)])

        with contextlib.ExitStack() as ph:
            c_row = T("c_row", [1, D], stack=ph)
            one11 = T("one11", [1, 1], stack=ph)
            pc = PS("pc", [128, 8], stack=ph)
            ld(c_row[:], IN["c"][:, :], ["c_row"])
            S_().op("dve", lambda e: e.memset(one11[:], 1.0), writes=["one11"])
            for k in range(8):
                mm(pc[:, k:k + 1], c_row[0:1, k * 128:(k + 1) * 128], one11[0:1, 0:1], True, True,
                   ["c_row", "one11"], ["pc"])
            S_().op("act", lambda e: e.activation(out=c_act[:], in_=pc[:], func=AF.Silu), reads=["pc"], writes=["c_act"])
            pos_i = T("pos_i", [128, NT], I32, stack=ph)
            pos_f = T("pos_f", [128, NT], stack=ph)
            invf = T("invf", [128, 32], stack=ph)
            yy = T("yy", [128, NT, 32], stack=ph); y2 = T("y2", [128, NT, 32], stack=ph)
            ki = T("ki", [128, NT, 32], I32, stack=ph); kf = T("kf", [128, NT, 32], stack=ph)
            ld(pos_i[:], IN["positions"][0, :].rearrange("(n p) -> p n", p=128), ["pos_i"], slow=True)
            ld(invf[:], IN["k_invf"][:, :], ["invf"])
            S_().op("dve", lambda e: e.tensor_copy(out=pos_f[:], in_=pos_i[:]), reads=["pos_i"], writes=["pos_f"])
            S_().op("dve", lambda e: e.tensor_tensor(out=yy[:], in0=pos_f[:].unsqueeze(2).to_broadcast([128, NT, 32]),
                                                  in1=invf[:].unsqueeze(1).to_broadcast([128, NT, 32]), op=ALU.mult),
                 reads=["pos_f", "invf"], writes=["yy"])
            S_().op("dve", lambda e: e.tensor_copy(out=ki[:], in_=yy[:]), reads=["yy"], writes=["ki"])
            S_().op("dve", lambda e: e.tensor_copy(out=kf[:], in_=ki[:]), reads=["ki"], writes=["kf"])
            S_().op("dve", lambda e: e.tensor_tensor(out=y2[:], in0=yy[:], in1=kf[:], op=ALU.subtract), reads=["yy", "kf"], writes=["y2"])
            S_().op("act", lambda e: e.activation(out=sinT[:], in_=y2[:], func=AF.Sin, scale=2.0 * math.pi), reads=["y2"], writes=["sinT"])
            S_().op("dve", lambda e: e.tensor_scalar(out=nsinT[:], in0=sinT[:], scalar1=-1.0, scalar2=None, op0=ALU.mult), reads=["sinT"], writes=["nsinT"])
            S_().op("dve", lambda e: e.tensor_scalar(out=yy[:], in0=yy[:], scalar1=0.25, scalar2=None, op0=ALU.add), reads=["yy"], writes=["yy"])
            S_().op("dve", lambda e: e.tensor_copy(out=ki[:], in_=yy[:]), reads=["yy"], writes=["ki"])
            S_().op("dve", lambda e: e.tensor_copy(out=kf[:], in_=ki[:]), reads=["ki"], writes=["kf"])
            S_().op("dve", lambda e: e.tensor_tensor(out=y2[:], in0=yy[:], in1=kf[:], op=ALU.subtract), reads=["yy", "kf"], writes=["y2"])
            S_().op("act", lambda e: e.activation(out=cosT[:], in_=y2[:], func=AF.Sin, scale=2.0 * math.pi), reads=["y2"], writes=["cosT"])
            s.flush()

        def mod_rows(l):
            with contextlib.ExitStack() as ph:
                wt = [T("adaw%d" % i, [128, 8, 512], stack=ph) for i in range(2)]
                brow = T("adab", [1, 6 * D], stack=ph)
                mrow = T("mrow", [1, 6 * D], stack=ph)
                pm = [PS("pm%d" % i, [1, 512], stack=ph) for i in range(2)]
                ld(brow[:], IN["ada_b"][l:l + 1, :], ["adab"])
                for cb in range(12):
                    w = wt[cb % 2]; wk = "adaw%d" % (cb % 2); pk = "pm%d" % (cb % 2)
                    ld(w[:], IN["ada_w"][l, :, cb * 512:(cb + 1) * 512].rearrange("(k p) f -> p k f", p=128), [wk])
                    for k in range(8):
                        mm(pm[cb % 2][:, :], c_act[:, k:k + 1], w[:, k, :], k == 0, k == 7, ["c_act", wk], [pk])
                    S_().op("dve", lambda e, cb=cb: e.tensor_tensor(out=mrow[0:1, cb * 512:(cb + 1) * 512], in0=pm[cb % 2][:, :],
                                                                 in1=brow[0:1, cb * 512:(cb + 1) * 512], op=ALU.add),
                         reads=[pk, "adab"], writes=["mrow"])
                ld(modrows[l:l + 1, :], mrow[:], [("modrows", l)], r=["mrow"])
                S_().flush()

        def compute_mod(l):
            with contextlib.ExitStack() as ph:
                mrow = T("mrow", [1, 6 * D], stack=ph)
                nbc = [T("nbc%d" % i, [128, D], stack=ph) for i in range(2)]
                pb = [PS("pb%d" % i, [128, 512], stack=ph) for i in range(2)]
                ld(mrow[:], modrows[l:l + 1, :], ["mrow"], r=[("modrows", l)])
                ld(nbc[0][:], IN["norm_mix"][l, :].partition_broadcast(128), ["nbc0"])
                ld(nbc[1][:], IN["norm_ffn"][l, :].partition_broadcast(128), ["nbc1"])
                dst = {0: (B1, None), 1: (A1, 0), 2: (G1, None), 3: (B2, None), 4: (A2, 1), 5: (G2, None)}
                n = 0
                for i in range(6):
                    tgt, nb = dst[i]
                    for half in range(2):
                        pbk = "pb%d" % (n % 2); pbt = pb[n % 2]; n += 1
                        mm(pbt[:, :], ones_row[0:1, :], mrow[0:1, i * D + half * 512: i * D + (half + 1) * 512], True, True,
                           ["ones_row", "mrow"], [pbk])
                        o = tgt[:, half * 512:(half + 1) * 512]
                        if nb is None:
                            S_().op("act", lambda e, o=o, pbt=pbt: e.copy(out=o, in_=pbt[:, :]), reads=[pbk], writes=[tgt.name])
                        else:
                            nbt = nbc[nb][:, half * 512:(half + 1) * 512]
                            S_().op("dve", lambda e, o=o, pbt=pbt, nbt=nbt: e.scalar_tensor_tensor(
                                out=o, in0=pbt[:, :], scalar=1.0, in1=nbt, op0=ALU.add, op1=ALU.mult),
                                reads=[pbk, "nbc%d" % nb], writes=[tgt.name])
                S_().flush()

        for l_ in layers:
            mod_rows(l_)

        def load_x(xt, key, src, i):
            ld(xt[:], src[i * 128:(i + 1) * 128, :], [key], r=[(src.tensor.name, i)])

        def norm_mod(ph_t, xt, xkey, A, B, want_f32=False):
            junk, ss, rs, hf, hb = ph_t["junk"], ph_t["ss"], ph_t["rs"], ph_t["hf"], ph_t["hb"]
            S_().op("act", lambda e: e.activation(out=junk[:], in_=xt[:], func=AF.Square, accum_out=ss[:]),
                 reads=[xkey], writes=["junk", "ss"])
            S_().op("act", lambda e: e.activation(out=rs[:], in_=ss[:], func=AF.Sqrt, scale=1.0 / D, bias=EPS),
                 reads=["ss"], writes=["rs"])
            S_().op("dve", lambda e: e.reciprocal(out=rs[:], in_=rs[:]), reads=["rs"], writes=["rs"])
            S_().op("dve", lambda e: e.scalar_tensor_tensor(out=hf[:], in0=xt[:], scalar=rs[:, 0:1], in1=A[:],
                                                         op0=ALU.mult, op1=ALU.mult),
                 reads=[xkey, "rs", A.name], writes=["hf"])
            if want_f32:
                S_().op("pool", lambda e: e.tensor_tensor(out=hf[:], in0=hf[:], in1=B[:], op=ALU.add),
                     reads=["hf", B.name], writes=["hf"])
                S_().op("act", lambda e: e.copy(out=hb[:], in_=hf[:]), reads=["hf"], writes=["hb"])
            else:
                S_().op("pool", lambda e: e.tensor_tensor(out=hb[:], in0=hf[:], in1=B[:], op=ALU.add),
                     reads=["hf", B.name], writes=["hb"])

        def transpose8(hb, hbkey, tp, hT):
            for k in range(8):
                tr(tp[:, k, :], hb[:, k * 128:(k + 1) * 128], ident_b[:], [hbkey, "ident_b"], ["tp"])
            S_().op("act", lambda e: e.copy(out=hT[:], in_=tp[:]), reads=["tp"], writes=["hT"])

        def wload_bf16(dst, key, src, nk=8, parts=4):
            step = nk // parts
            for a in range(parts):
                ld(dst[:, a * step:(a + 1) * step, :],
                   src[a * step * 128:(a + 1) * step * 128, :].rearrange("(k p) f -> p k f", p=128), [key], q="pool")

        def residual_out(pso, psk, Gt, xres, xreskey, xo, dst, i, fin=None):
            for half in range(2):
                sl = slice(half * 512, (half + 1) * 512)
                S_().op("dve", lambda e, half=half, sl=sl: e.tensor_tensor(out=xo[:, sl], in0=pso[half][:, :], in1=Gt[:, sl], op=ALU.mult),
                     reads=[psk[half], Gt.name], writes=["xo"])
            S_().op("pool", lambda e: e.tensor_tensor(out=xo[:], in0=xo[:], in1=xres[:], op=ALU.add),
                 reads=["xo", xreskey], writes=["xo"])
            if fin is None:
                ld(dst[i * 128:(i + 1) * 128, :], xo[:], [(dst.tensor.name, i)], r=["xo"])
            else:
                junk, ss, rs, fnbc, yo = fin
                S_().op("act", lambda e: e.activation(out=junk[:], in_=xo[:], func=AF.Square, accum_out=ss[:]),
                     reads=["xo"], writes=["fss"])
                S_().op("act", lambda e: e.activation(out=rs[:], in_=ss[:], func=AF.Sqrt, scale=1.0 / D, bias=EPS),
                     reads=["fss"], writes=["frs"])
                S_().op("dve", lambda e: e.reciprocal(out=rs[:], in_=rs[:]), reads=["frs"], writes=["frs"])
                S_().op("dve", lambda e: e.scalar_tensor_tensor(out=yo[:], in0=xo[:], scalar=rs[:, 0:1], in1=fnbc[:],
                                                             op0=ALU.mult, op1=ALU.mult),
                     reads=["xo", "frs", "fnbc"], writes=["xo"])
                ld(dst[i * 128:(i + 1) * 128, :], yo[:], [(dst.tensor.name, i)], r=["xo"])

        def common_tiles(ph):
            d = {}
            d["junk"] = T("junk", [128, D], BF16, stack=ph)
            d["ss"] = T("ss", [128, 1], stack=ph)
            d["rs"] = T("rs", [128, 1], stack=ph)
            d["hf"] = T("hf", [128, D], stack=ph)
            d["hb"] = T("hb", [128, D], BF16, stack=ph)
            d["hT"] = T("hT", [128, 8, 128], BF16, stack=ph)
            d["xo"] = T("xo", [128, D], stack=ph)
            d["xt"] = [T("xt%d" % i, [128, D], stack=ph) for i in range(2)]
            return d

        def even_mixer(l, src, dst):
            e_ = l // 2
            with contextlib.ExitStack() as ph:
                ct = common_tiles(ph)
                Win = T("Win", [128, 8, 1536], BF16, stack=ph)
                Wout = T("Wout", [128, 8, D], BF16, stack=ph)
                wsraw = T("wsraw", [128, 4, 128], stack=ph)
                wsT = T("wsT", [128, 4, 128], BF16, stack=ph)
                bsrow = T("bsrow", [1, 512], stack=ph)
                wpool = T("wpool", [128, 4, 128], BF16, stack=ph)
                gvbc = T("gvbc", [128, 512], stack=ph)
                lscol = T("lscol", [128, 4], stack=ph)
                poolm = T("poolm", [128, 12, 128], stack=ph)
                uTs = T("uTs", [128, 4, 128], stack=ph)
                vs = T("vs", [128, 512], stack=ph)
                vn = T("vn", [128, 512], BF16, stack=ph)
                bst = T("bst", [128, 6], stack=ph); bag = T("bag", [128, 2], stack=ph); sd = T("sd", [128, 1], stack=ph)
                pcur = [T("pcur%d" % i, [128, 512], stack=ph) for i in range(2)]
                pooledT = T("pooledT", [128, 4, 128], BF16, stack=ph)
                yabT = T("yabT", [128, 8, 128], BF16, stack=ph)
                tp = PS("tp", [128, 8, 128], BF16, stack=ph)
                psU = PS("psU", [128, 4, 128], stack=ph)
                psV = PS("psV", [128, 512], stack=ph)
                psP = PS("psP", [128, 512], stack=ph)
                psS = PS("psS", [128, 4, 128], stack=ph)
                psQ = PS("psQ", [128, 4, 128], stack=ph)
                pso = [PS("pso%d" % i, [128, 512], stack=ph) for i in range(2)]

                wload_bf16(Win, "Win", IN["ev_w_in"][e_])
                wload_bf16(Wout, "Wout", IN["ev_w_out"][e_])
                ld(wsraw[:], IN["ev_w_s"][e_].rearrange("g t s -> t g s"), ["wsraw"])
                ld(bsrow[:], IN["ev_b_s"][e_:e_ + 1, :], ["bsrow"])
                ld(wpool[:], IN["ev_w_pool"][e_].rearrange("g c d -> c g d"), ["wpool"], q="pool")
                ld(gvbc[:], IN["ev_g_v"][e_, :].partition_broadcast(128), ["gvbc"])
                ld(lscol[:], IN["ev_pool_scale"][e_, :].rearrange("(g d) -> d g", d=128), ["lscol"], slow=True)
                ld(poolm[:], IN["k_poolm"][:, :, :], ["poolm"])
                for g in range(4):
                    tr(psS[:, g, :], wsraw[:, g, :], ident_f[:], ["wsraw", "ident_f"], ["psS"])
                S_().op("dve", lambda e: e.tensor_tensor(out=wsT[:], in0=psS[:], in1=triu_f[:].unsqueeze(1).to_broadcast([128, 4, 128]), op=ALU.mult),
                     reads=["psS", "triu_f"], writes=["wsT"])

                load_x(ct["xt"][0], "xt0", src, 0)
                for i in range(NT):
                    xt = ct["xt"][i % 2]; xk = "xt%d" % (i % 2)
                    if i + 1 < NT:
                        load_x(ct["xt"][(i + 1) % 2], "xt%d" % ((i + 1) % 2), src, i + 1)
                    norm_mod(ct, xt, xk, A1, B1)
                    hb, hT = ct["hb"], ct["hT"]
                    transpose8(hb, "hb", tp, hT)
                    for fc in range(4):
                        for k in range(8):
                            mm(psU[:, fc, :], Win[:, k, fc * 128:(fc + 1) * 128], hT[:, k, :], k == 0, k == 7, ["Win", "hT"], ["psU"])
                    S_().op("act", lambda e: e.activation(out=uTs[:], in_=psU[:], func=AF.Gelu), reads=["psU"], writes=["uTs"])
                    for k in range(8):
                        mm(psV[:, :], hT[:, k, :], Win[:, k, 512:1024], k == 0, k == 7, ["Win", "hT"], ["psV"])
                    S_().op("act", lambda e: e.activation(out=vs[:], in_=psV[:], func=AF.Gelu), reads=["psV"], writes=["vs"])
                    S_().op("dve", lambda e: e.bn_stats(out=bst[:], in_=vs[:]), reads=["vs"], writes=["bst"])
                    S_().op("dve", lambda e: e.bn_aggr(out=bag[:], in_=bst[:]), reads=["bst"], writes=["bag"])
                    S_().op("act", lambda e: e.activation(out=sd[:], in_=bag[:, 1:2], func=AF.Sqrt, scale=1.0, bias=EPS), reads=["bag"], writes=["sd"])
                    S_().op("dve", lambda e: e.reciprocal(out=sd[:], in_=sd[:]), reads=["sd"], writes=["sd"])
                    S_().op("dve", lambda e: e.tensor_scalar(out=vs[:], in0=vs[:], scalar1=bag[:, 0:1], scalar2=sd[:, 0:1],
                                                          op0=ALU.subtract, op1=ALU.mult), reads=["vs", "bag", "sd"], writes=["vs"])
                    S_().op("pool", lambda e: e.tensor_tensor(out=vn[:], in0=vs[:], in1=gvbc[:], op=ALU.mult), reads=["vs", "gvbc"], writes=["vn"])
                    pc_ = pcur[i % 2]; pck = "pcur%d" % (i % 2); pp_ = pcur[(i + 1) % 2]; ppk = "pcur%d" % ((i + 1) % 2)
                    for k in range(8):
                        mm(psP[:, :], hT[:, k, :], Win[:, k, 1024:1536], k == 0, k == 7, ["Win", "hT"], ["psP"])
                    S_().op("act", lambda e, pc_=pc_: e.copy(out=pc_[:], in_=psP[:]), reads=["psP"], writes=[pck])
                    for g in range(4):
                        mm(psS[:, g, :], vn[:, g * 128:(g + 1) * 128], wsT[:, g, :], True, False, ["vn", "wsT"], ["psS"])
                        mm(psS[:, g, :], ones_row[0:1, :], bsrow[0:1, g * 128:(g + 1) * 128], False, True, ["ones_row", "bsrow"], ["psS"])
                    S_().op("dve", lambda e: e.tensor_tensor(out=yabT[:, 0:4, :], in0=psS[:], in1=uTs[:], op=ALU.mult),
                         reads=["psS", "uTs"], writes=["yabT"])
                    for g in range(4):
                        gi = (4 + g) if i == 0 else g
                        mm(psQ[:, g, :], pc_[:, g * 128:(g + 1) * 128], poolm[:, gi, :], True, i == 0, [pck, "poolm"], ["psQ"])
                        if i > 0:
                            mm(psQ[:, g, :], pp_[:, g * 128:(g + 1) * 128], poolm[:, 8 + g, :], False, True, [ppk, "poolm"], ["psQ"])
                    S_().op("act", lambda e: e.copy(out=pooledT[:], in_=psQ[:]), reads=["psQ"], writes=["pooledT"])
                    for g in range(4):
                        mm(psU[:, g, :], wpool[:, g, :], pooledT[:, g, :], True, True, ["wpool", "pooledT"], ["psU"])
                    for g in range(4):
                        S_().op("dve", lambda e, g=g: e.tensor_scalar(out=yabT[:, 4 + g, :], in0=psU[:, g, :], scalar1=lscol[:, g:g + 1],
                                                                   scalar2=None, op0=ALU.mult), reads=["psU", "lscol"], writes=["yabT"])
                    for half in range(2):
                        for fc in range(8):
                            mm(pso[half][:, :], yabT[:, fc, :], Wout[:, fc, half * 512:(half + 1) * 512], fc == 0, fc == 7,
                               ["yabT", "Wout"], ["pso%d" % half])
                    residual_out(pso, ["pso0", "pso1"], G1, xt, xk, ct["xo"], dst, i)
                s.flush()

        def odd_mixer(l, hsrc, rsrc, dst, hg):
            o_ = l // 2
            lam_init = 0.8 - 0.6 * math.exp(-0.3 * l)
            with contextlib.ExitStack() as ph:
                ct = common_tiles(ph)
                Wq = T("Wq_", [128, 8, 512], BF16, stack=ph)
                Wk = T("Wk_", [128, 8, 512], BF16, stack=ph)
                Wv = T("Wv_", [128, 8, 512], BF16, stack=ph)
                Wout = T("Wout", [128, 4, D], BF16, stack=ph)
                KT = T("KT", [128, 4, S], BF16, stack=ph)
                V = T("V", [128, NT, 4, 130], BF16, stack=ph)
                QT = T("QT", [128, 4, 128], BF16, stack=ph)
                lamt = T("lamt", [128, 4, 64], stack=ph)
                lj = T("lj", [128, 64], stack=ph); ld1 = T("ld1", [128, 2], stack=ph)
                nlam = T("nlam", [128, 1], stack=ph)
                gsub = T("gsub", [128, 128], stack=ph)
                xr = [T("xr%d" % i, [128, D], stack=ph) for i in range(2)] if rsrc is not hsrc else None
                ropeA = T("ropeA", [128, 512], stack=ph); ropeB = T("ropeB", [128, 512], stack=ph)
                qr = T("qr", [128, 512], BF16, stack=ph); kr = T("kr", [128, 512], BF16, stack=ph)
                PT = [T("PT%d" % i, [128, 4, 128], BF16, stack=ph) for i in range(3)]
                mhalf = T("mhalf", [128, 1], stack=ph)
                rz = T("rz", [128, 2], stack=ph); of = T("of", [128, 128], stack=ph); oj = T("oj", [128, 128], stack=ph)
                oss = T("oss", [128, 1], stack=ph)
                Oall = T("Oall", [128, 4, 128], BF16, stack=ph)
                oT = T("oT", [128, 4, 128], BF16, stack=ph)
                tp = PS("tp", [128, 8, 128], BF16, stack=ph)
                psq = PS("psq", [128, 512], stack=ph); psk = PS("psk", [128, 512], stack=ph); psv = PS("psv", [128, 4, 128], stack=ph)
                pss = [PS("pss%d" % i, [128, 4, 128], stack=ph) for i in range(2)]
                pss3 = [pss[0], pss[1], psv]; pss3k = ["pss0", "pss1", "psv"]
                S_().op("dve", lambda e: e.memset(mhalf[:], -0.5), writes=["mhalf"])
                pso_ = [PS("psoh%d" % i, [128, 2, 130], stack=ph) for i in range(2)]

                c0 = hg * 512
                wload_bf16(Wq, "Wq_", IN["od_w_in"][o_][:, c0:c0 + 512])
                wload_bf16(Wk, "Wk_", IN["od_w_in"][o_][:, 1024 + c0:1024 + c0 + 512])
                wload_bf16(Wv, "Wv_", IN["od_w_in"][o_][:, 2048 + c0:2048 + c0 + 512])
                wload_bf16(Wout, "Wout", IN["od_w_out"][o_][hg * 512:(hg + 1) * 512, :], nk=4, parts=2)
                for j, nm in enumerate(("od_lam_q1", "od_lam_k1", "od_lam_q2", "od_lam_k2")):
                    ld(lamt[:, j, :], IN[nm][o_, :].partition_broadcast(128), ["lamt"])
                ld(gsub[:], IN["od_g_sub"][o_, :].partition_broadcast(128), ["gsub"])
                S_().op("dve", lambda e: e.tensor_scalar(out=gsub[:], in0=gsub[:], scalar1=1.0 - lam_init, scalar2=None, op0=ALU.mult),
                     reads=["gsub"], writes=["gsub"])
                for j in range(2):
                    S_().op("dve", lambda e, j=j: e.scalar_tensor_tensor(out=lj[:], in0=lamt[:, 2 * j, :], scalar=1.0, in1=lamt[:, 2 * j + 1, :],
                                                                      op0=ALU.mult, op1=ALU.mult, accum_out=ld1[:, j:j + 1]),
                         reads=["lamt"], writes=["lj", "ld1"])
                S_().op("act", lambda e: e.activation(out=ld1[:], in_=ld1[:], func=AF.Exp), reads=["ld1"], writes=["ld1"])
                S_().op("dve", lambda e: e.tensor_tensor(out=nlam[:], in0=ld1[:, 1:2], in1=ld1[:, 0:1], op=ALU.subtract), reads=["ld1"], writes=["nlam"])
                S_().op("dve", lambda e: e.tensor_scalar(out=nlam[:], in0=nlam[:], scalar1=-lam_init, scalar2=None, op0=ALU.add), reads=["nlam"], writes=["nlam"])
                S_().op("dve", lambda e: e.memset(V[:, :, :, 128:130], 1.0), writes=[("V", i_) for i_ in range(NT)])

                def rope(ps, pskey, out, outkey, i):
                    v4 = lambda ap: ap.rearrange("p (g two i) -> p g two i", g=8, two=2)
                    cb = cosT[:, i, :].unsqueeze(1).unsqueeze(1).to_broadcast([128, 8, 2, 32])
                    sb = sinT[:, i, :].unsqueeze(1).to_broadcast([128, 8, 32])
                    nsb = nsinT[:, i, :].unsqueeze(1).to_broadcast([128, 8, 32])
                    S_().op("dve", lambda e: e.tensor_tensor(out=v4(ropeA[:]), in0=v4(ps[:, :]), in1=cb, op=ALU.mult),
                         reads=[pskey, "cosT"], writes=["ropeA"])
                    S_().op("dve", lambda e: e.tensor_tensor(out=v4(ropeB[:])[:, :, 0, :], in0=v4(ps[:, :])[:, :, 1, :], in1=nsb, op=ALU.mult),
                         reads=[pskey, "nsinT"], writes=["ropeB"])
                    S_().op("dve", lambda e: e.tensor_tensor(out=v4(ropeB[:])[:, :, 1, :], in0=v4(ps[:, :])[:, :, 0, :], in1=sb, op=ALU.mult),
                         reads=[pskey, "sinT"], writes=["ropeB"])
                    S_().op("pool", lambda e: e.tensor_tensor(out=out[:], in0=ropeA[:], in1=ropeB[:], op=ALU.add),
                         reads=["ropeA", "ropeB"], writes=[outkey])

                QTs = [QT, T("QTb", [128, 4, 128], BF16, stack=ph)]

                def stageA(i):
                    o = S_()
                    xt = ct["xt"][i % 2]; xk = "xt%d" % (i % 2)
                    load_x(xt, xk, hsrc, i)
                    if xr is not None:
                        load_x(xr[i % 2], "xr%d" % (i % 2), rsrc, i)
                    norm_mod(ct, xt, xk, A1, B1)
                    hb, hT = ct["hb"], ct["hT"]
                    transpose8(hb, "hb", tp, hT)
                    for k in range(8):
                        mm(psq[:, :], hT[:, k, :], Wq[:, k, :], k == 0, k == 7, ["Wq_", "hT"], ["psq"])
                    for k in range(8):
                        mm(psk[:, :], hT[:, k, :], Wk[:, k, :], k == 0, k == 7, ["Wk_", "hT"], ["psk"])
                    rope(psq, "psq", qr, "qr", i)
                    for k in range(8):
                        mm(psq[:, :], hT[:, k, :], Wv[:, k, :], k == 0, k == 7, ["Wv_", "hT"], ["psq"])
                    rope(psk, "psk", kr, "kr", i)
                    o.op("act", lambda e, i=i: e.copy(out=V[:, i, :, 0:128], in_=psq[:, :].rearrange("p (h d) -> p h d", h=4)), reads=["psq"], writes=[("V", i)])
                    for hh in range(4):
                        tr(tp[:, hh, :], qr[:, hh * 128:(hh + 1) * 128], ident_b[:], ["qr", "ident_b"], ["tp"])
                    for hh in range(4):
                        tr(tp[:, 4 + hh, :], kr[:, hh * 128:(hh + 1) * 128], ident_b[:], ["kr", "ident_b"], ["tp"])
                    qt_ = QTs[i % 2]
                    o.op("act", lambda e: e.copy(out=qt_[:], in_=tp[:, 0:4, :]), reads=["tp"], writes=["QT%d" % (i % 2)])
                    o.op("act", lambda e, i=i: e.copy(out=KT[:, :, i * 128:(i + 1) * 128], in_=tp[:, 4:8, :]), reads=["tp"], writes=[("KT", i)])

                def stageB(i, filler):
                    o = S_()
                    xt = ct["xt"][i % 2]; xk = "xt%d" % (i % 2)
                    qt_ = QTs[i % 2]; qtk = "QT%d" % (i % 2)
                    items = []
                    for hh in range(4):
                        for m in range(2):
                            for g0 in range(0, i + 1, 4):
                                items.append((hh, m, g0, list(range(g0, min(g0 + 4, i + 1)))))

                    def emitS(it, sl_):
                        hh, m, g0, kbs = it
                        rows = slice(m * 64, (m + 1) * 64)
                        bank = pss3[sl_]; bk = pss3k[sl_]
                        pt = PT[sl_]; ptk = "PT%d" % sl_
                        for j, kb in enumerate(kbs):
                            mm(bank[:, j, :], KT[rows, hh, kb * 128:(kb + 1) * 128], qt_[rows, hh, :], True, True, [("KT", kb), qtk], [bk])
                        n = len(kbs)
                        o.op("act", lambda e, pt=pt, bank=bank, n=n: e.activation(out=pt[:, 0:n, :], in_=bank[:, 0:n, :], func=AF.Exp, scale=0.125),
                             reads=[bk], writes=[ptk])
                        if i in kbs:
                            jd = i - g0
                            o.op("pool", lambda e, pt=pt, jd=jd: e.tensor_tensor(out=pt[:, jd, :], in0=pt[:, jd, :], in1=triu_b[:], op=ALU.mult),
                                 reads=[ptk, "triu_b"], writes=[ptk])

                    def emitAV(it, sl_):
                        hh, m, g0, kbs = it
                        po = pso_[hh % 2]; pok = "psoh%d" % (hh % 2)
                        pt = PT[sl_]; ptk = "PT%d" % sl_
                        for j, kb in enumerate(kbs):
                            mm(po[:, m, 0:129], pt[:, j, :], V[:, kb, hh, 0:129], kb == 0, kb == i, [ptk, ("V", kb)], [pok])
                        if m == 1 and kbs[-1] == i:
                            o.op("dve", lambda e, po=po: e.reciprocal(out=rz[:], in_=po[:, :, 128]), reads=[pok], writes=["rz"])
                            o.op("dve", lambda e: e.tensor_tensor(out=rz[:, 1:2], in0=rz[:, 1:2], in1=nlam[:], op=ALU.mult), reads=["rz", "nlam"], writes=["rz"])
                            o.op("dve", lambda e, po=po: e.tensor_scalar(out=of[:], in0=po[:, 0, 0:128], scalar1=rz[:, 0:1], scalar2=None, op0=ALU.mult),
                                 reads=[pok, "rz"], writes=["of"])
                            o.op("dve", lambda e, po=po: e.scalar_tensor_tensor(out=of[:], in0=po[:, 1, 0:128], scalar=rz[:, 1:2], in1=of[:], op0=ALU.mult, op1=ALU.add),
                                 reads=[pok, "rz", "of"], writes=["of"])
                            o.op("dve", lambda e: e.scalar_tensor_tensor(out=oj[:], in0=of[:], scalar=1.0, in1=of[:], op0=ALU.mult, op1=ALU.mult, accum_out=oss[:]),
                                 reads=["of"], writes=["oj", "oss"])
                            o.op("dve", lambda e: e.tensor_scalar(out=oss[:], in0=oss[:], scalar1=1.0 / 128, scalar2=EPS, op0=ALU.mult, op1=ALU.add),
                                 reads=["oss"], writes=["oss"])
                            o.op("pool", lambda e: e.tensor_tensor(out=oss[:], in0=oss[:], in1=mhalf[:], op=ALU.pow), reads=["oss", "mhalf"], writes=["oss"])
                            o.op("dve", lambda e, hh=hh: e.scalar_tensor_tensor(out=Oall[:, hh, :], in0=of[:], scalar=oss[:, 0:1], in1=gsub[:], op0=ALU.mult, op1=ALU.mult),
                                 reads=["of", "oss", "gsub"], writes=["Oall"])

                    prev = None
                    for n_, it in enumerate(items):
                        emitS(it, n_ % 3)
                        if prev is not None:
                            emitAV(*prev)
                        prev = (it, n_ % 3)
                        if FILL_MODE == 0 or it[3][-1] == i:
                            filler(len(items) if FILL_MODE == 0 else 8)
                    emitAV(*prev)
                    for hh in range(4):
                        tr(tp[:, hh, :], Oall[:, hh, :], ident_b[:], ["Oall", "ident_b"], ["tp"])
                    o.op("act", lambda e: e.copy(out=oT[:], in_=tp[:, 0:4, :]), reads=["tp"], writes=["oT"])
                    pso = [psq, psk]
                    for half in range(2):
                        for hh in range(4):
                            mm(pso[half][:, :], oT[:, hh, :], Wout[:, hh, half * 512:(half + 1) * 512], hh == 0, hh == 3,
                               ["oT", "Wout"], [["psq", "psk"][half]])
                    if xr is not None:
                        residual_out(pso, ["psq", "psk"], G1, xr[i % 2], "xr%d" % (i % 2), ct["xo"], dst, i)
                    else:
                        residual_out(pso, ["psq", "psk"], G1, xt, xk, ct["xo"], dst, i)

                stageA(0)
                for i in range(NT):
                    nxt = Deferred()
                    if i + 1 < NT:
                        sink[0] = nxt
                        stageA(i + 1)
                        sink[0] = s
                    tot = len(nxt.q)
                    if FILL_MODE == 2:
                        import os
                        NPRE = int(os.environ.get("NPRE", "0"))
                        done = [False]

                        def f2(nit):
                            if not done[0]:
                                nxt.run(s, NPRE); done[0] = True
                        stageB(i, f2)
                    else:
                        stageB(i, lambda nit: nxt.run(s, (tot + nit - 1) // nit))
                    nxt.run(s, None)
                s.flush()

        def peer_pass(l, src, dst, fin):
            with contextlib.ExitStack() as ph:
                junk = T("junk", [128, D], BF16, stack=ph)
                ss = T("ss", [128, 1], stack=ph); rs = T("rs", [128, 1], stack=ph)
                hf = T("hf", [128, D], stack=ph); hb = T("hb", [128, D], BF16, stack=ph)
                hT = T("hT", [128, 8, 128], BF16, stack=ph)
                xo = T("xo", [128, D], stack=ph)
                xts = [T("xt%d" % i, [128, D], stack=ph) for i in range(2)]
                Wq = T("Wqp", [128, 8, 2048], BF16, stack=ph)
                skT = T("skT", [128, 16, 128], stack=ph)
                qT = T("qT", [128, 16, 128], stack=ph)
                sc = T("sc", [128, 16, 128], stack=ph)
                sc2 = T("sc2", [128, 16, 128], stack=ph)
                skraw = sc2
                sv = T("sv", [128, 16, 16], stack=ph)
                si = T("si", [128, 16, 16], U32, stack=ph)
                sif = T("sif", [128, 16, 16], stack=ph)
                cand = sc[:].rearrange("p (h a) n -> p h (a n)", h=8)
                cand2 = sc2[:].rearrange("p (h a) n -> p h (a n)", h=8)
                fv = T("fv", [128, 8, 16], stack=ph)
                fpos = T("fpos", [128, 8, 16], U32, stack=ph)
                fa = T("fa", [128, 8, 16], U32, stack=ph); fb = T("fb", [128, 8, 16], U32, stack=ph)
                faf = T("faf", [128, 8, 16], stack=ph); fbf = T("fbf", [128, 8, 16], stack=ph)
                oh = sc2[:].rearrange("p (h a) (b c) -> p h (a b) c", h=8, c=16)
                Ii = T("Ii", [128, 8, 16], stack=ph); Jj = T("Jj", [128, 8, 16], stack=ph)
                eif = T("eif", [128, 128], stack=ph)
                eidxs = [T("eidx%d" % i, [128, 128], U32, stack=ph) for i in range(2)]
                ge = T("ge", [128, 8, 16], stack=ph); gz = T("gz", [128, 8], stack=ph)
                gates = [T("gate%d" % i, [128, 128], stack=ph) for i in range(2)]
                act = T("act", [128, 128], stack=ph)
                wgt = T("wgt", [128, 128], stack=ph)
                NB = 16
                UV = [T("UV%d" % i, [128, 2 * D], BF16, stack=ph) for i in range(NB)]
                diag = [T("diag%d" % i, [128, 128], BF16, stack=ph) for i in range(2)]
                dj = T("dj", [128, D], BF16, stack=ph)
                fin_t = None
                if fin:
                    fnbc = T("fnbc", [128, D], stack=ph)
                    fss = T("fss", [128, 1], stack=ph); frs = T("frs", [128, 1], stack=ph)
                    ld(fnbc[:], IN["final_norm"][0, :].partition_broadcast(128), ["fnbc"])
                    fin_t = (dj, fss, frs, fnbc, xo)
                tp = PS("tp", [128, 8, 128], BF16, stack=ph)
                psx = [PS("psx%d" % i, [128, 4, 128], stack=ph) for i in range(2)]
                pso = [PS("psop%d" % i, [128, 512], stack=ph) for i in range(2)]
                hps = [PS("hps%d" % i, [128, D], BF16, stack=ph) for i in range(2)]

                wload_bf16(Wq, "Wqp", IN["peer_w_q"][l])
                ld(skraw[:], IN["peer_sub_keys"][l].rearrange("g n k -> n g k"), ["sc2_%d" % g for g in range(16)])
                for r in range(4):
                    for c_ in range(4):
                        tr(psx[r % 2][:, c_, :], skraw[:, r * 4 + c_, :], ident_f[:], ["sc2_%d" % (r * 4 + c_), "ident_f"], ["psx%d" % (r % 2)])
                    S_().op("act", lambda e, r=r: e.copy(out=skT[:, r * 4:(r + 1) * 4, :], in_=psx[r % 2][:]), reads=["psx%d" % (r % 2)], writes=["skT"])

                def stageA(i):
                    xt = xts[i % 2]; xk = "xt%d" % (i % 2)
                    eidx = eidxs[i % 2]; ek = "eidx%d" % (i % 2)
                    gate = gates[i % 2]; gk = "gate%d" % (i % 2)
                    hp_ = hps[i % 2]; hpk = "hps%d" % (i % 2)
                    o = S_()
                    load_x(xt, xk, src, i)
                    o.op("act", lambda e: e.activation(out=junk[:], in_=xt[:], func=AF.Square, accum_out=ss[:]), reads=[xk], writes=["junk", "ss"])
                    o.op("act", lambda e: e.activation(out=rs[:], in_=ss[:], func=AF.Sqrt, scale=1.0 / D, bias=EPS), reads=["ss"], writes=["rs"])
                    o.op("dve", lambda e: e.reciprocal(out=rs[:], in_=rs[:]), reads=["rs"], writes=["rs"])
                    o.op("dve", lambda e: e.scalar_tensor_tensor(out=hf[:], in0=xt[:], scalar=rs[:, 0:1], in1=A2[:], op0=ALU.mult, op1=ALU.mult),
                         reads=[xk, "rs", A2.name], writes=["hf"])
                    o.op("pool", lambda e: e.tensor_tensor(out=hb[:], in0=hf[:], in1=B2[:], op=ALU.add), reads=["hf", B2.name], writes=["hb"])
                    for k in range(8):
                        tr(tp[:, k, :], hb[:, k * 128:(k + 1) * 128], ident_b[:], ["hb", "ident_b"], ["tp"])
                    o.op("act", lambda e: e.copy(out=hT[:], in_=tp[:]), reads=["tp"], writes=["hT"])
                    for k in range(8):
                        tr(hp_[:, k * 128:(k + 1) * 128], hT[:, k, :], ident_b[:], ["hT", "ident_b"], [hpk])
                    for r in range(4):
                        pq = psx[r % 2]; pqk = "psx%d" % (r % 2)
                        for c_ in range(4):
                            hp = r * 4 + c_
                            for k in range(8):
                                mm(pq[:, c_, :], Wq[:, k, hp * 128:(hp + 1) * 128], hT[:, k, :], k == 0, k == 7, ["Wqp", "hT"], [pqk])
                        o.op("act", lambda e, r=r, pq=pq: e.copy(out=qT[:, r * 4:(r + 1) * 4, :], in_=pq[:]), reads=[pqk], writes=["qT"])
                    for r in range(4):
                        pz = psx[r % 2]; pzk = "psx%d" % (r % 2)
                        for c_ in range(4):
                            hp = r * 4 + c_
                            mm(pz[:, c_, :], qT[:, hp, :], skT[:, hp, :], True, True, ["qT", "skT"], [pzk])
                        o.op("act", lambda e, r=r, pz=pz: e.copy(out=sc[:, r * 4:(r + 1) * 4, :], in_=pz[:]), reads=[pzk],
                             writes=["sc_%d" % g for g in range(r * 4, r * 4 + 4)])
                    G16 = range(16)
                    SCK = ["sc_%d" % g for g in G16]; SC2K = ["sc2_%d" % g for g in G16]
                    SVA = ["sva%d" % g for g in G16]; SVB = ["svb%d" % g for g in G16]
                    SIA = ["sia%d" % g for g in G16]; SIB = ["sib%d" % g for g in G16]
                    for g in G16:
                        o.op("dve", lambda e, g=g: e.max(out=sv[:, g, 0:8], in_=sc[:, g, :]), reads=[SCK[g]], writes=[SVA[g]])
                    for g in G16:
                        o.op("dve", lambda e, g=g: e.max_index(out=si[:, g, 0:8], in_max=sv[:, g, 0:8], in_values=sc[:, g, :]), reads=[SCK[g], SVA[g]], writes=[SIA[g]])
                    for g in G16:
                        o.op("dve", lambda e, g=g: e.match_replace(out=sc2[:, g, :], in_to_replace=sv[:, g, 0:8], in_values=sc[:, g, :], imm_value=-1e30),
                             reads=[SCK[g], SVA[g]], writes=[SC2K[g]])
                    for g in G16:
                        o.op("dve", lambda e, g=g: e.max(out=sv[:, g, 8:16], in_=sc2[:, g, :]), reads=[SC2K[g]], writes=[SVB[g]])
                    for g in G16:
                        o.op("dve", lambda e, g=g: e.max_index(out=si[:, g, 8:16], in_max=sv[:, g, 8:16], in_values=sc2[:, g, :]), reads=[SC2K[g], SVB[g]], writes=[SIB[g]])
                    o.op("dve", lambda e: e.tensor_copy(out=sif[:], in_=si[:]), reads=SIA + SIB, writes=["sif"])
                    sv4 = sv[:].rearrange("p (h two) k -> p h two k", two=2)
                    sif4 = sif[:].rearrange("p (h two) k -> p h two k", two=2)
                    o.op("dve", lambda e: e.tensor_tensor(out=cand.rearrange("p h (a b) -> p h a b", a=16),
                                                          in0=sv4[:, :, 0, :].unsqueeze(3).to_broadcast([128, 8, 16, 16]),
                                                          in1=sv4[:, :, 1, :].unsqueeze(2).to_broadcast([128, 8, 16, 16]), op=ALU.add),
                         reads=SVA + SVB, writes=SCK)
                    H8 = range(8)
                    CK = [[SCK[2 * h], SCK[2 * h + 1]] for h in H8]; C2K = [[SC2K[2 * h], SC2K[2 * h + 1]] for h in H8]
                    FVA = ["fva%d" % h for h in H8]; FVB = ["fvb%d" % h for h in H8]
                    FPA = ["fpa%d" % h for h in H8]; FPB = ["fpb%d" % h for h in H8]
                    for h in H8:
                        o.op("dve", lambda e, h=h: e.max(out=fv[:, h, 0:8], in_=cand[:, h, :]), reads=CK[h], writes=[FVA[h]])
                    for h in H8:
                        o.op("dve", lambda e, h=h: e.max_index(out=fpos[:, h, 0:8], in_max=fv[:, h, 0:8], in_values=cand[:, h, :]), reads=CK[h] + [FVA[h]], writes=[FPA[h]])
                    for h in H8:
                        o.op("dve", lambda e, h=h: e.match_replace(out=cand2[:, h, :], in_to_replace=fv[:, h, 0:8], in_values=cand[:, h, :], imm_value=-1e30),
                             reads=CK[h] + [FVA[h]], writes=C2K[h])
                    for h in H8:
                        o.op("dve", lambda e, h=h: e.max(out=fv[:, h, 8:16], in_=cand2[:, h, :]), reads=C2K[h], writes=[FVB[h]])
                    for h in H8:
                        o.op("dve", lambda e, h=h: e.max_index(out=fpos[:, h, 8:16], in_max=fv[:, h, 8:16], in_values=cand2[:, h, :]), reads=C2K[h] + [FVB[h]], writes=[FPB[h]])
                    FV = FVA + FVB; FP = FPA + FPB
                    o.op("dve", lambda e: e.tensor_tensor(out=ge[:], in0=fv[:], in1=fv[:, :, 0:1].to_broadcast([128, 8, 16]), op=ALU.subtract),
                         reads=FV, writes=["ge"])
                    o.op("act", lambda e: e.activation(out=ge[:], in_=ge[:], func=AF.Exp), reads=["ge"], writes=["ge"])
                    o.op("dve", lambda e: e.tensor_reduce(out=gz[:], in_=ge[:], axis=AX.X, op=ALU.add), reads=["ge"], writes=["gz"])
                    o.op("dve", lambda e: e.reciprocal(out=gz[:], in_=gz[:]), reads=["gz"], writes=["gz"])
                    o.op("dve", lambda e: e.tensor_tensor(out=gate[:].rearrange("p (h k) -> p h k", h=8), in0=ge[:],
                                                          in1=gz[:].unsqueeze(2).to_broadcast([128, 8, 16]), op=ALU.mult),
                         reads=["ge", "gz"], writes=[gk])
                    o.op("dve", lambda e: e.tensor_single_scalar(out=fa[:], in_=fpos[:], scalar=4, op=ALU.logical_shift_right), reads=FP, writes=["fa"])
                    o.op("dve", lambda e: e.tensor_single_scalar(out=fb[:], in_=fpos[:], scalar=15, op=ALU.bitwise_and), reads=FP, writes=["fb"])
                    o.op("dve", lambda e: e.tensor_copy(out=faf[:], in_=fa[:]), reads=["fa"], writes=["faf"])
                    o.op("dve", lambda e: e.tensor_copy(out=fbf[:], in_=fb[:]), reads=["fb"], writes=["fbf"])
                    io4 = iota16[:].unsqueeze(1).unsqueeze(1).to_broadcast([128, 8, 16, 16])
                    for (srcf, sk_, half, dstt, dk) in ((faf, "faf", 0, Ii, "Ii"), (fbf, "fbf", 1, Jj, "Jj")):
                        o.op("dve", lambda e, srcf=srcf: e.tensor_tensor(out=oh, in0=srcf[:].unsqueeze(3).to_broadcast([128, 8, 16, 16]), in1=io4, op=ALU.is_equal),
                             reads=[sk_, "iota16"], writes=SC2K)
                        o.op("dve", lambda e, half=half: e.tensor_tensor(out=oh, in0=oh, in1=sif4[:, :, half, :].unsqueeze(2).to_broadcast([128, 8, 16, 16]), op=ALU.mult),
                             reads=SC2K + ["sif"], writes=SC2K)
                        o.op("dve", lambda e, dstt=dstt: e.tensor_reduce(out=dstt[:], in_=oh, axis=AX.X, op=ALU.add), reads=SC2K, writes=[dk])
                    o.op("dve", lambda e: e.scalar_tensor_tensor(out=eif[:].rearrange("p (h k) -> p h k", h=8), in0=Ii[:], scalar=128.0, in1=Jj[:], op0=ALU.mult, op1=ALU.add),
                         reads=["Ii", "Jj"], writes=["eif"])
                    if l > 0:
                        o.op("dve", lambda e: e.tensor_scalar(out=eif[:], in0=eif[:], scalar1=float(l * NEXP), scalar2=None, op0=ALU.add),
                             reads=["eif"], writes=["eif"])
                    o.op("dve", lambda e: e.tensor_copy(out=eidx[:], in_=eif[:]), reads=["eif"], writes=[ek])

                nuv = [0]

                def stageB(i, filler):
                    xt = xts[i % 2]; xk = "xt%d" % (i % 2)
                    eidx = eidxs[i % 2]; ek = "eidx%d" % (i % 2)
                    gate = gates[i % 2]; gk = "gate%d" % (i % 2)
                    hp_ = hps[i % 2]; hpk = "hps%d" % (i % 2)
                    o = S_()
                    LOOK = 8
                    order = list(range(128))
                    bufs = {}

                    def issue(sl):
                        b = nuv[0] % NB; nuv[0] += 1
                        bufs[sl] = b
                        ub = UV[b]
                        o.dma("pool", lambda e, ub=ub, sl=sl: e.indirect_dma_start(
                            out=ub[:], out_offset=None, in_=uvb[:, :],
                            in_offset=bass.IndirectOffsetOnAxis(ap=eidx[:, sl:sl + 1], axis=0)), reads=[ek, ("uvb", l)], writes=["UV%d" % b])

                    for sl in range(min(LOOK, 128)):
                        issue(sl)
                    GS = 2

                    def finish(g0):
                        hs = slice(g0, g0 + GS)
                        wk = "wgt%d" % (g0 // GS)
                        o.op("dve", lambda e, hs=hs: e.tensor_tensor(out=wgt[:, hs], in0=wgt[:, hs], in1=gate[:, hs], op=ALU.mult), reads=[wk, gk], writes=[wk])
                        for sl in range(g0, g0 + GS):
                            b = bufs[sl]; ub = UV[b]; ubk = "UV%d" % b
                            dg = diag[sl % 2]; dgk = "diag%d" % (sl % 2)
                            o.op("act", lambda e, dg=dg, sl=sl: e.activation(out=dg[:], in_=ident_f[:], func=AF.Copy, scale=wgt[:, sl:sl + 1]),
                                 reads=["ident_f", wk], writes=[dgk])
                            for half in range(2):
                                mm(pso[half][:, :], dg[:], ub[:, D + half * 512: D + (half + 1) * 512], sl == 0, sl == 127, [dgk, ubk], ["psop%d" % half])

                    for g0 in range(0, 128, GS):
                        ak = "act%d" % (g0 // GS); wk = "wgt%d" % (g0 // GS)
                        for sl in range(g0, g0 + GS):
                            if sl + LOOK < 128:
                                issue(sl + LOOK)
                            b = bufs[sl]; ub = UV[b]; ubk = "UV%d" % b
                            o.op("dve", lambda e, ub=ub, sl=sl: e.scalar_tensor_tensor(out=dj[:], in0=ub[:, 0:D], scalar=1.0, in1=hp_[:], op0=ALU.mult, op1=ALU.mult,
                                                                                       accum_out=act[:, sl:sl + 1]),
                                 reads=[ubk, hpk], writes=[ak])
                            filler()
                        hs = slice(g0, g0 + GS)
                        o.op("act", lambda e, hs=hs: e.activation(out=wgt[:, hs], in_=act[:, hs], func=AF.Gelu), reads=[ak], writes=[wk])
                        if g0 > 0:
                            finish(g0 - GS)
                    finish(128 - GS)
                    residual_out(pso, ["psop0", "psop1"], G2, xt, xk, xo, dst, i, fin=fin_t)

                stageA(0)
                for i in range(NT):
                    nxt = Deferred()
                    if i + 1 < NT:
                        sink[0] = nxt
                        stageA(i + 1)
                        sink[0] = s
                    per = (len(nxt.q) + 127) // 128
                    stageB(i, lambda: nxt.run(s, per))
                    nxt.run(s, None)
                s.flush()

        cur = IN["x"]
        free = [0, 1, 2]

        def take(exclude):
            for b in free:
                if xs[b] is not exclude and all(xs[b] is not e_ for e_ in exclude if e_ is not None):
                    return xs[b]
            raise RuntimeError("no buffer")

        nl = len(layers)
        for li, l in enumerate(layers):
            compute_mod(l)
            if do_mixer:
                if l % 2 == 0:
                    d1 = [b for b in xs if b is not cur][0]
                    even_mixer(l, cur, d1)
                    cur = d1
                else:
                    others = [b for b in xs if b is not cur]
                    odd_mixer(l, cur, cur, others[0], 0)
                    odd_mixer(l, cur, others[0], others[1], 1)
                    cur = others[1]
            if do_peer:
                last = (li == nl - 1)
                d2 = y_out if last else [b for b in xs if b is not cur][0]
                peer_pass(l, cur, d2, fin=(last and final_norm))
                cur = d2
        if cur is not y_out:
            with contextlib.ExitStack() as ph:
                tt = T("cp", [128, D], stack=ph)
                for i in range(NT):
                    ld(tt[:], cur[i * 128:(i + 1) * 128, :], ["cp"], r=[(cur.tensor.name, i)])
                    ld(y_out[i * 128:(i + 1) * 128, :], tt[:], [("y", i)], r=["cp"])
                s.flush()
        s.wait_all("sp")
        s.flush()
        build.n_instr = s.n_instr
    return nc


def make_in_maps(inputs, n_cores, S=4096):
    consts = host_consts()
    shared = {}
    for k in IN_SHAPES:
        if k in consts:
            shared[k] = consts[k]
        else:
            shared[k] = np.ascontiguousarray(np.asarray(inputs[k], dtype=np.float32)).reshape(IN_SHAPES[k][0])
    maps = []
    x = np.asarray(inputs["x"], dtype=np.float32)
    c = np.asarray(inputs["c"], dtype=np.float32)
    pos = np.asarray(inputs["positions"]).astype(np.int32)
    for b in range(n_cores):
        m = dict(shared)
        m["x"] = np.ascontiguousarray(x[b, :S])
        m["c"] = np.ascontiguousarray(c[b:b + 1])
        m["positions"] = np.ascontiguousarray(pos[b:b + 1, :S])
        maps.append(m)
    return maps


def kernel(**inputs):
    n = 8
    nc = build(4096)
    maps = make_in_maps(inputs, n)
    res = run_bass_kernel_spmd(nc, maps, core_ids=list(range(n)))
    return np.stack([np.asarray(r["y"], dtype=np.float32) for r in res.results], axis=0)
```
